# Optimizing a Trainium2 kernel written in Bass

```python
import math
import jax, jax.numpy as jnp
from jax import lax
import numpy as np

D_MODEL = 1024
BATCH = 32
SEQ = 2048
DEPTH = 2

HEAD_DIM = 64
SB_HEADS = 8
MB_HEADS = 8
SB_W = SB_HEADS * HEAD_DIM
MB_W = MB_HEADS * HEAD_DIM
CONV_CH = D_MODEL // 2
CONV_WIDTH = 31
MOBA_BLOCK = 256
MOBA_TOPK = 3
Q_BLOCK = 128
MOBA_Q_CHUNK = 32
D_FF = 2816
N_BRANCH = 3
EPS = 1e-6

OFF_SBQ = 0
OFF_SBK = OFF_SBQ + SB_W
OFF_SBV = OFF_SBK + SB_W
OFF_MBQ = OFF_SBV + SB_W
OFF_MBK = OFF_MBQ + MB_W
OFF_MBV = OFF_MBK + MB_W
OFF_CONV = OFF_MBV + MB_W
OFF_GATE = OFF_CONV + 2 * CONV_CH
IN_COLS = OFF_GATE + N_BRANCH * D_MODEL

kernel_name = "hybrid_stickbreak_moba_conformer_macaron"


def rmsnorm(x, g):
    xf = x.astype(jnp.float32)
    y = xf * lax.rsqrt(jnp.mean(xf * xf, axis=-1, keepdims=True) + EPS)
    return (y * g.astype(jnp.float32)).astype(x.dtype)


def layernorm(x, g, b):
    xf = x.astype(jnp.float32)
    mu = jnp.mean(xf, axis=-1, keepdims=True)
    var = jnp.mean(jnp.square(xf - mu), axis=-1, keepdims=True)
    y = (xf - mu) * lax.rsqrt(var + EPS)
    return (y * g.astype(jnp.float32) + b.astype(jnp.float32)).astype(x.dtype)


def swiglu(x, w_in, w_out):
    a = x @ w_in
    return (jax.nn.silu(a[..., :D_FF]) * a[..., D_FF:]) @ w_out


def to_heads(a, n_heads):
    b, s, _ = a.shape
    return a.reshape(b, s, n_heads, HEAD_DIM).transpose(0, 2, 1, 3)


def from_heads(a):
    b, h, s, d = a.shape
    return a.transpose(0, 2, 1, 3).reshape(b, s, h * d)


def stick_breaking_attention(q, k, v):
    B, H, S, Dh = q.shape
    n_blk = -(-S // Q_BLOCK)
    s_pad = n_blk * Q_BLOCK
    qp = jnp.pad(q, ((0, 0), (0, 0), (0, s_pad - S), (0, 0)))
    scale = HEAD_DIM ** -0.5
    key_pos = jnp.arange(S)

    def block(start):
        qb = lax.dynamic_slice_in_dim(qp, start, Q_BLOCK, axis=2)
        t = start + jnp.arange(Q_BLOCK)
        z = jnp.einsum('bhqd,bhkd->bhqk', qb, k).astype(jnp.float32) * scale
        past = key_pos[None, :] < t[:, None]
        log_beta = jax.nn.log_sigmoid(z)
        log_1m = jnp.where(past, jax.nn.log_sigmoid(-z), 0.0)
        between = lax.cumsum(log_1m, axis=3, reverse=True) - log_1m
        w = jnp.where(past, jnp.exp(log_beta + between), 0.0)
        return jnp.einsum('bhqk,bhkd->bhqd', w.astype(v.dtype), v)

    starts = jnp.arange(n_blk, dtype=jnp.int32) * Q_BLOCK
    out = lax.map(block, starts)
    out = out.transpose(1, 2, 0, 3, 4).reshape(B, H, s_pad, Dh)
    return out[:, :, :S]


def moba_attention(q, k, v, slopes):
    B, H, S, Dh = q.shape
    nb = -(-S // MOBA_BLOCK)
    s_pad = nb * MOBA_BLOCK
    pad = ((0, 0), (0, 0), (0, s_pad - S), (0, 0))
    qp, kp, vp = jnp.pad(q, pad), jnp.pad(k, pad), jnp.pad(v, pad)
    kblk = kp.reshape(B, H, nb, MOBA_BLOCK, Dh)
    vblk = vp.reshape(B, H, nb, MOBA_BLOCK, Dh)
    kmean = jnp.mean(kblk.astype(jnp.float32), axis=3)
    n_sel = min(MOBA_TOPK, nb - 1)
    scale = HEAD_DIM ** -0.5
    bi = jnp.arange(B)[:, None, None, None]
    hi = jnp.arange(H)[None, :, None, None]
    blk_pos = jnp.arange(MOBA_BLOCK)

    def chunk(start):
        own = start // MOBA_BLOCK
        qb = lax.dynamic_slice_in_dim(qp, start, MOBA_Q_CHUNK, axis=2)
        t = start + jnp.arange(MOBA_Q_CHUNK)
        k_own = lax.dynamic_index_in_dim(kblk, own, axis=2, keepdims=False)
        v_own = lax.dynamic_index_in_dim(vblk, own, axis=2, keepdims=False)
        s_own = own * MOBA_BLOCK + blk_pos
        dist_own = (t[:, None] - s_own[None, :]).astype(jnp.float32)
        sc_own = (jnp.einsum('bhqd,bhkd->bhqk', qb, k_own).astype(jnp.float32) * scale
                  - slopes[:, None, None] * dist_own)
        sc_own = jnp.where(s_own[None, :] <= t[:, None], sc_own, -jnp.inf)
        if n_sel > 0:
            gate = jnp.einsum('bhqd,bhnd->bhqn', qb.astype(jnp.float32), kmean)
            gate = jnp.where(jnp.arange(nb) < own, gate, -jnp.inf)
            _, idx = lax.top_k(gate, n_sel)
            k_sel = kblk[bi, hi, idx]
            v_sel = vblk[bi, hi, idx]
            s_sel = idx[..., None] * MOBA_BLOCK + blk_pos
            dist_sel = (t[:, None, None] - s_sel).astype(jnp.float32)
            sc_sel = (jnp.einsum('bhqd,bhqnkd->bhqnk', qb, k_sel).astype(jnp.float32) * scale
                      - slopes[:, None, None, None] * dist_sel)
            sc_sel = jnp.where((idx < own)[..., None], sc_sel, -jnp.inf)
            n_k = n_sel * MOBA_BLOCK
            sc = jnp.concatenate([sc_sel.reshape(B, H, MOBA_Q_CHUNK, n_k), sc_own], axis=-1)
            p = jax.nn.softmax(sc, axis=-1).astype(v.dtype)
            p_sel = p[..., :n_k].reshape(B, H, MOBA_Q_CHUNK, n_sel, MOBA_BLOCK)
            o = (jnp.einsum('bhqnk,bhqnkd->bhqd', p_sel, v_sel)
                 + jnp.einsum('bhqk,bhkd->bhqd', p[..., n_k:], v_own))
        else:
            p = jax.nn.softmax(sc_own, axis=-1).astype(v.dtype)
            o = jnp.einsum('bhqk,bhkd->bhqd', p, v_own)
        return o

    n_chunks = s_pad // MOBA_Q_CHUNK
    starts = jnp.arange(n_chunks, dtype=jnp.int32) * MOBA_Q_CHUNK
    out = lax.map(chunk, starts)
    out = out.transpose(1, 2, 0, 3, 4).reshape(B, H, s_pad, Dh)
    return out[:, :, :S]


def conformer_conv(a, w_dw, b_dw, ln_g, ln_b, w_pw):
    h = a[..., :CONV_CH] * jax.nn.sigmoid(a[..., CONV_CH:])
    h = lax.conv_general_dilated(h, w_dw, window_strides=(1,),
                                 padding=[(CONV_WIDTH - 1, 0)],
                                 dimension_numbers=('NWC', 'WIO', 'NWC'),
                                 feature_group_count=CONV_CH) + b_dw
    h = jax.nn.silu(layernorm(h, ln_g, ln_b))
    return h @ w_pw


def alibi_slopes(n_heads):
    return jnp.exp2(-8.0 * jnp.arange(1, n_heads + 1, dtype=jnp.float32) / n_heads)


def setup_inputs(seed: int = 0) -> dict:
    key = jax.random.key(seed)
    ks = jax.random.split(key, 24)
    L, D = DEPTH, D_MODEL

    def nrm(k, shape, fan_in):
        return jax.random.normal(k, shape, jnp.float32) * (fan_in ** -0.5)

    def gain(k, shape):
        return 1.0 + 0.02 * jax.random.normal(k, shape, jnp.float32)

    def small(k, shape):
        return 0.01 * jax.random.normal(k, shape, jnp.float32)

    return {
        "x": jax.random.normal(ks[0], (BATCH, SEQ, D), jnp.float32),
        "ffn1_norm": gain(ks[1], (L, D)),
        "ffn1_w_in": nrm(ks[2], (L, D, 2 * D_FF), D),
        "ffn1_w_out": nrm(ks[3], (L, D_FF, D), D_FF),
        "mix_norm": gain(ks[4], (L, D)),
        "w_in": nrm(ks[5], (L, D, IN_COLS), D),
        "gate_bias": small(ks[6], (L, N_BRANCH * D)),
        "sb_w_out": nrm(ks[7], (L, SB_W, D), SB_W),
        "mb_w_out": nrm(ks[8], (L, MB_W, D), MB_W),
        "conv_dw": nrm(ks[9], (L, CONV_WIDTH, 1, CONV_CH), CONV_WIDTH),
        "conv_dw_bias": small(ks[10], (L, CONV_CH)),
        "conv_ln_g": gain(ks[11], (L, CONV_CH)),
        "conv_ln_b": small(ks[12], (L, CONV_CH)),
        "conv_w_out": nrm(ks[13], (L, CONV_CH, D), CONV_CH),
        "w_o": nrm(ks[14], (L, D, D), D),
        "ffn2_norm": gain(ks[15], (L, D)),
        "ffn2_w_in": nrm(ks[16], (L, D, 2 * D_FF), D),
        "ffn2_w_out": nrm(ks[17], (L, D_FF, D), D_FF),
        "final_norm": gain(ks[18], (D,)),
    }


def reference(x, ffn1_norm, ffn1_w_in, ffn1_w_out, mix_norm, w_in, gate_bias,
              sb_w_out, mb_w_out, conv_dw, conv_dw_bias, conv_ln_g, conv_ln_b,
              conv_w_out, w_o, ffn2_norm, ffn2_w_in, ffn2_w_out, final_norm):
    slopes = alibi_slopes(MB_HEADS)
    h = x
    for l in range(DEPTH):
        h = h + 0.5 * swiglu(rmsnorm(h, ffn1_norm[l]), ffn1_w_in[l], ffn1_w_out[l])
        u = rmsnorm(h, mix_norm[l])
        p = u @ w_in[l]
        q_a = to_heads(p[..., OFF_SBQ:OFF_SBK], SB_HEADS)
        k_a = to_heads(p[..., OFF_SBK:OFF_SBV], SB_HEADS)
        v_a = to_heads(p[..., OFF_SBV:OFF_MBQ], SB_HEADS)
        q_b = to_heads(p[..., OFF_MBQ:OFF_MBK], MB_HEADS)
        k_b = to_heads(p[..., OFF_MBK:OFF_MBV], MB_HEADS)
        v_b = to_heads(p[..., OFF_MBV:OFF_CONV], MB_HEADS)
        y_a = from_heads(stick_breaking_attention(q_a, k_a, v_a)) @ sb_w_out[l]
        y_b = from_heads(moba_attention(q_b, k_b, v_b, slopes)) @ mb_w_out[l]
        y_c = conformer_conv(p[..., OFF_CONV:OFF_GATE], conv_dw[l], conv_dw_bias[l],
                             conv_ln_g[l], conv_ln_b[l], conv_w_out[l])
        g = jax.nn.sigmoid(p[..., OFF_GATE:] + gate_bias[l])
        mixed = (g[..., :D_MODEL] * y_a + g[..., D_MODEL:2 * D_MODEL] * y_b
                 + g[..., 2 * D_MODEL:] * y_c)
        h = h + mixed @ w_o[l]
        h = h + 0.5 * swiglu(rmsnorm(h, ffn2_norm[l]), ffn2_w_in[l], ffn2_w_out[l])
    return rmsnorm(h, final_norm)
```

```python
import contextlib
import numpy as np
import concourse.bass as bass
import concourse.mybir as mybir
from concourse.bass_utils import run_bass_kernel_spmd

F32 = mybir.dt.float32
BF16 = mybir.dt.bfloat16
AF = mybir.ActivationFunctionType
ALU = mybir.AluOpType
AX = mybir.AxisListType

D = 1024
S = 2048
DFF = 2816
NFC = 22
DEPTH_FULL = 2
NCORES = 8
SEQ_PER_CORE = 4
OFF_SBQ, OFF_SBK, OFF_SBV = 0, 512, 1024
OFF_MBQ, OFF_MBK, OFF_MBV = 1536, 2048, 2560
OFF_CONV = 3072
OFF_GATE = 4096
IN_COLS = 7168
EPS = 1e-6
BIG = 29952.0
SLOPES = [2.0 ** (-(h + 1)) for h in range(8)]
ENGS = ("pe", "act", "dve", "pool", "sp")


class Op:
    __slots__ = ("eng", "fn", "pos", "sig", "waits", "dma", "val")

    def __init__(self, eng, fn, dma=None):
        self.eng, self.fn, self.dma = eng, fn, dma
        self.pos, self.sig, self.waits, self.val = -1, False, [], 0


class Prog:
    def __init__(self):
        self.streams = {e: [] for e in ENGS}
        self.last_w, self.readers = {}, {}
        self.seen = {e: {} for e in ENGS}
        self.dma_cnt = {}
        self.fences = {}

    def _need(self, x, y, raw):
        if y is None or y is x:
            return
        if y.dma is not None:
            key = ("d", y.dma)
            val = self.dma_cnt[y.dma] - (16 if x.dma == y.dma else 0)
            if self.seen[x.eng].get(key, 0) >= val:
                return
            self.seen[x.eng][key] = val
            x.waits.append(("d", y.dma, val))
            return
        if y.eng == x.eng and x.dma is None:
            if x.eng == "pe" or not raw:
                return
            if len(self.streams[x.eng]) - y.pos > 3:
                return
        key = ("e", y.eng)
        if self.seen[x.eng].get(key, -1) >= y.pos:
            return
        self.seen[x.eng][key] = y.pos
        y.sig = True
        x.waits.append(("e", y.eng, y))

    def fence(self, block):
        ops = {}
        for k in [k for k in self.last_w if k[0] == block]:
            o = self.last_w.pop(k)
            ops[id(o)] = o
        for k in [k for k in self.readers if k[0] == block]:
            for o in self.readers.pop(k):
                ops[id(o)] = o
        best = {}
        for o in ops.values():
            kk = (o.eng, o.dma)
            rank = o.val if o.dma is not None else o.pos
            if kk not in best or rank > best[kk][0]:
                best[kk] = (rank, o)
        if best:
            self.fences[block] = [v[1] for v in best.values()]

    def add(self, eng, fn, reads=(), writes=(), dma=None):
        x = Op(eng, fn, dma)
        if dma is not None:
            self.dma_cnt[dma] = self.dma_cnt.get(dma, 0) + 16
            x.val = self.dma_cnt[dma]
        reads = list(reads)
        writes = list(writes)
        for r in list(reads):
            if r[0] == "ps":
                reads.remove(r)
                writes.append(r)
        for k in reads + writes:
            if k not in self.last_w and k[0] in self.fences:
                for o in self.fences[k[0]]:
                    self._need(x, o, True)
        for r in reads:
            self._need(x, self.last_w.get(r), True)
        for w in writes:
            self._need(x, self.last_w.get(w), True)
            for rd in self.readers.get(w, ()):
                self._need(x, rd, False)
        x.pos = len(self.streams[eng])
        self.streams[eng].append(x)
        for r in reads:
            self.readers.setdefault(r, []).append(x)
        for w in writes:
            self.last_w[w] = x
            self.readers[w] = []
        return x

    def emit(self, nc, final_waits=()):
        with contextlib.ExitStack() as es:
            esem = {e: es.enter_context(nc.semaphore("s_" + e)) for e in ENGS}
            dsem = {n: es.enter_context(nc.semaphore("d_" + n)) for n in self.dma_cnt}
            block = es.enter_context(nc.Block())
            for e in ENGS:
                c = 0
                for op in self.streams[e]:
                    if op.dma is None:
                        if op.sig:
                            c += 1
                        op.val = c
            hooks = {"pe": block.tensor, "act": block.scalar, "dve": block.vector,
                     "pool": block.gpsimd, "sp": block.sync}

            def mk(e):
                def body(eng):
                    for op in self.streams[e]:
                        for w in op.waits:
                            if w[0] == "d":
                                eng.wait_ge(dsem[w[1]], w[2])
                            else:
                                eng.wait_ge(esem[w[1]], w[2].val)
                        ins = op.fn(eng)
                        if op.dma is not None:
                            ins.then_inc(dsem[op.dma], 16)
                        elif op.sig:
                            ins.then_inc(esem[e], 1)
                    if e == "sp":
                        for op in final_waits:
                            eng.wait_ge(dsem[op.dma], op.val)
                return body
            for e in ENGS:
                hooks[e](mk(e))


NVEC_L = 8 + 8 + 8 + 24 + 4 + 4 + 4 + 124
V_FFN1N, V_MIXN, V_FFN2N, V_GB, V_DWB, V_LNG, V_LNB, V_DW = 0, 8, 16, 24, 48, 52, 56, 60
C_TRI, C_MLT, C_MLE, C_ID, C_GM, C_L = 0, 128, 256, 384, 512, 640
NCST = 704


def host_consts():
    c = np.zeros((128, NCST), np.float32)
    j = np.arange(128)[:, None]
    s = np.arange(128)[None, :]
    c[:, C_TRI:C_TRI + 128] = (j >= s)
    c[:, C_MLT:C_MLT + 128] = (j < s)
    c[:, C_MLE:C_MLE + 128] = (j <= s)
    c[:, C_ID:C_ID + 128] = (j == s)
    own = np.arange(8)[:, None]
    n = np.arange(8)[None, :]
    gm = np.where(n < own, 0.0, -BIG).astype(np.float32)
    ll = np.where(n < own, -BIG, 0.0).astype(np.float32)
    c[:, C_GM:C_GM + 128] = np.repeat(gm[:, None, :], 2, axis=1).reshape(1, 128)
    c[:, C_L:C_L + 64] = ll.reshape(1, 64)
    kst = np.zeros((128, S), np.float32)
    pos = np.arange(S)
    for nn in range(8):
        kst[64 + nn] = (pos // 256 == nn)
    kst[72] = 1.0
    kst[73] = 1.0
    kst[74] = pos % 128
    sel = np.zeros((128, 8, 4, 80), np.float32)
    q = np.arange(128)
    for h in range(8):
        for m in range(4):
            i = m * 128 + q
            sel[:, h, m, 72] = -8.0 * SLOPES[h] * (256 * (i // 256))
            sel[:, h, m, 73] = -8.0 * SLOPES[h] * (i % 256)
            sel[:, h, m, 74] = 8.0 * SLOPES[h]
    return c, kst, sel.reshape(128, 8 * 4 * 80)


def host_vecs(inp, depth):
    def col(v):
        return np.ascontiguousarray(np.asarray(v, np.float32).reshape(-1, 128).T)
    cols = []
    for l in range(depth):
        cols += [col(inp["ffn1_norm"][l]), col(inp["mix_norm"][l]), col(inp["ffn2_norm"][l]),
                 col(inp["gate_bias"][l]), col(inp["conv_dw_bias"][l]), col(inp["conv_ln_g"][l]),
                 col(inp["conv_ln_b"][l])]
        dw = np.asarray(inp["conv_dw"][l], np.float32).reshape(31, 512)
        cols.append(np.ascontiguousarray(dw.reshape(31, 4, 128).transpose(2, 0, 1).reshape(128, 124)))
    cols.append(col(inp["final_norm"]))
    return np.ascontiguousarray(np.concatenate(cols, axis=1))


def build(nseq=SEQ_PER_CORE, depth=DEPTH_FULL, stages=("ffn1", "mix", "ffn2", "final"),
          mix_parts=("conv", "sb", "mb"), dbg=None):
    nc = bass.Bass("TRN2", target_bir_lowering=False)
    dram = {}

    def din(name, shape):
        dram[name] = nc.dram_tensor(name, list(shape), F32, kind="ExternalInput").ap()
        return dram[name]

    xT = din("xT", [nseq, D, S])
    W1i = din("ffn1_w_in", [depth, D, 2 * DFF])
    W1o = din("ffn1_w_out", [depth, DFF, D])
    W2i = din("ffn2_w_in", [depth, D, 2 * DFF])
    W2o = din("ffn2_w_out", [depth, DFF, D])
    Win = din("w_in", [depth, D, IN_COLS])
    WA = din("sb_w_out", [depth, 512, D])
    WB = din("mb_w_out", [depth, 512, D])
    WC = din("conv_w_out", [depth, 512, D])
    WO = din("w_o", [depth, D, D])
    NV = depth * NVEC_L + 8
    vecs_d = din("vecs", [128, NV])
    cst_d = din("cst", [128, NCST])
    kst_d = din("kst", [128, S])
    sel_d = din("selst", [128, 8 * 4 * 80])
    outT = nc.dram_tensor("outT", [nseq, D, S], F32, kind="ExternalOutput").ap()
    dbg_d = nc.dram_tensor("dbg", [128, 8192], F32, kind="ExternalOutput").ap() if dbg else None
    dbg_ops = []

    def dump(name, ap, keys):
        if dbg == name and not dbg_ops:
            n = ap.shape[1]
            dbg_ops.append(P.add("pool", lambda e: e.dma_start(out=dbg_d[:, 0:n], in_=ap), reads=keys, dma="dbg"))

    P = Prog()
    es = contextlib.ExitStack()
    with es:
        def sb(name, shape, dt):
            return es.enter_context(nc.sbuf_tensor(name, list(shape), dt))

        h = sb("h", [128, 8, S], F32)
        xn = sb("xn", [128, 8, S], BF16)
        bigA = sb("bigA", [128, 4, S], BF16)
        bigB = sb("bigB", [128, 4, S], BF16)
        bigC = sb("bigC", [128, 4, S], BF16)
        scr = sb("scr", [128, 8192], F32)
        NW = 4
        WSZ = 2048
        wsl = [sb("w%d" % i, [128, WSZ], BF16) for i in range(NW)]
        vecs = sb("vecs_sb", [128, NV], F32)
        cst = sb("cst_sb", [128, NCST], F32)
        cb = sb("cst_bf", [128, 512], BF16)
        ones_d = sb("ones_d", [128, 128], BF16)
        ones_c = sb("ones_c", [128, 128], BF16)
        ones1 = sb("ones1", [128, 128], BF16)
        zer = sb("zer", [128, 128], BF16)
        eps_c = sb("eps_c", [128, 1], F32)
        kaug = scr[:, 2048:4096].bitcast(BF16).rearrange("p (j n) -> p j n", j=2)
        qaug = scr[:, 4096:6144].bitcast(BF16).rearrange("p (j n) -> p j n", j=2)
        vext = scr[:, 6144:8192].bitcast(BF16).rearrange("p (t j n) -> p t j n", t=16, j=2)
        selT = sb("selT", [128, 8, 4, 80], BF16)
        ksumb = sb("ksumb", [64, 2, 8], BF16)
        ps = [es.enter_context(nc.psum_tensor("ps%d" % i, [128, 512], F32)) for i in range(8)]

        class RR:
            def __init__(self, ids):
                self.ids, self.i = list(ids), 0

            def __call__(self):
                b = self.ids[self.i % len(self.ids)]
                self.i += 1
                return b
        rr8 = RR(range(8))
        rr6 = RR(range(6))
        rrO = RR([6, 7])
        wstate = {"i": 0}

        def wload(parts, eng="pool"):
            si = wstate["i"] % NW
            wstate["i"] += 1
            P.fence("w%d" % si)
            off = 0
            views, keys = [], []
            for pi, ap in enumerate(parts):
                K, n = ap.shape[1], ap.shape[2]
                v = wsl[si][:, off:off + K * n].rearrange("p (k n) -> p k n", k=K)
                key = ("w%d" % si, pi)
                P.add(eng, lambda e, v=v, ap=ap: e.dma_start(out=v, in_=ap), writes=[key], dma="w%d" % si)
                views.append(v)
                keys.append(key)
                off += K * n
            assert off <= WSZ
            return keys, views

        def wview(Wd, l, r0, nrow, c0, ncol):
            return Wd[l, r0:r0 + nrow, c0:c0 + ncol].rearrange("(k p) n -> p k n", p=128)

        def vcol(l, base, i):
            c = l * NVEC_L + base + i
            return vecs[:, c:c + 1]

        def TT(t):
            return slice(t * 512, (t + 1) * 512)

        P.add("sp", lambda e: e.dma_start(out=vecs[:], in_=vecs_d), writes=[("vecs",)], dma="c0")
        P.add("sp", lambda e: e.dma_start(out=cst[:], in_=cst_d), writes=[("cst",)], dma="c0")
        P.add("dve", lambda e: e.tensor_copy(out=cb[:], in_=cst[:, 0:512]), reads=[("cst",)], writes=[("cb",)])
        P.add("dve", lambda e: e.memset(ones_d[:], 1.0 / 1024), writes=[("ones",)])
        P.add("dve", lambda e: e.memset(ones_c[:], 1.0 / 512), writes=[("ones",)])
        P.add("dve", lambda e: e.memset(ones1[:], 1.0), writes=[("ones",)])
        P.add("dve", lambda e: e.memset(zer[:], 0.0), writes=[("ones",)])
        P.add("dve", lambda e: e.memset(eps_c[:], EPS), writes=[("ones",)])
        P.add("pool", lambda e: e.dma_start(out=selT[:].rearrange("p a b c -> p (a b c)"), in_=sel_d),
              writes=[("selst",)], dma="c1")
        TRI = cb[:, C_TRI:C_TRI + 128]
        MLE = cb[:, C_MLE:C_MLE + 128]
        IDN = cb[:, C_ID:C_ID + 128]
        MLT = cst[:, C_MLT:C_MLT + 128]

        def rmsnorm(l, base, out_bf=True, out_f32=None):
            P.fence("scr")
            sqb = scr[:, 0:2048].bitcast(BF16).rearrange("p (k n) -> p k n", k=8)
            rstd = scr[:, 2048:2560]
            for t in range(4):
                for dc in range(8):
                    P.add("act", lambda e, dc=dc, t=t: e.activation(out=sqb[:, dc, :], in_=h[:, dc, TT(t)], func=AF.Square),
                          reads=[("h", dc, t)], writes=[("scr", "sq", dc)])
                b = rr8()
                for dc in range(8):
                    P.add("pe", lambda e, dc=dc, b=b: e.matmul(ps[b][:], lhsT=ones_d[:], rhs=sqb[:, dc, :],
                                                               start=(dc == 0), stop=(dc == 7)),
                          reads=[("scr", "sq", dc), ("ones",)], writes=[("ps", b)])
                P.add("act", lambda e, b=b: e.activation(out=rstd, in_=ps[b][:], func=AF.Sqrt, bias=eps_c[:, 0:1]),
                      reads=[("ps", b), ("ones",)], writes=[("scr", "rstd")])
                P.add("dve", lambda e: e.reciprocal(out=rstd, in_=rstd),
                      reads=[("scr", "rstd")], writes=[("scr", "rstd")])
                for dc in range(8):
                    if out_f32 is None:
                        o = xn[:, dc, TT(t)]
                        wk = ("xn", dc, t)
                    else:
                        o = out_f32[:, dc, TT(t)]
                        wk = ("h", dc, t)
                    c = base + dc if l is None else l * NVEC_L + base + dc
                    P.add("dve", lambda e, o=o, dc=dc, t=t, c=c: e.scalar_tensor_tensor(
                        out=o, in0=h[:, dc, TT(t)], scalar=vecs[:, c:c + 1], in1=rstd, op0=ALU.mult, op1=ALU.mult),
                        reads=[("h", dc, t), ("scr", "rstd"), ("vecs",)], writes=[wk])

        def ffn(l, Wi, Wo, nbase):
            rmsnorm(l, nbase)
            P.fence("scr")
            P.fence("bigA")
            P.fence("bigB")
            sil = [scr[:, 2560:3072], scr[:, 3072:3584]]
            groups = [(0, 8), (8, 7), (15, 7)]
            sidx = 0
            for (g0, gn) in groups:
                def hid(c, t):
                    return (bigA if c < 4 else bigB)[:, c % 4, TT(t)]
                for c0 in range(0, gn, 1):
                    ncnk = 1
                    fc = g0 + c0
                    keys, (wa, wb) = wload([wview(Wi, l, 0, D, fc * 128, ncnk * 128),
                                            wview(Wi, l, 0, D, DFF + fc * 128, ncnk * 128)])
                    for ci in range(ncnk):
                        c = c0 + ci
                        for t in range(4):
                            ba, bb = rr8(), rr8()
                            for kc in range(8):
                                P.add("pe", lambda e, kc=kc, t=t, ba=ba, ci=ci, wa=wa: e.matmul(
                                    ps[ba][:], lhsT=wa[:, kc, ci * 128:(ci + 1) * 128], rhs=xn[:, kc, TT(t)],
                                    start=(kc == 0), stop=(kc == 7)),
                                    reads=[keys[0], ("xn", kc, t)], writes=[("ps", ba)])
                            for kc in range(8):
                                P.add("pe", lambda e, kc=kc, t=t, bb=bb, ci=ci, wb=wb: e.matmul(
                                    ps[bb][:], lhsT=wb[:, kc, ci * 128:(ci + 1) * 128], rhs=xn[:, kc, TT(t)],
                                    start=(kc == 0), stop=(kc == 7)),
                                    reads=[keys[1], ("xn", kc, t)], writes=[("ps", bb)])
                            st = sil[sidx % 2]
                            sk = ("scr", "sil", sidx % 2)
                            sidx += 1
                            P.add("act", lambda e, st=st, ba=ba: e.activation(out=st, in_=ps[ba][:], func=AF.Silu),
                                  reads=[("ps", ba)], writes=[sk])
                            P.add("dve", lambda e, st=st, bb=bb, c=c, t=t: e.tensor_tensor(
                                out=hid(c, t), in0=st, in1=ps[bb][:], op=ALU.mult),
                                reads=[sk, ("ps", bb)], writes=[("bigA" if c < 4 else "bigB", c % 4, t)])
                for d0 in range(0, 8, 2):
                    keys, (wo,) = wload([wview(Wo, l, g0 * 128, gn * 128, d0 * 128, 256)])
                    for di in range(2):
                        dc = d0 + di
                        for t in range(4):
                            b = rr8()
                            for c in range(gn):
                                P.add("pe", lambda e, c=c, t=t, b=b, di=di, wo=wo, gn=gn: e.matmul(
                                    ps[b][:], lhsT=wo[:, c, di * 128:(di + 1) * 128], rhs=hid(c, t),
                                    start=(c == 0), stop=(c == gn - 1)),
                                    reads=[keys[0], ("bigA" if c < 4 else "bigB", c % 4, t)], writes=[("ps", b)])
                            P.add("dve", lambda e, b=b, dc=dc, t=t: e.scalar_tensor_tensor(
                                out=h[:, dc, TT(t)], in0=ps[b][:], scalar=0.5, in1=h[:, dc, TT(t)],
                                op0=ALU.mult, op1=ALU.add),
                                reads=[("ps", b)], writes=[("h", dc, t)])

        def proj_fm(keyw, wv, evac):
            for t in range(4):
                b = rr8()
                for kc in range(8):
                    P.add("pe", lambda e, kc=kc, t=t, b=b: e.matmul(ps[b][:], lhsT=wv[:, kc, :], rhs=xn[:, kc, TT(t)],
                                                                     start=(kc == 0), stop=(kc == 7)),
                          reads=[keyw, ("xn", kc, t)], writes=[("ps", b)])
                evac(t, b)

        def conv_branch(l):
            for blk in ("bigA", "bigB", "bigC", "scr"):
                P.fence(blk)
            glu = [bigB[:, 0:2, :].bitcast(F32), bigB[:, 2:4, :].bitcast(F32),
                   bigC[:, 0:2, :].bitcast(F32), bigC[:, 2:4, :].bitcast(F32)]
            glu = [g.rearrange("p a n -> p (a n)") for g in glu]
            gkey = [("bigB", "g0"), ("bigB", "g1"), ("bigC", "g2"), ("bigC", "g3")]
            sg = [scr[:, 0:512], scr[:, 512:1024]]
            si = 0
            for cc in range(4):
                keys, (wv, wg) = wload([wview(Win, l, 0, D, OFF_CONV + cc * 128, 128),
                                        wview(Win, l, 0, D, OFF_CONV + 512 + cc * 128, 128)])
                for t in range(4):
                    bv, bg = rr8(), rr8()
                    for kc in range(8):
                        P.add("pe", lambda e, kc=kc, t=t, bv=bv, wv=wv: e.matmul(
                            ps[bv][:], lhsT=wv[:, kc, :], rhs=xn[:, kc, TT(t)], start=(kc == 0), stop=(kc == 7)),
                            reads=[keys[0], ("xn", kc, t)], writes=[("ps", bv)])
                    for kc in range(8):
                        P.add("pe", lambda e, kc=kc, t=t, bg=bg, wg=wg: e.matmul(
                            ps[bg][:], lhsT=wg[:, kc, :], rhs=xn[:, kc, TT(t)], start=(kc == 0), stop=(kc == 7)),
                            reads=[keys[1], ("xn", kc, t)], writes=[("ps", bg)])
                    s_ = sg[si % 2]
                    sk = ("scr", "sg", si % 2)
                    si += 1
                    P.add("act", lambda e, s_=s_, bg=bg: e.activation(out=s_, in_=ps[bg][:], func=AF.Sigmoid),
                          reads=[("ps", bg)], writes=[sk])
                    P.add("dve", lambda e, s_=s_, bv=bv, cc=cc, t=t: e.tensor_tensor(
                        out=glu[cc][:, TT(t)], in0=s_, in1=ps[bv][:], op=ALU.mult),
                        reads=[sk, ("ps", bv)], writes=[gkey[cc]])
            dump("glu", glu[0], [gkey[0]])
            for cpair in range(2):
                P.fence("scr")
                accs = [scr[:, 0:2048], scr[:, 2048:4096]]
                tmp = scr[:, 4096:4608]
                tmpb = scr[:, 4608:5120].bitcast(BF16)
                xcb = scr[:, 5120:6144].bitcast(BF16).rearrange("p (k n) -> p k n", k=4)
                rstd = scr[:, 6144:6656]
                for ci in range(2):
                    cc = cpair * 2 + ci
                    P.add("dve", lambda e, ci=ci, cc=cc: e.tensor_scalar(
                        out=accs[ci], in0=glu[cc], scalar1=vcol(l, V_DW, 30 * 4 + cc), scalar2=vcol(l, V_DWB, cc),
                        op0=ALU.mult, op1=ALU.add),
                        reads=[gkey[cc], ("vecs",)], writes=[("scr", "acc", ci)])
                for tap in range(30):
                    dsh = 30 - tap
                    for ci in range(2):
                        cc = cpair * 2 + ci
                        P.add("dve", lambda e, ci=ci, cc=cc, tap=tap, dsh=dsh: e.scalar_tensor_tensor(
                            out=accs[ci][:, dsh:S], in0=glu[cc][:, 0:S - dsh], scalar=vcol(l, V_DW, tap * 4 + cc),
                            in1=accs[ci][:, dsh:S], op0=ALU.mult, op1=ALU.add),
                            reads=[gkey[cc], ("vecs",), ("scr", "acc", ci)], writes=[("scr", "acc", ci)])
                for ci in range(2):
                    cc = cpair * 2 + ci
                    P.add("act", lambda e, ci=ci, cc=cc: e.copy(out=glu[cc], in_=accs[ci]),
                          reads=[("scr", "acc", ci)], writes=[gkey[cc]])
            dump("convout", glu[0], [gkey[0]])
            P.fence("scr")
            xb = scr[:, 0:1024].bitcast(BF16).rearrange("p (k n) -> p k n", k=4)
            xc = scr[:, 1024:3072].rearrange("p (k n) -> p k n", k=4)
            rstd = scr[:, 3072:3584]
            yt = scr[:, 3584:4096]
            for t in range(4):
                for cc in range(4):
                    P.add("act", lambda e, cc=cc, t=t: e.copy(out=xb[:, cc, :], in_=glu[cc][:, TT(t)]),
                          reads=[gkey[cc]], writes=[("scr", "xb", cc)])
                bm = rr8()
                for cc in range(4):
                    P.add("pe", lambda e, cc=cc, bm=bm: e.matmul(ps[bm][:], lhsT=ones_c[:], rhs=xb[:, cc, :],
                                                                 start=(cc == 0), stop=(cc == 3)),
                          reads=[("scr", "xb", cc), ("ones",)], writes=[("ps", bm)])
                for cc in range(4):
                    P.add("dve", lambda e, cc=cc, t=t, bm=bm: e.tensor_tensor(
                        out=xc[:, cc, :], in0=glu[cc][:, TT(t)], in1=ps[bm][:], op=ALU.subtract),
                        reads=[gkey[cc], ("ps", bm)], writes=[("scr", "xc", cc)])
                for cc in range(4):
                    P.add("act", lambda e, cc=cc: e.activation(out=xb[:, cc, :], in_=xc[:, cc, :], func=AF.Square),
                          reads=[("scr", "xc", cc)], writes=[("scr", "xb", cc)])
                bv = rr8()
                for cc in range(4):
                    P.add("pe", lambda e, cc=cc, bv=bv: e.matmul(ps[bv][:], lhsT=ones_c[:], rhs=xb[:, cc, :],
                                                                 start=(cc == 0), stop=(cc == 3)),
                          reads=[("scr", "xb", cc), ("ones",)], writes=[("ps", bv)])
                P.add("act", lambda e, bv=bv: e.activation(out=rstd, in_=ps[bv][:], func=AF.Sqrt, bias=eps_c[:, 0:1]),
                      reads=[("ps", bv), ("ones",)], writes=[("scr", "rstd")])
                P.add("dve", lambda e: e.reciprocal(out=rstd, in_=rstd),
                      reads=[("scr", "rstd")], writes=[("scr", "rstd")])
                for cc in range(4):
                    P.add("dve", lambda e, cc=cc: e.scalar_tensor_tensor(
                        out=xc[:, cc, :], in0=xc[:, cc, :], scalar=vcol(l, V_LNG, cc), in1=rstd,
                        op0=ALU.mult, op1=ALU.mult),
                        reads=[("scr", "xc", cc), ("scr", "rstd"), ("vecs",)], writes=[("scr", "xc", cc)])
                    P.add("act", lambda e, cc=cc, t=t: e.activation(
                        out=bigA[:, cc, TT(t)], in_=xc[:, cc, :], func=AF.Silu, bias=vcol(l, V_LNB, cc)),
                        reads=[("scr", "xc", cc), ("vecs",)], writes=[("bigA", cc, t)])
            dump("c", bigA[:].rearrange("p a n -> p (a n)"), [("bigA", cc, t) for cc in range(4) for t in range(4)])
            P.fence("bigB")
            P.fence("bigC")

        def sb_attention(l):
            P.fence("scr")
            P.fence("bigB")
            qT = scr[:, 0:1024].bitcast(BF16)
            kT = scr[:, 1024:2048].bitcast(BF16)
            vv = scr[:, 2048:3072].bitcast(BF16).rearrange("p (t n) -> p t n", t=16)
            Eb = [scr[:, 3072:3584], scr[:, 3584:4096]]
            Gb = [scr[:, 4096:4608], scr[:, 4608:5120]]
            SPb = [scr[:, 5120:5376].bitcast(BF16), scr[:, 5376:5632].bitcast(BF16)]
            SSb = [scr[:, 5632:5888].bitcast(BF16), scr[:, 5888:6144].bitcast(BF16)]
            Wb = [scr[:, 6144:6400].bitcast(BF16), scr[:, 6400:6656].bitcast(BF16)]
            cnt = {"e": 0, "ss": 0}
            for hp in range(4):
                keys, (wq, wk) = wload([wview(Win, l, 0, D, OFF_SBQ + hp * 128, 128),
                                        wview(Win, l, 0, D, OFF_SBK + hp * 128, 128)])
                keys2, (wv,) = wload([wview(Win, l, 0, D, OFF_SBV + hp * 128, 128)])
                keys = keys + keys2
                proj_fm(keys[0], wq, lambda t, b: P.add(
                    "act", lambda e, t=t, b=b: e.copy(out=qT[:, TT(t)], in_=ps[b][:]),
                    reads=[("ps", b)], writes=[("scr", "q", t)]))
                proj_fm(keys[1], wk, lambda t, b: P.add(
                    "dve", lambda e, t=t, b=b: e.tensor_copy(out=kT[:, TT(t)], in_=ps[b][:]),
                    reads=[("ps", b)], writes=[("scr", "k", t)]))
                for g in range(4):
                    b = rr8()
                    for ti in range(4):
                        t16 = g * 4 + ti
                        for kc in range(8):
                            P.add("pe", lambda e, kc=kc, t16=t16, ti=ti, b=b, wv=wv: e.matmul(
                                ps[b][:, ti * 128:(ti + 1) * 128], lhsT=xn[:, kc, t16 * 128:(t16 + 1) * 128],
                                rhs=wv[:, kc, :], start=(kc == 0), stop=(kc == 7)),
                                reads=[keys[2], ("xn", kc, t16 // 4)], writes=[("ps", b)])
                    P.add("act", lambda e, g=g, b=b: e.copy(
                        out=vv[:, g * 4:(g + 1) * 4, :], in_=ps[b][:].rearrange("p (t n) -> p t n", t=4)),
                        reads=[("ps", b)], writes=[("scr", "v", g)])
                for j in range(2):
                    pj = slice(64 * j, 64 * j + 64)
                    for qt in range(4):
                        q0 = qt * 512
                        nkt = (qt + 1) * 4
                        bo = rrO()
                        P.add("pe", lambda e, bo=bo: e.matmul(ps[bo][0:64, :], lhsT=zer[:, 0:64], rhs=cb[:, 0:512],
                                                              start=True, stop=False),
                              reads=[("ones",), ("cb",)], writes=[("ps", bo)])
                        prev_ss = None
                        for kt in reversed(range(nkt)):
                            k0 = kt * 128
                            diag = k0 >= q0
                            c0 = k0 - q0 if diag else 0
                            cols = slice(c0, 512)
                            qcols = slice(q0 + c0, q0 + 512)
                            i = cnt["e"] % 2
                            cnt["e"] += 1
                            bs = rr6()
                            P.add("pe", lambda e, bs=bs, cols=cols, qcols=qcols, k0=k0, pj=pj: e.matmul(
                                ps[bs][:, cols], lhsT=kT[pj, k0:k0 + 128], rhs=qT[pj, qcols], start=True, stop=True),
                                reads=[("scr", "k", kt // 4)] + [("scr", "q", qt)], writes=[("ps", bs)])
                            E, G, SP, Wt = Eb[i], Gb[i], SPb[i], Wb[i]
                            P.add("act", lambda e, E=E, bs=bs, cols=cols: e.activation(
                                out=E[:, cols], in_=ps[bs][:, cols], func=AF.Exp, scale=0.125),
                                reads=[("ps", bs)], writes=[("scr", "E", i)])
                            if diag:
                                P.add("dve", lambda e, E=E, c0=c0: e.tensor_tensor(
                                    out=E[:, c0:c0 + 128], in0=E[:, c0:c0 + 128], in1=MLT, op=ALU.mult),
                                    reads=[("scr", "E", i), ("cst",)], writes=[("scr", "E", i)])
                            P.add("act", lambda e, E=E, SP=SP, cols=cols: e.activation(
                                out=SP[:, cols], in_=E[:, cols], func=AF.Ln, bias=1.0),
                                reads=[("scr", "E", i)], writes=[("scr", "SP", i)])
                            bc = rr6()
                            P.add("pe", lambda e, bc=bc, SP=SP, cols=cols, last=(prev_ss is None): e.matmul(
                                ps[bc][:, cols], lhsT=TRI, rhs=SP[:, cols], start=True, stop=last),
                                reads=[("scr", "SP", i), ("cb",)], writes=[("ps", bc)])
                            if prev_ss is not None:
                                pss, pk, pc0 = prev_ss
                                P.add("pe", lambda e, bc=bc, pss=pss, pc0=pc0: e.matmul(
                                    ps[bc][:, pc0:512], lhsT=ones1[:], rhs=pss[:, pc0:512], start=False, stop=True),
                                    reads=[pk, ("ones",)], writes=[("ps", bc)])
                            if kt > 0:
                                si = cnt["ss"] % 2
                                cnt["ss"] += 1
                                SSn = SSb[si]
                                nk = ("scr", "SS", si)
                                if prev_ss is None:
                                    P.add("pool", lambda e, SSn=SSn, SP=SP, cols=cols: e.tensor_copy(
                                        out=SSn[:, cols], in_=SP[:, cols]),
                                        reads=[("scr", "SP", i)], writes=[nk])
                                    prev_ss = (SSn, nk, c0)
                                else:
                                    pss, pk, pc0 = prev_ss
                                    if pc0 > c0:
                                        P.add("pool", lambda e, SSn=SSn, SP=SP, c0=c0, pc0=pc0: e.tensor_copy(
                                            out=SSn[:, c0:pc0], in_=SP[:, c0:pc0]),
                                            reads=[("scr", "SP", i)], writes=[nk])
                                    P.add("pool", lambda e, SSn=SSn, SP=SP, pss=pss, pc0=pc0: e.tensor_tensor(
                                        out=SSn[:, pc0:512], in0=SP[:, pc0:512], in1=pss[:, pc0:512], op=ALU.add),
                                        reads=[("scr", "SP", i), pk], writes=[nk])
                                    prev_ss = (SSn, nk, c0)
                            P.add("act", lambda e, G=G, bc=bc, cols=cols: e.activation(
                                out=G[:, cols], in_=ps[bc][:, cols], func=AF.Exp, scale=-1.0),
                                reads=[("ps", bc)], writes=[("scr", "G", i)])
                            P.add("dve", lambda e, Wt=Wt, E=E, G=G, cols=cols: e.tensor_tensor(
                                out=Wt[:, cols], in0=E[:, cols], in1=G[:, cols], op=ALU.mult),
                                reads=[("scr", "E", i), ("scr", "G", i)], writes=[("scr", "W", i)])
                            P.add("pe", lambda e, bo=bo, Wt=Wt, cols=cols, kt=kt, pj=pj, last=(kt == 0): e.matmul(
                                ps[bo][0:64, cols], lhsT=vv[:, kt, pj], rhs=Wt[:, cols], start=False, stop=last),
                                reads=[("scr", "W", i), ("scr", "v", kt // 4)], writes=[("ps", bo)])
                        P.add("dve", lambda e, bo=bo, hp=hp, pj=pj, qt=qt: e.tensor_copy(
                            out=bigB[pj, hp, TT(qt)], in_=ps[bo][0:64, :]),
                            reads=[("ps", bo)], writes=[("bigB", hp, qt, pj.start)])

        def moba_attention(l):
            P.fence("scr")
            P.fence("bigC")
            P.add("dve", lambda e: e.memset(vext, 1.0), writes=[("scr", "vext")])
            for j in range(2):
                P.add("pool", lambda e, j=j: e.dma_start(out=kaug[64:75, j, :], in_=kst_d[64:75, :]),
                      writes=[("scr", "kst", j)], dma="c1")
            ksf = scr[0:64, 0:16].rearrange("p (j n) -> p j n", j=2)
            gm = scr[:, 64:128].rearrange("p (g n) -> p g n", g=8)
            cmp_ = scr[:, 128:640].rearrange("p (g n m) -> p g n m", g=8, n=8)
            cntt = scr[:, 640:704].rearrange("p (g n) -> p g n", g=8)
            t1 = scr[:, 704:768].rearrange("p (g n) -> p g n", g=8)
            rden = scr[0:64, 768:1280]
            Pm = [scr[:, 1280:1536].bitcast(BF16), scr[:, 1536:1792].bitcast(BF16)]
            cnt = {"p": 0}
            GM = cst[:, C_GM:C_GM + 128].rearrange("p (o j n) -> p o j n", o=8, j=2)
            LL = cst[:, C_L:C_L + 64].rearrange("p (o n) -> p o n", o=8)
            for hp in range(4):
                keys, (wq, wk) = wload([wview(Win, l, 0, D, OFF_MBQ + hp * 128, 128),
                                        wview(Win, l, 0, D, OFF_MBK + hp * 128, 128)])
                keys2, (wv,) = wload([wview(Win, l, 0, D, OFF_MBV + hp * 128, 128)])
                keys = keys + keys2

                def evq(t, b):
                    P.add("act", lambda e, t=t, b=b: e.copy(out=qaug[0:64, 0, TT(t)], in_=ps[b][0:64, :]),
                          reads=[("ps", b)], writes=[("scr", "qaug", 0, t)])
                    P.add("dve", lambda e, t=t, b=b: e.tensor_copy(out=qaug[0:64, 1, TT(t)], in_=ps[b][64:128, :]),
                          reads=[("ps", b)], writes=[("scr", "qaug", 1, t)])
                proj_fm(keys[0], wq, evq)

                def evk(t, b):
                    P.add("act", lambda e, t=t, b=b: e.copy(out=kaug[0:64, 0, TT(t)], in_=ps[b][0:64, :]),
                          reads=[("ps", b)], writes=[("scr", "kaug", 0, t)])
                    P.add("dve", lambda e, t=t, b=b: e.tensor_copy(out=kaug[0:64, 1, TT(t)], in_=ps[b][64:128, :]),
                          reads=[("ps", b)], writes=[("scr", "kaug", 1, t)])
                    for j in range(2):
                        P.add("dve", lambda e, t=t, b=b, j=j: e.tensor_reduce(
                            out=ksf[:, j, 2 * t:2 * t + 2], in_=ps[b][64 * j:64 * j + 64, :].rearrange("p (a n) -> p a n", a=2),
                            axis=AX.X, op=ALU.add),
                            reads=[("ps", b)], writes=[("scr", "ksf", j, t)])
                proj_fm(keys[1], wk, evk)
                P.add("dve", lambda e: e.tensor_copy(out=ksumb[:], in_=ksf),
                      reads=[("scr", "ksf", j, t) for j in range(2) for t in range(4)], writes=[("ksumb",)])
                for g in range(4):
                    b = rr8()
                    for ti in range(4):
                        t16 = g * 4 + ti
                        for kc in range(8):
                            P.add("pe", lambda e, kc=kc, t16=t16, ti=ti, b=b, wv=wv: e.matmul(
                                ps[b][:, ti * 128:(ti + 1) * 128], lhsT=xn[:, kc, t16 * 128:(t16 + 1) * 128],
                                rhs=wv[:, kc, :], start=(kc == 0), stop=(kc == 7)),
                                reads=[keys[2], ("xn", kc, t16 // 4)], writes=[("ps", b)])
                    P.add("act", lambda e, g=g, b=b: e.copy(
                        out=vext[:, g * 4:(g + 1) * 4, :, 0:64],
                        in_=ps[b][:].rearrange("p (t j n) -> p t j n", t=4, j=2)),
                        reads=[("ps", b), ("scr", "vext")], writes=[("scr", "vext", g)])
                for qt in range(4):
                    bg = rr8()
                    for ti in range(4):
                        t16 = qt * 4 + ti
                        for j in range(2):
                            g8 = ti * 2 + j
                            P.add("pe", lambda e, bg=bg, g8=g8, j=j, t16=t16: e.matmul(
                                ps[bg][:, g8 * 8:(g8 + 1) * 8], lhsT=qaug[0:64, j, t16 * 128:(t16 + 1) * 128],
                                rhs=ksumb[:, j, :], start=True, stop=True),
                                reads=[("scr", "qaug", j, qt), ("ksumb",)], writes=[("ps", bg)])
                    for ti in range(4):
                        own = (qt * 4 + ti) // 2
                        P.add("dve", lambda e, bg=bg, ti=ti, own=own: e.tensor_tensor(
                            out=gm[:, 2 * ti:2 * ti + 2, :],
                            in0=ps[bg][:, 16 * ti:16 * ti + 16].rearrange("p (j n) -> p j n", j=2),
                            in1=GM[:, own, :, :], op=ALU.add),
                            reads=[("ps", bg), ("cst",)], writes=[("scr", "gm")])
                    gap = [list(a) for a in gm.ap]
                    gm_m = bass.AP(gm.tensor, gm.offset, [gap[0], gap[1], [0, 8], gap[2]])
                    gm_n = bass.AP(gm.tensor, gm.offset, [gap[0], gap[1], gap[2], [0, 8]])
                    P.add("dve", lambda e, gm_m=gm_m, gm_n=gm_n: e.tensor_tensor(
                        out=cmp_, in0=gm_m, in1=gm_n, op=ALU.is_gt),
                        reads=[("scr", "gm")], writes=[("scr", "cmp")])
                    P.add("dve", lambda e: e.tensor_reduce(out=cntt, in_=cmp_, axis=AX.X, op=ALU.add),
                          reads=[("scr", "cmp")], writes=[("scr", "cnt")])
                    P.add("dve", lambda e: e.tensor_scalar(out=t1, in0=cntt, scalar1=2.5, scalar2=BIG,
                                                           op0=ALU.is_lt, op1=ALU.mult),
                          reads=[("scr", "cnt")], writes=[("scr", "t1")])
                    for ti in range(4):
                        own = (qt * 4 + ti) // 2
                        for j in range(2):
                            hh = 2 * hp + j
                            P.add("dve", lambda e, ti=ti, j=j, hh=hh, own=own: e.scalar_tensor_tensor(
                                out=selT[:, hh, ti, 64:72], in0=t1[:, 2 * ti + j, :], scalar=-BIG, in1=LL[:, own, :],
                                op0=ALU.add, op1=ALU.max),
                                reads=[("scr", "t1"), ("cst",), ("selst",)], writes=[("selT", hh, ti)])
                    for j in range(2):
                        hh = 2 * hp + j
                        bt = rr8()
                        for ti in range(4):
                            P.add("pe", lambda e, bt=bt, ti=ti, hh=hh: e.matmul(
                                ps[bt][0:80, ti * 128:(ti + 1) * 128], lhsT=selT[:, hh, ti, :], rhs=IDN,
                                start=True, stop=True),
                                reads=[("selT", hh, ti), ("cb",)], writes=[("ps", bt)])
                        P.add("act", lambda e, bt=bt, j=j, qt=qt: e.copy(out=qaug[64:75, j, TT(qt)], in_=ps[bt][64:75, :]),
                              reads=[("ps", bt)], writes=[("scr", "qst", j, qt)])
                for j in range(2):
                    hh = 2 * hp + j
                    pj = slice(64 * j, 64 * j + 64)
                    for qt in range(4):
                        q0 = qt * 512
                        nkt = (qt + 1) * 4
                        bo = rrO()
                        for kt in range(nkt):
                            k0 = kt * 128
                            diag = k0 >= q0
                            c0 = k0 - q0 if diag else 0
                            cols = slice(c0, 512)
                            qcols = slice(q0 + c0, q0 + 512)
                            i = cnt["p"] % 2
                            cnt["p"] += 1
                            bs = rr6()
                            P.add("pe", lambda e, bs=bs, cols=cols, qcols=qcols, k0=k0, j=j: e.matmul(
                                ps[bs][:, cols], lhsT=kaug[0:75, j, k0:k0 + 128], rhs=qaug[0:75, j, qcols],
                                start=True, stop=True),
                                reads=[("scr", "kaug", j, kt // 4), ("scr", "kst", j), ("scr", "qaug", j, qt), ("scr", "qst", j, qt)],
                                writes=[("ps", bs)])
                            pm = Pm[i]
                            biasc = -SLOPES[hh] * (q0 - k0)
                            P.add("act", lambda e, pm=pm, bs=bs, cols=cols, biasc=biasc: e.activation(
                                out=pm[:, cols], in_=ps[bs][:, cols], func=AF.Exp, scale=0.125, bias=float(biasc)),
                                reads=[("ps", bs)], writes=[("scr", "Pm", i)])
                            if diag:
                                P.add("pool", lambda e, pm=pm, c0=c0: e.tensor_tensor(
                                    out=pm[:, c0:c0 + 128], in0=pm[:, c0:c0 + 128], in1=MLE, op=ALU.mult),
                                    reads=[("scr", "Pm", i), ("cb",)], writes=[("scr", "Pm", i)])
                            P.add("pe", lambda e, bo=bo, pm=pm, cols=cols, kt=kt, j=j, nkt=nkt: e.matmul(
                                ps[bo][:, cols], lhsT=vext[:, kt, j, :], rhs=pm[:, cols],
                                start=(kt == 0), stop=(kt == nkt - 1)),
                                reads=[("scr", "Pm", i), ("scr", "vext", kt // 4), ("scr", "vext")], writes=[("ps", bo)])
                        P.add("dve", lambda e, bo=bo: e.reciprocal(out=rden, in_=ps[bo][64:128, :]),
                              reads=[("ps", bo)], writes=[("scr", "rden")])
                        P.add("dve", lambda e, bo=bo, hp=hp, pj=pj, qt=qt: e.tensor_tensor(
                            out=bigC[pj, hp, TT(qt)], in0=ps[bo][0:64, :], in1=rden, op=ALU.mult),
                            reads=[("ps", bo), ("scr", "rden")], writes=[("bigC", hp, qt, pj.start)])

        def mix_out(l, have):
            P.fence("scr")
            mixed = scr[:, 0:4096].bitcast(BF16).rearrange("p (k n) -> p k n", k=8)
            gt = [scr[:, 4096 + 512 * i:4608 + 512 * i] for i in range(3)]
            tt_ = [scr[:, 5632 + 512 * i:6144 + 512 * i] for i in range(3)]
            srcs = [("sb", bigB, WA, "bigB"), ("mb", bigC, WB, "bigC"), ("conv", bigA, WC, "bigA")]
            for half in range(2):
                for dc in range(8):
                    kg1, gw1 = wload([wview(Win, l, 0, D, OFF_GATE + br * D + dc * 128, 128) for br in range(2)])
                    kg2, gw2 = wload([wview(Win, l, 0, D, OFF_GATE + 2 * D + dc * 128, 128),
                                      wview(WA, l, 0, 512, dc * 128, 128), wview(WB, l, 0, 512, dc * 128, 128)])
                    kg3, gw3 = wload([wview(WC, l, 0, 512, dc * 128, 128)])
                    keysg, gw = kg1 + kg2[0:1], gw1 + gw2[0:1]
                    keysy, yw = kg2[1:3] + kg3, gw2[1:3] + gw3
                    for t2 in range(2):
                        t = half * 2 + t2
                        first = True
                        for br, (nm, ob, W_, blk) in enumerate(srcs):
                            if nm not in have:
                                continue
                            bgate = rr8()
                            for kc in range(8):
                                P.add("pe", lambda e, kc=kc, t=t, bgate=bgate, gwb=gw[br]: e.matmul(
                                    ps[bgate][:], lhsT=gwb[:, kc, :], rhs=xn[:, kc, TT(t)],
                                    start=(kc == 0), stop=(kc == 7)),
                                    reads=[keysg[br], ("xn", kc, t)], writes=[("ps", bgate)])
                            by = rr8()
                            for kc in range(4):
                                if nm == "conv":
                                    rk = [("bigA", kc, t)]
                                else:
                                    rk = [(blk, kc, t, 0), (blk, kc, t, 64)]
                                P.add("pe", lambda e, kc=kc, t=t, by=by, ywb=yw[br], ob=ob: e.matmul(
                                    ps[by][:], lhsT=ywb[:, kc, :], rhs=ob[:, kc, TT(t)],
                                    start=(kc == 0), stop=(kc == 3)),
                                    reads=[keysy[br]] + rk, writes=[("ps", by)])
                            P.add("act", lambda e, br=br, bgate=bgate, dc=dc: e.activation(
                                out=gt[br], in_=ps[bgate][:], func=AF.Sigmoid, bias=vcol(l, V_GB, br * 8 + dc)),
                                reads=[("ps", bgate), ("vecs",)], writes=[("scr", "gt", br)])
                            P.add("dve", lambda e, br=br, by=by: e.tensor_tensor(
                                out=tt_[br], in0=gt[br], in1=ps[by][:], op=ALU.mult),
                                reads=[("scr", "gt", br), ("ps", by)], writes=[("scr", "tt", br)])
                        live = [br for br, s_ in enumerate(srcs) if s_[0] in have]
                        mo = mixed[:, dc, t2 * 512:(t2 + 1) * 512]
                        mk_ = ("scr", "mixed", dc, t2)
                        if len(live) == 1:
                            P.add("pool", lambda e, mo=mo, a=live[0]: e.tensor_copy(out=mo, in_=tt_[a]),
                                  reads=[("scr", "tt", live[0])], writes=[mk_])
                        elif len(live) == 2:
                            P.add("pool", lambda e, mo=mo, a=live[0], b_=live[1]: e.tensor_tensor(
                                out=mo, in0=tt_[a], in1=tt_[b_], op=ALU.add),
                                reads=[("scr", "tt", live[0]), ("scr", "tt", live[1])], writes=[mk_])
                        else:
                            P.add("pool", lambda e: e.tensor_tensor(out=tt_[0], in0=tt_[0], in1=tt_[1], op=ALU.add),
                                  reads=[("scr", "tt", 0), ("scr", "tt", 1)], writes=[("scr", "tt", 0)])
                            P.add("pool", lambda e, mo=mo: e.tensor_tensor(out=mo, in0=tt_[0], in1=tt_[2], op=ALU.add),
                                  reads=[("scr", "tt", 0), ("scr", "tt", 2)], writes=[mk_])
                dump("mixed", scr[:, 0:4096].bitcast(BF16), [("scr", "mixed", dc_, t_) for dc_ in range(8) for t_ in range(2)])
                for d0 in range(0, 8, 2):
                    keys, (wo,) = wload([wview(WO, l, 0, D, d0 * 128, 256)])
                    for di in range(2):
                        dc = d0 + di
                        for t2 in range(2):
                            t = half * 2 + t2
                            b = rr8()
                            for kc in range(8):
                                P.add("pe", lambda e, kc=kc, t2=t2, b=b, di=di, wo=wo: e.matmul(
                                    ps[b][:], lhsT=wo[:, kc, di * 128:(di + 1) * 128],
                                    rhs=mixed[:, kc, t2 * 512:(t2 + 1) * 512], start=(kc == 0), stop=(kc == 7)),
                                    reads=[keys[0], ("scr", "mixed", kc, t2)], writes=[("ps", b)])
                            P.add("dve", lambda e, b=b, dc=dc, t=t: e.tensor_tensor(
                                out=h[:, dc, TT(t)], in0=ps[b][:], in1=h[:, dc, TT(t)], op=ALU.add),
                                reads=[("ps", b)], writes=[("h", dc, t)])

        outs = []
        for s_i in range(nseq):
            for dc in range(8):
                P.add("sp", lambda e, dc=dc, s_i=s_i: e.dma_start(out=h[:, dc, :], in_=xT[s_i, dc * 128:(dc + 1) * 128, :]),
                      writes=[("h", dc, t) for t in range(4)], dma="x%d" % dc)
            for l in range(depth):
                if "ffn1" in stages:
                    ffn(l, W1i, W1o, V_FFN1N)
                if "mix" in stages:
                    rmsnorm(l, V_MIXN)
                    if "conv" in mix_parts:
                        conv_branch(l)
                    if "sb" in mix_parts:
                        sb_attention(l)
                    if "mb" in mix_parts:
                        moba_attention(l)
                    mix_out(l, mix_parts)
                if "ffn2" in stages:
                    ffn(l, W2i, W2o, V_FFN2N)
            if "final" in stages:
                rmsnorm(None, depth * NVEC_L, out_f32=h)
            for dc in range(8):
                outs.append(P.add("sp", lambda e, dc=dc, s_i=s_i: e.dma_start(
                    out=outT[s_i, dc * 128:(dc + 1) * 128, :], in_=h[:, dc, :]),
                    reads=[("h", dc, t) for t in range(4)], dma="o%d" % dc))
        P.emit(nc, final_waits=outs[-8:] + dbg_ops)
    return nc


_CACHE = {}


def kernel(**inputs):
    x = np.asarray(inputs["x"], np.float32)
    B = x.shape[0]
    per = B // NCORES
    cst, kst, sel = host_consts()
    vecs = host_vecs(inputs, DEPTH_FULL)
    nc = build(per, DEPTH_FULL)
    shared = {k: np.ascontiguousarray(np.asarray(inputs[k], np.float32)) for k in
              ("ffn1_w_in", "ffn1_w_out", "ffn2_w_in", "ffn2_w_out", "w_in", "sb_w_out", "mb_w_out",
               "conv_w_out", "w_o")}
    shared.update({"vecs": vecs, "cst": cst, "kst": kst, "selst": sel})
    in_maps = []
    for c in range(NCORES):
        xs = x[c * per:(c + 1) * per]
        m = dict(shared)
        m["xT"] = np.ascontiguousarray(xs.transpose(0, 2, 1))
        in_maps.append(m)
    res = run_bass_kernel_spmd(nc, in_maps, core_ids=list(range(NCORES)))
    out = np.empty((B, S, D), np.float32)
    for c in range(NCORES):
        out[c * per:(c + 1) * per] = res.results[c]["outT"].transpose(0, 2, 1)
    return out
```

```python
import contextlib
import numpy as np
import concourse.bass as bass
import concourse.mybir as mybir
from concourse.bass_utils import run_bass_kernel_spmd

F32 = mybir.dt.float32
BF16 = mybir.dt.bfloat16
AF = mybir.ActivationFunctionType
ALU = mybir.AluOpType
AX = mybir.AxisListType

D = 1024
S = 2048
DFF = 2816
NFC = 22
DEPTH_FULL = 2
NCORES = 8
SEQ_PER_CORE = 4
OFF_SBQ, OFF_SBK, OFF_SBV = 0, 512, 1024
OFF_MBQ, OFF_MBK, OFF_MBV = 1536, 2048, 2560
OFF_CONV = 3072
OFF_GATE = 4096
IN_COLS = 7168
EPS = 1e-6
BIG = 29952.0
SLOPES = [2.0 ** (-(h + 1)) for h in range(8)]
ENGS = ("pe", "act", "dve", "pool", "sp")


class Op:
    __slots__ = ("eng", "fn", "pos", "sig", "waits", "dma", "val")

    def __init__(self, eng, fn, dma=None):
        self.eng, self.fn, self.dma = eng, fn, dma
        self.pos, self.sig, self.waits, self.val = -1, False, [], 0


class Prog:
    def __init__(self):
        self.streams = {e: [] for e in ENGS}
        self.last_w, self.readers = {}, {}
        self.seen = {e: {} for e in ENGS}
        self.dma_cnt = {}
        self.fences = {}

    def _need(self, x, y, raw):
        if y is None or y is x:
            return
        if y.dma is not None:
            key = ("d", y.dma)
            val = self.dma_cnt[y.dma] - (16 if x.dma == y.dma else 0)
            if self.seen[x.eng].get(key, 0) >= val:
                return
            self.seen[x.eng][key] = val
            x.waits.append(("d", y.dma, val))
            return
        if y.eng == x.eng and x.dma is None:
            if x.eng == "pe" or not raw:
                return
            if len(self.streams[x.eng]) - y.pos > 3:
                return
        key = ("e", y.eng)
        if self.seen[x.eng].get(key, -1) >= y.pos:
            return
        self.seen[x.eng][key] = y.pos
        y.sig = True
        x.waits.append(("e", y.eng, y))

    def fence(self, block):
        ops = {}
        for k in [k for k in self.last_w if k[0] == block]:
            o = self.last_w.pop(k)
            ops[id(o)] = o
        for k in [k for k in self.readers if k[0] == block]:
            for o in self.readers.pop(k):
                ops[id(o)] = o
        best = {}
        for o in ops.values():
            kk = (o.eng, o.dma)
            rank = o.val if o.dma is not None else o.pos
            if kk not in best or rank > best[kk][0]:
                best[kk] = (rank, o)
        if best:
            self.fences[block] = [v[1] for v in best.values()]

    def add(self, eng, fn, reads=(), writes=(), dma=None):
        x = Op(eng, fn, dma)
        if dma is not None:
            self.dma_cnt[dma] = self.dma_cnt.get(dma, 0) + 16
            x.val = self.dma_cnt[dma]
        reads = list(reads)
        writes = list(writes)
        for r in list(reads):
            if r[0] == "ps":
                reads.remove(r)
                writes.append(r)
        for k in reads + writes:
            if k not in self.last_w and k[0] in self.fences:
                for o in self.fences[k[0]]:
                    self._need(x, o, True)
        for r in reads:
            self._need(x, self.last_w.get(r), True)
        for w in writes:
            self._need(x, self.last_w.get(w), True)
            for rd in self.readers.get(w, ()):
                self._need(x, rd, False)
        x.pos = len(self.streams[eng])
        self.streams[eng].append(x)
        for r in reads:
            self.readers.setdefault(r, []).append(x)
        for w in writes:
            self.last_w[w] = x
            self.readers[w] = []
        return x

    def emit(self, nc, final_waits=()):
        with contextlib.ExitStack() as es:
            esem = {e: es.enter_context(nc.semaphore("s_" + e)) for e in ENGS}
            dsem = {n: es.enter_context(nc.semaphore("d_" + n)) for n in self.dma_cnt}
            block = es.enter_context(nc.Block())
            for e in ENGS:
                c = 0
                for op in self.streams[e]:
                    if op.dma is None:
                        if op.sig:
                            c += 1
                        op.val = c
            hooks = {"pe": block.tensor, "act": block.scalar, "dve": block.vector,
                     "pool": block.gpsimd, "sp": block.sync}

            def mk(e):
                def body(eng):
                    for op in self.streams[e]:
                        for w in op.waits:
                            if w[0] == "d":
                                eng.wait_ge(dsem[w[1]], w[2])
                            else:
                                eng.wait_ge(esem[w[1]], w[2].val)
                        ins = op.fn(eng)
                        if op.dma is not None:
                            ins.then_inc(dsem[op.dma], 16)
                        elif op.sig:
                            ins.then_inc(esem[e], 1)
                    if e == "sp":
                        for op in final_waits:
                            eng.wait_ge(dsem[op.dma], op.val)
                return body
            for e in ENGS:
                hooks[e](mk(e))


NVEC_L = 8 + 8 + 8 + 24 + 4 + 4 + 4 + 124
V_FFN1N, V_MIXN, V_FFN2N, V_GB, V_DWB, V_LNG, V_LNB, V_DW = 0, 8, 16, 24, 48, 52, 56, 60
C_TRI, C_MLT, C_MLE, C_ID, C_GM, C_L = 0, 128, 256, 384, 512, 640
NCST = 704


def host_consts():
    c = np.zeros((128, NCST), np.float32)
    j = np.arange(128)[:, None]
    s = np.arange(128)[None, :]
    c[:, C_TRI:C_TRI + 128] = (j >= s)
    c[:, C_MLT:C_MLT + 128] = np.where(j >= s, -BIG, 0.0)
    c[:, C_MLE:C_MLE + 128] = np.where(j > s, -BIG, 0.0)
    c[:, C_ID:C_ID + 128] = (j == s)
    own = np.arange(8)[:, None]
    n = np.arange(8)[None, :]
    gm = np.where(n < own, 0.0, -BIG).astype(np.float32)
    ll = np.where(n < own, -BIG, 0.0).astype(np.float32)
    c[:, C_GM:C_GM + 128] = np.repeat(gm[:, None, :], 2, axis=1).reshape(1, 128)
    c[:, C_L:C_L + 64] = ll.reshape(1, 64)
    kst = np.zeros((128, S), np.float32)
    pos = np.arange(S)
    for nn in range(8):
        kst[64 + nn] = (pos // 256 == nn)
    kst[72] = 1.0
    kst[73] = 1.0
    kst[74] = pos % 128
    sel = np.zeros((128, 8, 4, 80), np.float32)
    q = np.arange(128)
    for h in range(8):
        for m in range(4):
            i = m * 128 + q
            sel[:, h, m, 72] = -8.0 * SLOPES[h] * (256 * (i // 256))
            sel[:, h, m, 73] = -8.0 * SLOPES[h] * (i % 256)
            sel[:, h, m, 74] = 8.0 * SLOPES[h]
    return c, kst, sel.reshape(128, 8 * 4 * 80)


def host_vecs(inp, depth):
    def col(v):
        return np.ascontiguousarray(np.asarray(v, np.float32).reshape(-1, 128).T)
    cols = []
    for l in range(depth):
        cols += [col(inp["ffn1_norm"][l]), col(inp["mix_norm"][l]), col(inp["ffn2_norm"][l]),
                 col(inp["gate_bias"][l]), col(inp["conv_dw_bias"][l]), col(inp["conv_ln_g"][l]),
                 col(inp["conv_ln_b"][l])]
        dw = np.asarray(inp["conv_dw"][l], np.float32).reshape(31, 512)
        cols.append(np.ascontiguousarray(dw.reshape(31, 4, 128).transpose(2, 0, 1).reshape(128, 124)))
    cols.append(col(inp["final_norm"]))
    return np.ascontiguousarray(np.concatenate(cols, axis=1))


def build(nseq=SEQ_PER_CORE, depth=DEPTH_FULL, stages=("ffn1", "mix", "ffn2", "final"),
          mix_parts=("conv", "sb", "mb"), dbg=None):
    nc = bass.Bass("TRN2", target_bir_lowering=False)
    dram = {}

    def din(name, shape):
        dram[name] = nc.dram_tensor(name, list(shape), F32, kind="ExternalInput").ap()
        return dram[name]

    xT = din("xT", [nseq, D, S])
    W1i = din("ffn1_w_in", [depth, D, 2 * DFF])
    W1o = din("ffn1_w_out", [depth, DFF, D])
    W2i = din("ffn2_w_in", [depth, D, 2 * DFF])
    W2o = din("ffn2_w_out", [depth, DFF, D])
    Win = din("w_in", [depth, D, IN_COLS])
    WA = din("sb_w_out", [depth, 512, D])
    WB = din("mb_w_out", [depth, 512, D])
    WC = din("conv_w_out", [depth, 512, D])
    WO = din("w_o", [depth, D, D])
    NV = depth * NVEC_L + 8
    vecs_d = din("vecs", [128, NV])
    cst_d = din("cst", [128, NCST])
    kst_d = din("kst", [128, S])
    sel_d = din("selst", [128, 8 * 4 * 80])
    outT = nc.dram_tensor("outT", [nseq, D, S], F32, kind="ExternalOutput").ap()
    dbg_d = nc.dram_tensor("dbg", [128, 8192], F32, kind="ExternalOutput").ap() if dbg else None
    dbg_ops = []

    def dump(name, ap, keys):
        if dbg == name and not dbg_ops:
            n = ap.shape[1]
            dbg_ops.append(P.add("pool", lambda e: e.dma_start(out=dbg_d[:, 0:n], in_=ap), reads=keys, dma="dbg"))

    P = Prog()
    es = contextlib.ExitStack()
    with es:
        def sb(name, shape, dt):
            return es.enter_context(nc.sbuf_tensor(name, list(shape), dt))

        h = sb("h", [128, 8, S], F32)
        xn = sb("xn", [128, 8, S], BF16)
        bigA = sb("bigA", [128, 4, S], BF16)
        bigB = sb("bigB", [128, 4, S], BF16)
        bigC = sb("bigC", [128, 4, S], BF16)
        scr = sb("scr", [128, 8192], F32)
        NW = 4
        WSZ = 2048
        wsl = [sb("w%d" % i, [128, WSZ], BF16) for i in range(NW)]
        vecs = sb("vecs_sb", [128, NV], F32)
        cst = sb("cst_sb", [128, NCST], F32)
        cb = sb("cst_bf", [128, 512], BF16)
        ones_d = sb("ones_d", [128, 128], BF16)
        ones_c = sb("ones_c", [128, 128], BF16)
        ones1 = sb("ones1", [128, 128], BF16)
        zer = sb("zer", [128, 128], BF16)
        eps_c = sb("eps_c", [128, 1], F32)
        kaug = scr[:, 2048:4096].bitcast(BF16).rearrange("p (j n) -> p j n", j=2)
        qaug = scr[:, 4096:6144].bitcast(BF16).rearrange("p (j n) -> p j n", j=2)
        vext = scr[:, 6144:8192].bitcast(BF16).rearrange("p (t j n) -> p t j n", t=16, j=2)
        selT = sb("selT", [128, 8, 4, 80], BF16)
        ksumb = sb("ksumb", [64, 2, 8], BF16)
        ps = [es.enter_context(nc.psum_tensor("ps%d" % i, [128, 512], F32)) for i in range(8)]

        class RR:
            def __init__(self, ids):
                self.ids, self.i = list(ids), 0

            def __call__(self):
                b = self.ids[self.i % len(self.ids)]
                self.i += 1
                return b
        rr8 = RR(range(8))
        rr6 = RR(range(5))
        NDUM = {"sb": 3}

        def dummies(n):
            for _ in range(n):
                P.add("pe", lambda e: e.matmul(ps[5][:], lhsT=zer[:], rhs=cb[:, 0:512], start=True, stop=True),
                      reads=[("ones",), ("cb",)], writes=[("psdum",)])
        rrO = RR([6, 7])
        wstate = {"i": 0}

        def wload(parts, eng="pool"):
            si = wstate["i"] % NW
            wstate["i"] += 1
            P.fence("w%d" % si)
            off = 0
            views, keys = [], []
            for pi, ap in enumerate(parts):
                K, n = ap.shape[1], ap.shape[2]
                v = wsl[si][:, off:off + K * n].rearrange("p (k n) -> p k n", k=K)
                key = ("w%d" % si, pi)
                P.add(eng, lambda e, v=v, ap=ap: e.dma_start(out=v, in_=ap), writes=[key], dma="w%d" % si)
                views.append(v)
                keys.append(key)
                off += K * n
            assert off <= WSZ
            return keys, views

        def wview(Wd, l, r0, nrow, c0, ncol):
            return Wd[l, r0:r0 + nrow, c0:c0 + ncol].rearrange("(k p) n -> p k n", p=128)

        def vcol(l, base, i):
            c = l * NVEC_L + base + i
            return vecs[:, c:c + 1]

        def TT(t):
            return slice(t * 512, (t + 1) * 512)

        P.add("sp", lambda e: e.dma_start(out=vecs[:], in_=vecs_d), writes=[("vecs",)], dma="c0")
        P.add("sp", lambda e: e.dma_start(out=cst[:], in_=cst_d), writes=[("cst",)], dma="c0")
        P.add("dve", lambda e: e.tensor_copy(out=cb[:], in_=cst[:, 0:512]), reads=[("cst",)], writes=[("cb",)])
        P.add("dve", lambda e: e.memset(ones_d[:], 1.0 / 1024), writes=[("ones",)])
        P.add("dve", lambda e: e.memset(ones_c[:], 1.0 / 512), writes=[("ones",)])
        P.add("dve", lambda e: e.memset(ones1[:], 1.0), writes=[("ones",)])
        P.add("dve", lambda e: e.memset(zer[:], 0.0), writes=[("ones",)])
        P.add("dve", lambda e: e.memset(eps_c[:], EPS), writes=[("ones",)])
        P.add("pool", lambda e: e.dma_start(out=selT[:].rearrange("p a b c -> p (a b c)"), in_=sel_d),
              writes=[("selst",)], dma="c1")
        TRI = cb[:, C_TRI:C_TRI + 128]
        NEGM2 = cb[:, C_MLE:C_MLE + 128]
        NEGM = cb[:, C_MLT:C_MLT + 128]
        IDN = cb[:, C_ID:C_ID + 128]
        MLT = cst[:, C_MLT:C_MLT + 128]

        def rmsnorm(l, base, out_bf=True, out_f32=None):
            P.fence("scr")
            sqb = scr[:, 0:2048].bitcast(BF16).rearrange("p (k n) -> p k n", k=8)
            rstd = scr[:, 2048:2560]
            for t in range(4):
                for dc in range(8):
                    P.add("act", lambda e, dc=dc, t=t: e.activation(out=sqb[:, dc, :], in_=h[:, dc, TT(t)], func=AF.Square),
                          reads=[("h", dc, t)], writes=[("scr", "sq", dc)])
                b = rr8()
                for dc in range(8):
                    P.add("pe", lambda e, dc=dc, b=b: e.matmul(ps[b][:], lhsT=ones_d[:], rhs=sqb[:, dc, :],
                                                               start=(dc == 0), stop=(dc == 7)),
                          reads=[("scr", "sq", dc), ("ones",)], writes=[("ps", b)])
                P.add("act", lambda e, b=b: e.activation(out=rstd, in_=ps[b][:], func=AF.Sqrt, bias=eps_c[:, 0:1]),
                      reads=[("ps", b), ("ones",)], writes=[("scr", "rstd")])
                P.add("dve", lambda e: e.reciprocal(out=rstd, in_=rstd),
                      reads=[("scr", "rstd")], writes=[("scr", "rstd")])
                for dc in range(8):
                    if out_f32 is None:
                        o = xn[:, dc, TT(t)]
                        wk = ("xn", dc, t)
                    else:
                        o = out_f32[:, dc, TT(t)]
                        wk = ("h", dc, t)
                    c = base + dc if l is None else l * NVEC_L + base + dc
                    P.add("dve", lambda e, o=o, dc=dc, t=t, c=c: e.scalar_tensor_tensor(
                        out=o, in0=h[:, dc, TT(t)], scalar=vecs[:, c:c + 1], in1=rstd, op0=ALU.mult, op1=ALU.mult),
                        reads=[("h", dc, t), ("scr", "rstd"), ("vecs",)], writes=[wk])

        def ffn(l, Wi, Wo, nbase):
            rmsnorm(l, nbase)
            P.fence("scr")
            P.fence("bigA")
            P.fence("bigB")
            sil = [scr[:, 2560:3072], scr[:, 3072:3584]]
            groups = [(0, 8), (8, 7), (15, 7)]
            sidx = 0
            for (g0, gn) in groups:
                def hid(c, t):
                    return (bigA if c < 4 else bigB)[:, c % 4, TT(t)]
                for c0 in range(0, gn, 1):
                    ncnk = 1
                    fc = g0 + c0
                    keys, (wa, wb) = wload([wview(Wi, l, 0, D, fc * 128, ncnk * 128),
                                            wview(Wi, l, 0, D, DFF + fc * 128, ncnk * 128)])
                    for ci in range(ncnk):
                        c = c0 + ci
                        for t in range(4):
                            ba, bb = rr8(), rr8()
                            for kc in range(8):
                                P.add("pe", lambda e, kc=kc, t=t, ba=ba, ci=ci, wa=wa: e.matmul(
                                    ps[ba][:], lhsT=wa[:, kc, ci * 128:(ci + 1) * 128], rhs=xn[:, kc, TT(t)],
                                    start=(kc == 0), stop=(kc == 7)),
                                    reads=[keys[0], ("xn", kc, t)], writes=[("ps", ba)])
                            for kc in range(8):
                                P.add("pe", lambda e, kc=kc, t=t, bb=bb, ci=ci, wb=wb: e.matmul(
                                    ps[bb][:], lhsT=wb[:, kc, ci * 128:(ci + 1) * 128], rhs=xn[:, kc, TT(t)],
                                    start=(kc == 0), stop=(kc == 7)),
                                    reads=[keys[1], ("xn", kc, t)], writes=[("ps", bb)])
                            st = sil[sidx % 2]
                            sk = ("scr", "sil", sidx % 2)
                            sidx += 1
                            P.add("act", lambda e, st=st, ba=ba: e.activation(out=st, in_=ps[ba][:], func=AF.Silu),
                                  reads=[("ps", ba)], writes=[sk])
                            P.add("dve", lambda e, st=st, bb=bb, c=c, t=t: e.tensor_tensor(
                                out=hid(c, t), in0=st, in1=ps[bb][:], op=ALU.mult),
                                reads=[sk, ("ps", bb)], writes=[("bigA" if c < 4 else "bigB", c % 4, t)])
                for d0 in range(0, 8, 2):
                    keys, (wo,) = wload([wview(Wo, l, g0 * 128, gn * 128, d0 * 128, 256)])
                    for di in range(2):
                        dc = d0 + di
                        for t in range(4):
                            b = rr8()
                            for c in range(gn):
                                P.add("pe", lambda e, c=c, t=t, b=b, di=di, wo=wo, gn=gn: e.matmul(
                                    ps[b][:], lhsT=wo[:, c, di * 128:(di + 1) * 128], rhs=hid(c, t),
                                    start=(c == 0), stop=(c == gn - 1)),
                                    reads=[keys[0], ("bigA" if c < 4 else "bigB", c % 4, t)], writes=[("ps", b)])
                            P.add("dve", lambda e, b=b, dc=dc, t=t: e.scalar_tensor_tensor(
                                out=h[:, dc, TT(t)], in0=ps[b][:], scalar=0.5, in1=h[:, dc, TT(t)],
                                op0=ALU.mult, op1=ALU.add),
                                reads=[("ps", b)], writes=[("h", dc, t)])

        def proj_fm(keyw, wv, evac):
            for t in range(4):
                b = rr8()
                for kc in range(8):
                    P.add("pe", lambda e, kc=kc, t=t, b=b: e.matmul(ps[b][:], lhsT=wv[:, kc, :], rhs=xn[:, kc, TT(t)],
                                                                     start=(kc == 0), stop=(kc == 7)),
                          reads=[keyw, ("xn", kc, t)], writes=[("ps", b)])
                evac(t, b)

        def conv_branch(l):
            for blk in ("bigA", "bigB", "bigC", "scr"):
                P.fence(blk)
            glu = [bigB[:, 0:2, :].bitcast(F32), bigB[:, 2:4, :].bitcast(F32),
                   bigC[:, 0:2, :].bitcast(F32), bigC[:, 2:4, :].bitcast(F32)]
            glu = [g.rearrange("p a n -> p (a n)") for g in glu]
            gkey = [("bigB", "g0"), ("bigB", "g1"), ("bigC", "g2"), ("bigC", "g3")]
            sg = [scr[:, 0:512], scr[:, 512:1024]]
            si = 0
            for cc in range(4):
                keys, (wv, wg) = wload([wview(Win, l, 0, D, OFF_CONV + cc * 128, 128),
                                        wview(Win, l, 0, D, OFF_CONV + 512 + cc * 128, 128)])
                for t in range(4):
                    bv, bg = rr8(), rr8()
                    for kc in range(8):
                        P.add("pe", lambda e, kc=kc, t=t, bv=bv, wv=wv: e.matmul(
                            ps[bv][:], lhsT=wv[:, kc, :], rhs=xn[:, kc, TT(t)], start=(kc == 0), stop=(kc == 7)),
                            reads=[keys[0], ("xn", kc, t)], writes=[("ps", bv)])
                    for kc in range(8):
                        P.add("pe", lambda e, kc=kc, t=t, bg=bg, wg=wg: e.matmul(
                            ps[bg][:], lhsT=wg[:, kc, :], rhs=xn[:, kc, TT(t)], start=(kc == 0), stop=(kc == 7)),
                            reads=[keys[1], ("xn", kc, t)], writes=[("ps", bg)])
                    s_ = sg[si % 2]
                    sk = ("scr", "sg", si % 2)
                    si += 1
                    P.add("act", lambda e, s_=s_, bg=bg: e.activation(out=s_, in_=ps[bg][:], func=AF.Sigmoid),
                          reads=[("ps", bg)], writes=[sk])
                    P.add("dve", lambda e, s_=s_, bv=bv, cc=cc, t=t: e.tensor_tensor(
                        out=glu[cc][:, TT(t)], in0=s_, in1=ps[bv][:], op=ALU.mult),
                        reads=[sk, ("ps", bv)], writes=[gkey[cc]])
            dump("glu", glu[0], [gkey[0]])
            for cpair in range(2):
                P.fence("scr")
                accs = [scr[:, 0:2048], scr[:, 2048:4096]]
                tmp = scr[:, 4096:4608]
                tmpb = scr[:, 4608:5120].bitcast(BF16)
                xcb = scr[:, 5120:6144].bitcast(BF16).rearrange("p (k n) -> p k n", k=4)
                rstd = scr[:, 6144:6656]
                for ci in range(2):
                    cc = cpair * 2 + ci
                    P.add("dve", lambda e, ci=ci, cc=cc: e.tensor_scalar(
                        out=accs[ci], in0=glu[cc], scalar1=vcol(l, V_DW, 30 * 4 + cc), scalar2=vcol(l, V_DWB, cc),
                        op0=ALU.mult, op1=ALU.add),
                        reads=[gkey[cc], ("vecs",)], writes=[("scr", "acc", ci)])
                for tap in range(30):
                    dsh = 30 - tap
                    for ci in range(2):
                        cc = cpair * 2 + ci
                        P.add("dve", lambda e, ci=ci, cc=cc, tap=tap, dsh=dsh: e.scalar_tensor_tensor(
                            out=accs[ci][:, dsh:S], in0=glu[cc][:, 0:S - dsh], scalar=vcol(l, V_DW, tap * 4 + cc),
                            in1=accs[ci][:, dsh:S], op0=ALU.mult, op1=ALU.add),
                            reads=[gkey[cc], ("vecs",), ("scr", "acc", ci)], writes=[("scr", "acc", ci)])
                for ci in range(2):
                    cc = cpair * 2 + ci
                    P.add("act", lambda e, ci=ci, cc=cc: e.copy(out=glu[cc], in_=accs[ci]),
                          reads=[("scr", "acc", ci)], writes=[gkey[cc]])
            dump("convout", glu[0], [gkey[0]])
            P.fence("scr")
            xb = scr[:, 0:1024].bitcast(BF16).rearrange("p (k n) -> p k n", k=4)
            xc = scr[:, 1024:3072].rearrange("p (k n) -> p k n", k=4)
            rstd = scr[:, 3072:3584]
            yt = scr[:, 3584:4096]
            for t in range(4):
                for cc in range(4):
                    P.add("act", lambda e, cc=cc, t=t: e.copy(out=xb[:, cc, :], in_=glu[cc][:, TT(t)]),
                          reads=[gkey[cc]], writes=[("scr", "xb", cc)])
                bm = rr8()
                for cc in range(4):
                    P.add("pe", lambda e, cc=cc, bm=bm: e.matmul(ps[bm][:], lhsT=ones_c[:], rhs=xb[:, cc, :],
                                                                 start=(cc == 0), stop=(cc == 3)),
                          reads=[("scr", "xb", cc), ("ones",)], writes=[("ps", bm)])
                for cc in range(4):
                    P.add("dve", lambda e, cc=cc, t=t, bm=bm: e.tensor_tensor(
                        out=xc[:, cc, :], in0=glu[cc][:, TT(t)], in1=ps[bm][:], op=ALU.subtract),
                        reads=[gkey[cc], ("ps", bm)], writes=[("scr", "xc", cc)])
                for cc in range(4):
                    P.add("act", lambda e, cc=cc: e.activation(out=xb[:, cc, :], in_=xc[:, cc, :], func=AF.Square),
                          reads=[("scr", "xc", cc)], writes=[("scr", "xb", cc)])
                bv = rr8()
                for cc in range(4):
                    P.add("pe", lambda e, cc=cc, bv=bv: e.matmul(ps[bv][:], lhsT=ones_c[:], rhs=xb[:, cc, :],
                                                                 start=(cc == 0), stop=(cc == 3)),
                          reads=[("scr", "xb", cc), ("ones",)], writes=[("ps", bv)])
                P.add("act", lambda e, bv=bv: e.activation(out=rstd, in_=ps[bv][:], func=AF.Sqrt, bias=eps_c[:, 0:1]),
                      reads=[("ps", bv), ("ones",)], writes=[("scr", "rstd")])
                P.add("dve", lambda e: e.reciprocal(out=rstd, in_=rstd),
                      reads=[("scr", "rstd")], writes=[("scr", "rstd")])
                for cc in range(4):
                    P.add("dve", lambda e, cc=cc: e.scalar_tensor_tensor(
                        out=xc[:, cc, :], in0=xc[:, cc, :], scalar=vcol(l, V_LNG, cc), in1=rstd,
                        op0=ALU.mult, op1=ALU.mult),
                        reads=[("scr", "xc", cc), ("scr", "rstd"), ("vecs",)], writes=[("scr", "xc", cc)])
                    P.add("act", lambda e, cc=cc, t=t: e.activation(
                        out=bigA[:, cc, TT(t)], in_=xc[:, cc, :], func=AF.Silu, bias=vcol(l, V_LNB, cc)),
                        reads=[("scr", "xc", cc), ("vecs",)], writes=[("bigA", cc, t)])
            dump("c", bigA[:].rearrange("p a n -> p (a n)"), [("bigA", cc, t) for cc in range(4) for t in range(4)])
            P.fence("bigB")
            P.fence("bigC")

        def sb_attention(l):
            P.fence("scr")
            P.fence("bigB")
            qT = scr[:, 0:1024].bitcast(BF16)
            kT = scr[:, 1024:2048].bitcast(BF16)
            vv = scr[:, 2048:3072].bitcast(BF16).rearrange("p (t n) -> p t n", t=16)
            Eb = [scr[:, 3072:3584], scr[:, 3584:4096], scr[:, 6912:7424]]
            Gb = [scr[:, 4096:4608], scr[:, 4608:5120]]
            SPb = [scr[:, 5120:5376].bitcast(BF16), scr[:, 5376:5632].bitcast(BF16)]
            SSb = [scr[:, 5632:5888].bitcast(BF16), scr[:, 5888:6144].bitcast(BF16), scr[:, 6656:6912].bitcast(BF16)]
            Wb = [scr[:, 6144:6400].bitcast(BF16), scr[:, 6400:6656].bitcast(BF16)]
            cnt = {"e": 0, "ss": 0}
            for hp in range(4):
                keys, (wq, wk) = wload([wview(Win, l, 0, D, OFF_SBQ + hp * 128, 128),
                                        wview(Win, l, 0, D, OFF_SBK + hp * 128, 128)])
                keys2, (wv,) = wload([wview(Win, l, 0, D, OFF_SBV + hp * 128, 128)])
                keys = keys + keys2
                proj_fm(keys[0], wq, lambda t, b: P.add(
                    "act", lambda e, t=t, b=b: e.copy(out=qT[:, TT(t)], in_=ps[b][:]),
                    reads=[("ps", b)], writes=[("scr", "q", t)]))
                proj_fm(keys[1], wk, lambda t, b: P.add(
                    "dve", lambda e, t=t, b=b: e.tensor_copy(out=kT[:, TT(t)], in_=ps[b][:]),
                    reads=[("ps", b)], writes=[("scr", "k", t)]))
                for g in range(4):
                    b = rr8()
                    for ti in range(4):
                        t16 = g * 4 + ti
                        for kc in range(8):
                            P.add("pe", lambda e, kc=kc, t16=t16, ti=ti, b=b, wv=wv: e.matmul(
                                ps[b][:, ti * 128:(ti + 1) * 128], lhsT=xn[:, kc, t16 * 128:(t16 + 1) * 128],
                                rhs=wv[:, kc, :], start=(kc == 0), stop=(kc == 7)),
                                reads=[keys[2], ("xn", kc, t16 // 4)], writes=[("ps", b)])
                    P.add("act", lambda e, g=g, b=b: e.copy(
                        out=vv[:, g * 4:(g + 1) * 4, :], in_=ps[b][:].rearrange("p (t n) -> p t n", t=4)),
                        reads=[("ps", b)], writes=[("scr", "v", g)])
                pairs = []
                for j in range(2):
                    for qt in range(4):
                        nkt = (qt + 1) * 4
                        bo = rrO()
                        for idx, kt in enumerate(reversed(range(nkt))):
                            k0, q0 = kt * 128, qt * 512
                            diag = k0 >= q0
                            c0 = k0 - q0 if diag else 0
                            pairs.append(dict(j=j, qt=qt, kt=kt, k0=k0, q0=q0, diag=diag, c0=c0, first=(idx == 0),
                                              last=(kt == 0), bo=bo, pj=slice(64 * j, 64 * j + 64)))
                state = {"prev_ss": None}

                def stA(p, n):
                    i = n % 2
                    c0, q0, k0, pj, kt, qt = p["c0"], p["q0"], p["k0"], p["pj"], p["kt"], p["qt"]
                    cols = slice(c0, 512)
                    qcols = slice(q0 + c0, q0 + 512)
                    if p["first"]:
                        state["prev_ss"] = None
                    bs = rr6()
                    P.add("pe", lambda e: e.matmul(
                        ps[bs][:, cols], lhsT=kT[pj, k0:k0 + 128], rhs=qT[pj, qcols], start=True, stop=not p["diag"]),
                        reads=[("scr", "k", kt // 4), ("scr", "q", qt)], writes=[("ps", bs)])
                    if p["diag"]:
                        P.add("pe", lambda e: e.matmul(ps[bs][:, c0:c0 + 128], lhsT=IDN, rhs=NEGM, start=False, stop=True),
                              reads=[("cb",)], writes=[("ps", bs)])
                    ie = n % 3
                    E, SP = Eb[ie], SPb[i]
                    P.add("act", lambda e: e.activation(out=E[:, cols], in_=ps[bs][:, cols], func=AF.Exp, scale=0.125),
                          reads=[("ps", bs)], writes=[("scr", "E", ie)])
                    P.add("act", lambda e: e.activation(out=SP[:, cols], in_=E[:, cols], func=AF.Ln, bias=1.0),
                          reads=[("scr", "E", ie)], writes=[("scr", "SP", i)])
                    p["pss"] = state["prev_ss"]
                    if kt > 0:
                        si = n % 3
                        SSn = SSb[si]
                        nk = ("scr", "SS", si)
                        if state["prev_ss"] is None:
                            P.add("pool", lambda e: e.tensor_copy(out=SSn[:, cols], in_=SP[:, cols]),
                                  reads=[("scr", "SP", i)], writes=[nk])
                        else:
                            pss, pk, pc0 = state["prev_ss"]
                            if pc0 > c0:
                                P.add("pool", lambda e: e.tensor_copy(out=SSn[:, c0:pc0], in_=SP[:, c0:pc0]),
                                      reads=[("scr", "SP", i)], writes=[nk])
                            P.add("pool", lambda e: e.tensor_tensor(
                                out=SSn[:, pc0:512], in0=SP[:, pc0:512], in1=pss[:, pc0:512], op=ALU.add),
                                reads=[("scr", "SP", i), pk], writes=[nk])
                        state["prev_ss"] = (SSn, nk, c0)

                def stB(p, n):
                    i = n % 2
                    c0 = p["c0"]
                    cols = slice(c0, 512)
                    ie = n % 3
                    E, SP, G, Wt = Eb[ie], SPb[i], Gb[i], Wb[i]
                    bc = rr6()
                    pss = p["pss"]
                    P.add("pe", lambda e: e.matmul(ps[bc][:, cols], lhsT=TRI, rhs=SP[:, cols], start=True,
                                                   stop=(pss is None)),
                          reads=[("scr", "SP", i), ("cb",)], writes=[("ps", bc)])
                    if pss is not None:
                        ssb, pk, pc0 = pss
                        P.add("pe", lambda e: e.matmul(ps[bc][:, pc0:512], lhsT=ones1[:], rhs=ssb[:, pc0:512],
                                                       start=False, stop=True),
                              reads=[pk, ("ones",)], writes=[("ps", bc)])
                    dummies(NDUM["sb"])
                    P.add("act", lambda e: e.activation(out=G[:, cols], in_=ps[bc][:, cols], func=AF.Exp, scale=-1.0),
                          reads=[("ps", bc)], writes=[("scr", "G", i)])
                    P.add("dve", lambda e: e.tensor_tensor(out=Wt[:, cols], in0=E[:, cols], in1=G[:, cols], op=ALU.mult),
                          reads=[("scr", "E", ie), ("scr", "G", i)], writes=[("scr", "W", i)])

                def stC(p, n, hp=hp):
                    i = n % 2
                    c0, bo, pj, kt, qt = p["c0"], p["bo"], p["pj"], p["kt"], p["qt"]
                    cols = slice(c0, 512)
                    Wt = Wb[i]
                    if p["first"]:
                        P.add("pe", lambda e: e.matmul(ps[bo][0:64, :], lhsT=zer[:, 0:64], rhs=cb[:, 0:512],
                                                       start=True, stop=False),
                              reads=[("ones",), ("cb",)], writes=[("ps", bo)])
                    P.add("pe", lambda e: e.matmul(ps[bo][0:64, cols], lhsT=vv[:, kt, pj], rhs=Wt[:, cols],
                                                   start=False, stop=p["last"]),
                          reads=[("scr", "W", i), ("scr", "v", kt // 4)], writes=[("ps", bo)])
                    if p["last"]:
                        P.add("dve", lambda e: e.tensor_copy(out=bigB[pj, hp, TT(qt)], in_=ps[bo][0:64, :]),
                              reads=[("ps", bo)], writes=[("bigB", hp, qt, pj.start)])

                NPR = len(pairs)
                for n in range(NPR + 2):
                    if n < NPR:
                        stA(pairs[n], n)
                    if 1 <= n <= NPR:
                        stB(pairs[n - 1], n - 1)
                    if n >= 2:
                        stC(pairs[n - 2], n - 2)

        def moba_attention(l):
            P.fence("scr")
            P.fence("bigC")
            P.add("dve", lambda e: e.memset(vext, 1.0), writes=[("scr", "vext")])
            for j in range(2):
                P.add("pool", lambda e, j=j: e.dma_start(out=kaug[64:75, j, :], in_=kst_d[64:75, :]),
                      writes=[("scr", "kst", j)], dma="c1")
            ksf = scr[0:64, 0:16].rearrange("p (j n) -> p j n", j=2)
            gm = scr[:, 64:128].rearrange("p (g n) -> p g n", g=8)
            cmp_ = scr[:, 128:640].rearrange("p (g n m) -> p g n m", g=8, n=8)
            cntt = scr[:, 640:704].rearrange("p (g n) -> p g n", g=8)
            t1 = scr[:, 704:768].rearrange("p (g n) -> p g n", g=8)
            rden = scr[0:64, 768:1280]
            Pm = [scr[:, 1280:1536].bitcast(BF16), scr[:, 1536:1792].bitcast(BF16), scr[:, 1792:2048].bitcast(BF16)]
            cnt = {"p": 0}
            GM = cst[:, C_GM:C_GM + 128].rearrange("p (o j n) -> p o j n", o=8, j=2)
            LL = cst[:, C_L:C_L + 64].rearrange("p (o n) -> p o n", o=8)
            for hp in range(4):
                keys, (wq, wk) = wload([wview(Win, l, 0, D, OFF_MBQ + hp * 128, 128),
                                        wview(Win, l, 0, D, OFF_MBK + hp * 128, 128)])
                keys2, (wv,) = wload([wview(Win, l, 0, D, OFF_MBV + hp * 128, 128)])
                keys = keys + keys2

                def evq(t, b):
                    P.add("act", lambda e, t=t, b=b: e.copy(out=qaug[0:64, 0, TT(t)], in_=ps[b][0:64, :]),
                          reads=[("ps", b)], writes=[("scr", "qaug", 0, t)])
                    P.add("dve", lambda e, t=t, b=b: e.tensor_copy(out=qaug[0:64, 1, TT(t)], in_=ps[b][64:128, :]),
                          reads=[("ps", b)], writes=[("scr", "qaug", 1, t)])
                proj_fm(keys[0], wq, evq)

                def evk(t, b):
                    P.add("act", lambda e, t=t, b=b: e.copy(out=kaug[0:64, 0, TT(t)], in_=ps[b][0:64, :]),
                          reads=[("ps", b)], writes=[("scr", "kaug", 0, t)])
                    P.add("dve", lambda e, t=t, b=b: e.tensor_copy(out=kaug[0:64, 1, TT(t)], in_=ps[b][64:128, :]),
                          reads=[("ps", b)], writes=[("scr", "kaug", 1, t)])
                    for j in range(2):
                        P.add("dve", lambda e, t=t, b=b, j=j: e.tensor_reduce(
                            out=ksf[:, j, 2 * t:2 * t + 2], in_=ps[b][64 * j:64 * j + 64, :].rearrange("p (a n) -> p a n", a=2),
                            axis=AX.X, op=ALU.add),
                            reads=[("ps", b)], writes=[("scr", "ksf", j, t)])
                proj_fm(keys[1], wk, evk)
                P.add("dve", lambda e: e.tensor_copy(out=ksumb[:], in_=ksf),
                      reads=[("scr", "ksf", j, t) for j in range(2) for t in range(4)], writes=[("ksumb",)])
                for g in range(4):
                    b = rr8()
                    for ti in range(4):
                        t16 = g * 4 + ti
                        for kc in range(8):
                            P.add("pe", lambda e, kc=kc, t16=t16, ti=ti, b=b, wv=wv: e.matmul(
                                ps[b][:, ti * 128:(ti + 1) * 128], lhsT=xn[:, kc, t16 * 128:(t16 + 1) * 128],
                                rhs=wv[:, kc, :], start=(kc == 0), stop=(kc == 7)),
                                reads=[keys[2], ("xn", kc, t16 // 4)], writes=[("ps", b)])
                    P.add("act", lambda e, g=g, b=b: e.copy(
                        out=vext[:, g * 4:(g + 1) * 4, :, 0:64],
                        in_=ps[b][:].rearrange("p (t j n) -> p t j n", t=4, j=2)),
                        reads=[("ps", b), ("scr", "vext")], writes=[("scr", "vext", g)])
                for qt in range(4):
                    bg = rr8()
                    for ti in range(4):
                        t16 = qt * 4 + ti
                        for j in range(2):
                            g8 = ti * 2 + j
                            P.add("pe", lambda e, bg=bg, g8=g8, j=j, t16=t16: e.matmul(
                                ps[bg][:, g8 * 8:(g8 + 1) * 8], lhsT=qaug[0:64, j, t16 * 128:(t16 + 1) * 128],
                                rhs=ksumb[:, j, :], start=True, stop=True),
                                reads=[("scr", "qaug", j, qt), ("ksumb",)], writes=[("ps", bg)])
                    for ti in range(4):
                        own = (qt * 4 + ti) // 2
                        P.add("dve", lambda e, bg=bg, ti=ti, own=own: e.tensor_tensor(
                            out=gm[:, 2 * ti:2 * ti + 2, :],
                            in0=ps[bg][:, 16 * ti:16 * ti + 16].rearrange("p (j n) -> p j n", j=2),
                            in1=GM[:, own, :, :], op=ALU.add),
                            reads=[("ps", bg), ("cst",)], writes=[("scr", "gm")])
                    gap = [list(a) for a in gm.ap]
                    gm_m = bass.AP(gm.tensor, gm.offset, [gap[0], gap[1], [0, 8], gap[2]])
                    gm_n = bass.AP(gm.tensor, gm.offset, [gap[0], gap[1], gap[2], [0, 8]])
                    P.add("dve", lambda e, gm_m=gm_m, gm_n=gm_n: e.tensor_tensor(
                        out=cmp_, in0=gm_m, in1=gm_n, op=ALU.is_gt),
                        reads=[("scr", "gm")], writes=[("scr", "cmp")])
                    P.add("dve", lambda e: e.tensor_reduce(out=cntt, in_=cmp_, axis=AX.X, op=ALU.add),
                          reads=[("scr", "cmp")], writes=[("scr", "cnt")])
                    P.add("dve", lambda e: e.tensor_scalar(out=t1, in0=cntt, scalar1=2.5, scalar2=BIG,
                                                           op0=ALU.is_lt, op1=ALU.mult),
                          reads=[("scr", "cnt")], writes=[("scr", "t1")])
                    for ti in range(4):
                        own = (qt * 4 + ti) // 2
                        for j in range(2):
                            hh = 2 * hp + j
                            P.add("dve", lambda e, ti=ti, j=j, hh=hh, own=own: e.scalar_tensor_tensor(
                                out=selT[:, hh, ti, 64:72], in0=t1[:, 2 * ti + j, :], scalar=-BIG, in1=LL[:, own, :],
                                op0=ALU.add, op1=ALU.max),
                                reads=[("scr", "t1"), ("cst",), ("selst",)], writes=[("selT", hh, ti)])
                    for j in range(2):
                        hh = 2 * hp + j
                        bt = rr8()
                        for ti in range(4):
                            P.add("pe", lambda e, bt=bt, ti=ti, hh=hh: e.matmul(
                                ps[bt][0:80, ti * 128:(ti + 1) * 128], lhsT=selT[:, hh, ti, :], rhs=IDN,
                                start=True, stop=True),
                                reads=[("selT", hh, ti), ("cb",)], writes=[("ps", bt)])
                        P.add("act", lambda e, bt=bt, j=j, qt=qt: e.copy(out=qaug[64:75, j, TT(qt)], in_=ps[bt][64:75, :]),
                              reads=[("ps", bt)], writes=[("scr", "qst", j, qt)])
                pairs = []
                for j in range(2):
                    for qt in range(4):
                        nkt = (qt + 1) * 4
                        bo = rrO()
                        for kt in range(nkt):
                            k0, q0 = kt * 128, qt * 512
                            diag = k0 >= q0
                            c0 = k0 - q0 if diag else 0
                            pairs.append(dict(j=j, qt=qt, kt=kt, k0=k0, q0=q0, diag=diag, c0=c0, first=(kt == 0),
                                              last=(kt == nkt - 1), bo=bo, hh=2 * hp + j))

                def mA(p, n):
                    c0, q0, k0, j, kt, qt = p["c0"], p["q0"], p["k0"], p["j"], p["kt"], p["qt"]
                    cols = slice(c0, 512)
                    qcols = slice(q0 + c0, q0 + 512)
                    bs = rr6()
                    p["bs"] = bs
                    P.add("pe", lambda e: e.matmul(
                        ps[bs][:, cols], lhsT=kaug[0:75, j, k0:k0 + 128], rhs=qaug[0:75, j, qcols],
                        start=True, stop=not p["diag"]),
                        reads=[("scr", "kaug", j, kt // 4), ("scr", "kst", j), ("scr", "qaug", j, qt), ("scr", "qst", j, qt)],
                        writes=[("ps", bs)])
                    if p["diag"]:
                        P.add("pe", lambda e: e.matmul(ps[bs][:, c0:c0 + 128], lhsT=IDN, rhs=NEGM2, start=False, stop=True),
                              reads=[("cb",)], writes=[("ps", bs)])

                def mB(p, n):
                    i = n % 3
                    c0, bs = p["c0"], p["bs"]
                    cols = slice(c0, 512)
                    pm = Pm[i]
                    biasc = float(-SLOPES[p["hh"]] * (p["q0"] - p["k0"]))
                    P.add("act", lambda e: e.activation(
                        out=pm[:, cols], in_=ps[bs][:, cols], func=AF.Exp, scale=0.125, bias=biasc),
                        reads=[("ps", bs)], writes=[("scr", "Pm", i)])

                def mC(p, n, hp=hp):
                    i = n % 3
                    c0, bo, j, kt, qt = p["c0"], p["bo"], p["j"], p["kt"], p["qt"]
                    cols = slice(c0, 512)
                    pm = Pm[i]
                    pj = slice(64 * j, 64 * j + 64)
                    P.add("pe", lambda e: e.matmul(ps[bo][:, cols], lhsT=vext[:, kt, j, :], rhs=pm[:, cols],
                                                   start=p["first"], stop=p["last"]),
                          reads=[("scr", "Pm", i), ("scr", "vext", kt // 4), ("scr", "vext")], writes=[("ps", bo)])
                    if p["last"]:
                        P.add("dve", lambda e: e.reciprocal(out=rden, in_=ps[bo][64:128, :]),
                              reads=[("ps", bo)], writes=[("scr", "rden")])
                        P.add("dve", lambda e: e.tensor_tensor(
                            out=bigC[pj, hp, TT(qt)], in0=ps[bo][0:64, :], in1=rden, op=ALU.mult),
                            reads=[("ps", bo), ("scr", "rden")], writes=[("bigC", hp, qt, pj.start)])

                NPR = len(pairs)
                for n in range(NPR + 2):
                    if n < NPR:
                        mA(pairs[n], n)
                    if 1 <= n <= NPR:
                        mB(pairs[n - 1], n - 1)
                    if n >= 2:
                        mC(pairs[n - 2], n - 2)

        def mix_out(l, have):
            P.fence("scr")
            mixed = scr[:, 0:4096].bitcast(BF16).rearrange("p (k n) -> p k n", k=8)
            gt = [scr[:, 4096 + 512 * i:4608 + 512 * i] for i in range(3)]
            tt_ = [scr[:, 5632 + 512 * i:6144 + 512 * i] for i in range(3)]
            srcs = [("sb", bigB, WA, "bigB"), ("mb", bigC, WB, "bigC"), ("conv", bigA, WC, "bigA")]
            for half in range(2):
                for dc in range(8):
                    kg1, gw1 = wload([wview(Win, l, 0, D, OFF_GATE + br * D + dc * 128, 128) for br in range(2)])
                    kg2, gw2 = wload([wview(Win, l, 0, D, OFF_GATE + 2 * D + dc * 128, 128),
                                      wview(WA, l, 0, 512, dc * 128, 128), wview(WB, l, 0, 512, dc * 128, 128)])
                    kg3, gw3 = wload([wview(WC, l, 0, 512, dc * 128, 128)])
                    keysg, gw = kg1 + kg2[0:1], gw1 + gw2[0:1]
                    keysy, yw = kg2[1:3] + kg3, gw2[1:3] + gw3
                    for t2 in range(2):
                        t = half * 2 + t2
                        first = True
                        for br, (nm, ob, W_, blk) in enumerate(srcs):
                            if nm not in have:
                                continue
                            bgate = rr8()
                            for kc in range(8):
                                P.add("pe", lambda e, kc=kc, t=t, bgate=bgate, gwb=gw[br]: e.matmul(
                                    ps[bgate][:], lhsT=gwb[:, kc, :], rhs=xn[:, kc, TT(t)],
                                    start=(kc == 0), stop=(kc == 7)),
                                    reads=[keysg[br], ("xn", kc, t)], writes=[("ps", bgate)])
                            by = rr8()
                            for kc in range(4):
                                if nm == "conv":
                                    rk = [("bigA", kc, t)]
                                else:
                                    rk = [(blk, kc, t, 0), (blk, kc, t, 64)]
                                P.add("pe", lambda e, kc=kc, t=t, by=by, ywb=yw[br], ob=ob: e.matmul(
                                    ps[by][:], lhsT=ywb[:, kc, :], rhs=ob[:, kc, TT(t)],
                                    start=(kc == 0), stop=(kc == 3)),
                                    reads=[keysy[br]] + rk, writes=[("ps", by)])
                            P.add("act", lambda e, br=br, bgate=bgate, dc=dc: e.activation(
                                out=gt[br], in_=ps[bgate][:], func=AF.Sigmoid, bias=vcol(l, V_GB, br * 8 + dc)),
                                reads=[("ps", bgate), ("vecs",)], writes=[("scr", "gt", br)])
                            P.add("dve", lambda e, br=br, by=by: e.tensor_tensor(
                                out=tt_[br], in0=gt[br], in1=ps[by][:], op=ALU.mult),
                                reads=[("scr", "gt", br), ("ps", by)], writes=[("scr", "tt", br)])
                        live = [br for br, s_ in enumerate(srcs) if s_[0] in have]
                        mo = mixed[:, dc, t2 * 512:(t2 + 1) * 512]
                        mk_ = ("scr", "mixed", dc, t2)
                        if len(live) == 1:
                            P.add("pool", lambda e, mo=mo, a=live[0]: e.tensor_copy(out=mo, in_=tt_[a]),
                                  reads=[("scr", "tt", live[0])], writes=[mk_])
                        elif len(live) == 2:
                            P.add("pool", lambda e, mo=mo, a=live[0], b_=live[1]: e.tensor_tensor(
                                out=mo, in0=tt_[a], in1=tt_[b_], op=ALU.add),
                                reads=[("scr", "tt", live[0]), ("scr", "tt", live[1])], writes=[mk_])
                        else:
                            P.add("pool", lambda e: e.tensor_tensor(out=tt_[0], in0=tt_[0], in1=tt_[1], op=ALU.add),
                                  reads=[("scr", "tt", 0), ("scr", "tt", 1)], writes=[("scr", "tt", 0)])
                            P.add("pool", lambda e, mo=mo: e.tensor_tensor(out=mo, in0=tt_[0], in1=tt_[2], op=ALU.add),
                                  reads=[("scr", "tt", 0), ("scr", "tt", 2)], writes=[mk_])
                dump("mixed", scr[:, 0:4096].bitcast(BF16), [("scr", "mixed", dc_, t_) for dc_ in range(8) for t_ in range(2)])
                for d0 in range(0, 8, 2):
                    keys, (wo,) = wload([wview(WO, l, 0, D, d0 * 128, 256)])
                    for di in range(2):
                        dc = d0 + di
                        for t2 in range(2):
                            t = half * 2 + t2
                            b = rr8()
                            for kc in range(8):
                                P.add("pe", lambda e, kc=kc, t2=t2, b=b, di=di, wo=wo: e.matmul(
                                    ps[b][:], lhsT=wo[:, kc, di * 128:(di + 1) * 128],
                                    rhs=mixed[:, kc, t2 * 512:(t2 + 1) * 512], start=(kc == 0), stop=(kc == 7)),
                                    reads=[keys[0], ("scr", "mixed", kc, t2)], writes=[("ps", b)])
                            P.add("dve", lambda e, b=b, dc=dc, t=t: e.tensor_tensor(
                                out=h[:, dc, TT(t)], in0=ps[b][:], in1=h[:, dc, TT(t)], op=ALU.add),
                                reads=[("ps", b)], writes=[("h", dc, t)])

        outs = []
        for s_i in range(nseq):
            for dc in range(8):
                P.add("sp", lambda e, dc=dc, s_i=s_i: e.dma_start(out=h[:, dc, :], in_=xT[s_i, dc * 128:(dc + 1) * 128, :]),
                      writes=[("h", dc, t) for t in range(4)], dma="x%d" % dc)
            for l in range(depth):
                if "ffn1" in stages:
                    ffn(l, W1i, W1o, V_FFN1N)
                if "mix" in stages:
                    rmsnorm(l, V_MIXN)
                    if "conv" in mix_parts:
                        conv_branch(l)
                    if "sb" in mix_parts:
                        sb_attention(l)
                    if "mb" in mix_parts:
                        moba_attention(l)
                    mix_out(l, mix_parts)
                if "ffn2" in stages:
                    ffn(l, W2i, W2o, V_FFN2N)
            if "final" in stages:
                rmsnorm(None, depth * NVEC_L, out_f32=h)
            for dc in range(8):
                outs.append(P.add("sp", lambda e, dc=dc, s_i=s_i: e.dma_start(
                    out=outT[s_i, dc * 128:(dc + 1) * 128, :], in_=h[:, dc, :]),
                    reads=[("h", dc, t) for t in range(4)], dma="o%d" % dc))
        P.emit(nc, final_waits=outs[-8:] + dbg_ops)
    return nc


_CACHE = {}


def kernel(**inputs):
    x = np.asarray(inputs["x"], np.float32)
    B = x.shape[0]
    per = B // NCORES
    cst, kst, sel = host_consts()
    vecs = host_vecs(inputs, DEPTH_FULL)
    nc = build(per, DEPTH_FULL)
    shared = {k: np.ascontiguousarray(np.asarray(inputs[k], np.float32)) for k in
              ("ffn1_w_in", "ffn1_w_out", "ffn2_w_in", "ffn2_w_out", "w_in", "sb_w_out", "mb_w_out",
               "conv_w_out", "w_o")}
    shared.update({"vecs": vecs, "cst": cst, "kst": kst, "selst": sel})
    in_maps = []
    for c in range(NCORES):
        xs = x[c * per:(c + 1) * per]
        m = dict(shared)
        m["xT"] = np.ascontiguousarray(xs.transpose(0, 2, 1))
        in_maps.append(m)
    res = run_bass_kernel_spmd(nc, in_maps, core_ids=list(range(NCORES)))
    out = np.empty((B, S, D), np.float32)
    for c in range(NCORES):
        out[c * per:(c + 1) * per] = res.results[c]["outT"].transpose(0, 2, 1)
    return out
```

```python
import contextlib
import numpy as np
import concourse.bass as bass
import concourse.mybir as mybir
from concourse.bass_utils import run_bass_kernel_spmd

F32 = mybir.dt.float32
BF16 = mybir.dt.bfloat16
AF = mybir.ActivationFunctionType
ALU = mybir.AluOpType
AX = mybir.AxisListType

D = 1024
S = 2048
DFF = 2816
NFC = 22
DEPTH_FULL = 2
NCORES = 8
SEQ_PER_CORE = 4
OFF_SBQ, OFF_SBK, OFF_SBV = 0, 512, 1024
OFF_MBQ, OFF_MBK, OFF_MBV = 1536, 2048, 2560
OFF_CONV = 3072
OFF_GATE = 4096
IN_COLS = 7168
EPS = 1e-6
BIG = 29952.0
SLOPES = [2.0 ** (-(h + 1)) for h in range(8)]
ENGS = ("pe", "act", "dve", "pool", "sp")


class Op:
    __slots__ = ("eng", "fn", "pos", "sig", "waits", "dma", "val")

    def __init__(self, eng, fn, dma=None):
        self.eng, self.fn, self.dma = eng, fn, dma
        self.pos, self.sig, self.waits, self.val = -1, False, [], 0


class Prog:
    def __init__(self):
        self.streams = {e: [] for e in ENGS}
        self.last_w, self.readers = {}, {}
        self.seen = {e: {} for e in ENGS}
        self.dma_cnt = {}
        self.fences = {}

    def _need(self, x, y, raw):
        if y is None or y is x:
            return
        if y.dma is not None:
            key = ("d", y.dma)
            val = self.dma_cnt[y.dma] - (16 if x.dma == y.dma else 0)
            if self.seen[x.eng].get(key, 0) >= val:
                return
            self.seen[x.eng][key] = val
            x.waits.append(("d", y.dma, val))
            return
        if y.eng == x.eng and x.dma is None:
            if x.eng == "pe" or not raw:
                return
            if len(self.streams[x.eng]) - y.pos > 3:
                return
        key = ("e", y.eng)
        if self.seen[x.eng].get(key, -1) >= y.pos:
            return
        self.seen[x.eng][key] = y.pos
        y.sig = True
        x.waits.append(("e", y.eng, y))

    def fence(self, block):
        ops = {}
        for k in [k for k in self.last_w if k[0] == block]:
            o = self.last_w.pop(k)
            ops[id(o)] = o
        for k in [k for k in self.readers if k[0] == block]:
            for o in self.readers.pop(k):
                ops[id(o)] = o
        best = {}
        for o in ops.values():
            kk = (o.eng, o.dma)
            rank = o.val if o.dma is not None else o.pos
            if kk not in best or rank > best[kk][0]:
                best[kk] = (rank, o)
        if best:
            self.fences[block] = [v[1] for v in best.values()]

    def add(self, eng, fn, reads=(), writes=(), dma=None):
        x = Op(eng, fn, dma)
        if dma is not None:
            self.dma_cnt[dma] = self.dma_cnt.get(dma, 0) + 16
            x.val = self.dma_cnt[dma]
        reads = list(reads)
        writes = list(writes)
        for r in list(reads):
            if r[0] == "ps":
                reads.remove(r)
                writes.append(r)
        for k in reads + writes:
            if k not in self.last_w and k[0] in self.fences:
                for o in self.fences[k[0]]:
                    self._need(x, o, True)
        for r in reads:
            self._need(x, self.last_w.get(r), True)
        for w in writes:
            self._need(x, self.last_w.get(w), True)
            for rd in self.readers.get(w, ()):
                self._need(x, rd, False)
        x.pos = len(self.streams[eng])
        self.streams[eng].append(x)
        for r in reads:
            self.readers.setdefault(r, []).append(x)
        for w in writes:
            self.last_w[w] = x
            self.readers[w] = []
        return x

    def emit(self, nc, final_waits=()):
        with contextlib.ExitStack() as es:
            esem = {e: es.enter_context(nc.semaphore("s_" + e)) for e in ENGS}
            dsem = {n: es.enter_context(nc.semaphore("d_" + n)) for n in self.dma_cnt}
            block = es.enter_context(nc.Block())
            for e in ENGS:
                c = 0
                for op in self.streams[e]:
                    if op.dma is None:
                        if op.sig:
                            c += 1
                        op.val = c
            hooks = {"pe": block.tensor, "act": block.scalar, "dve": block.vector,
                     "pool": block.gpsimd, "sp": block.sync}

            def mk(e):
                def body(eng):
                    for op in self.streams[e]:
                        for w in op.waits:
                            if w[0] == "d":
                                eng.wait_ge(dsem[w[1]], w[2])
                            else:
                                eng.wait_ge(esem[w[1]], w[2].val)
                        ins = op.fn(eng)
                        if op.dma is not None:
                            ins.then_inc(dsem[op.dma], 16)
                        elif op.sig:
                            ins.then_inc(esem[e], 1)
                    if e == "sp":
                        for op in final_waits:
                            eng.wait_ge(dsem[op.dma], op.val)
                return body
            for e in ENGS:
                hooks[e](mk(e))


NVEC_L = 8 + 8 + 8 + 24 + 4 + 4 + 4 + 124
V_FFN1N, V_MIXN, V_FFN2N, V_GB, V_DWB, V_LNG, V_LNB, V_DW = 0, 8, 16, 24, 48, 52, 56, 60
C_TRI, C_MLT, C_MLE, C_ID, C_GM, C_L = 0, 128, 256, 384, 512, 640
NCST = 704


def host_consts():
    c = np.zeros((128, NCST), np.float32)
    j = np.arange(128)[:, None]
    s = np.arange(128)[None, :]
    c[:, C_TRI:C_TRI + 128] = (j >= s)
    c[:, C_MLT:C_MLT + 128] = np.where(j >= s, -BIG, 0.0)
    c[:, C_MLE:C_MLE + 128] = np.where(j > s, -BIG, 0.0)
    c[:, C_ID:C_ID + 128] = (j == s)
    own = np.arange(8)[:, None]
    n = np.arange(8)[None, :]
    gm = np.where(n < own, 0.0, -BIG).astype(np.float32)
    ll = np.where(n < own, -BIG, 0.0).astype(np.float32)
    c[:, C_GM:C_GM + 128] = np.repeat(gm[:, None, :], 2, axis=1).reshape(1, 128)
    c[:, C_L:C_L + 64] = ll.reshape(1, 64)
    kst = np.zeros((128, S), np.float32)
    pos = np.arange(S)
    for nn in range(8):
        kst[64 + nn] = (pos // 256 == nn)
    kst[72] = 1.0
    kst[73] = 1.0
    kst[74] = pos % 128
    sel = np.zeros((128, 8, 4, 80), np.float32)
    q = np.arange(128)
    for h in range(8):
        for m in range(4):
            i = m * 128 + q
            sel[:, h, m, 72] = -8.0 * SLOPES[h] * (256 * (i // 256))
            sel[:, h, m, 73] = -8.0 * SLOPES[h] * (i % 256)
            sel[:, h, m, 74] = 8.0 * SLOPES[h]
    return c, kst, sel.reshape(128, 8 * 4 * 80)


def host_vecs(inp, depth):
    def col(v):
        return np.ascontiguousarray(np.asarray(v, np.float32).reshape(-1, 128).T)
    cols = []
    for l in range(depth):
        cols += [col(inp["ffn1_norm"][l]), col(inp["mix_norm"][l]), col(inp["ffn2_norm"][l]),
                 col(inp["gate_bias"][l]), col(inp["conv_dw_bias"][l]), col(inp["conv_ln_g"][l]),
                 col(inp["conv_ln_b"][l])]
        dw = np.asarray(inp["conv_dw"][l], np.float32).reshape(31, 512)
        cols.append(np.ascontiguousarray(dw.reshape(31, 4, 128).transpose(2, 0, 1).reshape(128, 124)))
    cols.append(col(inp["final_norm"]))
    return np.ascontiguousarray(np.concatenate(cols, axis=1))


def build(nseq=SEQ_PER_CORE, depth=DEPTH_FULL, stages=("ffn1", "mix", "ffn2", "final"),
          mix_parts=("conv", "sb", "mb"), dbg=None):
    nc = bass.Bass("TRN2", target_bir_lowering=False)
    dram = {}

    def din(name, shape):
        dram[name] = nc.dram_tensor(name, list(shape), F32, kind="ExternalInput").ap()
        return dram[name]

    xT = din("xT", [nseq, D, S])
    W1i = din("ffn1_w_in", [depth, D, 2 * DFF])
    W1o = din("ffn1_w_out", [depth, DFF, D])
    W2i = din("ffn2_w_in", [depth, D, 2 * DFF])
    W2o = din("ffn2_w_out", [depth, DFF, D])
    Win = din("w_in", [depth, D, IN_COLS])
    WA = din("sb_w_out", [depth, 512, D])
    WB = din("mb_w_out", [depth, 512, D])
    WC = din("conv_w_out", [depth, 512, D])
    WO = din("w_o", [depth, D, D])
    NV = depth * NVEC_L + 8
    vecs_d = din("vecs", [128, NV])
    cst_d = din("cst", [128, NCST])
    kst_d = din("kst", [128, S])
    sel_d = din("selst", [128, 8 * 4 * 80])
    outT = nc.dram_tensor("outT", [nseq, D, S], F32, kind="ExternalOutput").ap()
    dbg_d = nc.dram_tensor("dbg", [128, 8192], F32, kind="ExternalOutput").ap() if dbg else None
    dbg_ops = []

    def dump(name, ap, keys):
        if dbg == name and not dbg_ops:
            n = ap.shape[1]
            dbg_ops.append(P.add("pool", lambda e: e.dma_start(out=dbg_d[:, 0:n], in_=ap), reads=keys, dma="dbg"))

    P = Prog()
    es = contextlib.ExitStack()
    with es:
        def sb(name, shape, dt):
            return es.enter_context(nc.sbuf_tensor(name, list(shape), dt))

        h = sb("h", [128, 8, S], F32)
        xn = sb("xn", [128, 8, S], BF16)
        bigA = sb("bigA", [128, 4, S], BF16)
        bigB = sb("bigB", [128, 4, S], BF16)
        bigC = sb("bigC", [128, 4, S], BF16)
        scr = sb("scr", [128, 8192], F32)
        NW = 4
        WSZ = 2048
        wsl = [sb("w%d" % i, [128, WSZ], BF16) for i in range(NW)]
        vecs = sb("vecs_sb", [128, NV], F32)
        cst = sb("cst_sb", [128, NCST], F32)
        cb = sb("cst_bf", [128, 512], BF16)
        ones_d = sb("ones_d", [128, 128], BF16)
        ones_c = sb("ones_c", [128, 128], BF16)
        ones1 = sb("ones1", [128, 128], BF16)
        zer = sb("zer", [128, 128], BF16)
        eps_c = sb("eps_c", [128, 1], F32)
        kaug = scr[:, 2048:4096].bitcast(BF16).rearrange("p (j n) -> p j n", j=2)
        qaug = scr[:, 4096:6144].bitcast(BF16).rearrange("p (j n) -> p j n", j=2)
        vext = scr[:, 6144:8192].bitcast(BF16).rearrange("p (t j n) -> p t j n", t=16, j=2)
        selT = sb("selT", [128, 8, 4, 80], BF16)
        ksumb = sb("ksumb", [64, 2, 8], BF16)
        ps = [es.enter_context(nc.psum_tensor("ps%d" % i, [128, 512], F32)) for i in range(8)]

        class RR:
            def __init__(self, ids):
                self.ids, self.i = list(ids), 0

            def __call__(self):
                b = self.ids[self.i % len(self.ids)]
                self.i += 1
                return b
        rr8 = RR(range(8))
        rr6 = RR(range(5))
        NDUM = {"sb": 3}

        def dummies(n):
            for _ in range(n):
                P.add("pe", lambda e: e.matmul(ps[5][:], lhsT=zer[:], rhs=cb[:, 0:512], start=True, stop=True),
                      reads=[("ones",), ("cb",)], writes=[("ps", 5)])
        rrO = RR([6, 7])
        wstate = {"i": 0}

        def wload(parts, eng="pool"):
            si = wstate["i"] % NW
            wstate["i"] += 1
            P.fence("w%d" % si)
            off = 0
            views, keys = [], []
            for pi, ap in enumerate(parts):
                K, n = ap.shape[1], ap.shape[2]
                v = wsl[si][:, off:off + K * n].rearrange("p (k n) -> p k n", k=K)
                key = ("w%d" % si, pi)
                P.add(eng, lambda e, v=v, ap=ap: e.dma_start(out=v, in_=ap), writes=[key], dma="w%d" % si)
                views.append(v)
                keys.append(key)
                off += K * n
            assert off <= WSZ
            return keys, views

        def wview(Wd, l, r0, nrow, c0, ncol):
            return Wd[l, r0:r0 + nrow, c0:c0 + ncol].rearrange("(k p) n -> p k n", p=128)

        def vcol(l, base, i):
            c = l * NVEC_L + base + i
            return vecs[:, c:c + 1]

        def TT(t):
            return slice(t * 512, (t + 1) * 512)

        P.add("sp", lambda e: e.dma_start(out=vecs[:], in_=vecs_d), writes=[("vecs",)], dma="c0")
        P.add("sp", lambda e: e.dma_start(out=cst[:], in_=cst_d), writes=[("cst",)], dma="c0")
        P.add("dve", lambda e: e.tensor_copy(out=cb[:], in_=cst[:, 0:512]), reads=[("cst",)], writes=[("cb",)])
        P.add("dve", lambda e: e.memset(ones_d[:], 1.0 / 1024), writes=[("ones",)])
        P.add("dve", lambda e: e.memset(ones_c[:], 1.0 / 512), writes=[("ones",)])
        P.add("dve", lambda e: e.memset(ones1[:], 1.0), writes=[("ones",)])
        P.add("dve", lambda e: e.memset(zer[:], 0.0), writes=[("ones",)])
        P.add("dve", lambda e: e.memset(eps_c[:], EPS), writes=[("ones",)])
        P.add("pool", lambda e: e.dma_start(out=selT[:].rearrange("p a b c -> p (a b c)"), in_=sel_d),
              writes=[("selst",)], dma="c1")
        TRI = cb[:, C_TRI:C_TRI + 128]
        NEGM2 = cb[:, C_MLE:C_MLE + 128]
        NEGM = cb[:, C_MLT:C_MLT + 128]
        IDN = cb[:, C_ID:C_ID + 128]
        MLT = cst[:, C_MLT:C_MLT + 128]

        def rmsnorm(l, base, out_bf=True, out_f32=None):
            P.fence("scr")
            sqbs = [scr[:, 0:2048].bitcast(BF16).rearrange("p (k n) -> p k n", k=8),
                    scr[:, 2048:4096].bitcast(BF16).rearrange("p (k n) -> p k n", k=8)]
            rstds = [scr[:, 4096:4608], scr[:, 4608:5120]]
            banks = {}

            def sqs(t):
                sqb = sqbs[t % 2]
                for dc in range(8):
                    P.add("act", lambda e, dc=dc: e.activation(out=sqb[:, dc, :], in_=h[:, dc, TT(t)], func=AF.Square),
                          reads=[("h", dc, t)], writes=[("scr", "sq", t % 2, dc)])
                bnk = rr8()
                banks[t] = bnk
                for dc in range(8):
                    P.add("pe", lambda e, dc=dc: e.matmul(ps[bnk][:], lhsT=ones_d[:], rhs=sqb[:, dc, :],
                                                          start=(dc == 0), stop=(dc == 7)),
                          reads=[("scr", "sq", t % 2, dc), ("ones",)], writes=[("ps", bnk)])

            def fin(t):
                bnk = banks[t]
                rstd = rstds[t % 2]
                rk = ("scr", "rstd", t % 2)
                P.add("act", lambda e: e.activation(out=rstd, in_=ps[bnk][:], func=AF.Sqrt, bias=eps_c[:, 0:1]),
                      reads=[("ps", bnk), ("ones",)], writes=[rk])
                P.add("dve", lambda e: e.reciprocal(out=rstd, in_=rstd), reads=[rk], writes=[rk])
                for dc in range(8):
                    if out_f32 is None:
                        o = xn[:, dc, TT(t)]
                        wk = ("xn", dc, t)
                    else:
                        o = out_f32[:, dc, TT(t)]
                        wk = ("h", dc, t)
                    c = base + dc if l is None else l * NVEC_L + base + dc
                    P.add("dve", lambda e, o=o, dc=dc, c=c: e.scalar_tensor_tensor(
                        out=o, in0=h[:, dc, TT(t)], scalar=vecs[:, c:c + 1], in1=rstd, op0=ALU.mult, op1=ALU.mult),
                        reads=[("h", dc, t), rk, ("vecs",)], writes=[wk])

            sqs(0)
            for t in range(4):
                if t + 1 < 4:
                    sqs(t + 1)
                fin(t)

        def ffn(l, Wi, Wo, nbase):
            rmsnorm(l, nbase)
            P.fence("scr")
            P.fence("bigA")
            P.fence("bigB")
            sil = [scr[:, 5120:5632], scr[:, 5632:6144]]
            groups = [(0, 8), (8, 7), (15, 7)]
            sidx = 0
            for (g0, gn) in groups:
                def hid(c, t):
                    return (bigA if c < 4 else bigB)[:, c % 4, TT(t)]
                for c0 in range(0, gn, 1):
                    ncnk = 1
                    fc = g0 + c0
                    keys, (wa, wb) = wload([wview(Wi, l, 0, D, fc * 128, ncnk * 128),
                                            wview(Wi, l, 0, D, DFF + fc * 128, ncnk * 128)])
                    for ci in range(ncnk):
                        c = c0 + ci
                        for t in range(4):
                            ba, bb = rr8(), rr8()
                            for kc in range(8):
                                P.add("pe", lambda e, kc=kc, t=t, ba=ba, ci=ci, wa=wa: e.matmul(
                                    ps[ba][:], lhsT=wa[:, kc, ci * 128:(ci + 1) * 128], rhs=xn[:, kc, TT(t)],
                                    start=(kc == 0), stop=(kc == 7)),
                                    reads=[keys[0], ("xn", kc, t)], writes=[("ps", ba)])
                            for kc in range(8):
                                P.add("pe", lambda e, kc=kc, t=t, bb=bb, ci=ci, wb=wb: e.matmul(
                                    ps[bb][:], lhsT=wb[:, kc, ci * 128:(ci + 1) * 128], rhs=xn[:, kc, TT(t)],
                                    start=(kc == 0), stop=(kc == 7)),
                                    reads=[keys[1], ("xn", kc, t)], writes=[("ps", bb)])
                            st = sil[sidx % 2]
                            sk = ("scr", "sil", sidx % 2)
                            sidx += 1
                            P.add("act", lambda e, st=st, ba=ba: e.activation(out=st, in_=ps[ba][:], func=AF.Silu),
                                  reads=[("ps", ba)], writes=[sk])
                            P.add("dve", lambda e, st=st, bb=bb, c=c, t=t: e.tensor_tensor(
                                out=hid(c, t), in0=st, in1=ps[bb][:], op=ALU.mult),
                                reads=[sk, ("ps", bb)], writes=[("bigA" if c < 4 else "bigB", c % 4, t)])
                for d0 in range(0, 8, 2):
                    keys, (wo,) = wload([wview(Wo, l, g0 * 128, gn * 128, d0 * 128, 256)])
                    for di in range(2):
                        dc = d0 + di
                        for t in range(4):
                            b = rr8()
                            for c in range(gn):
                                P.add("pe", lambda e, c=c, t=t, b=b, di=di, wo=wo, gn=gn: e.matmul(
                                    ps[b][:], lhsT=wo[:, c, di * 128:(di + 1) * 128], rhs=hid(c, t),
                                    start=(c == 0), stop=(c == gn - 1)),
                                    reads=[keys[0], ("bigA" if c < 4 else "bigB", c % 4, t)], writes=[("ps", b)])
                            P.add("dve", lambda e, b=b, dc=dc, t=t: e.scalar_tensor_tensor(
                                out=h[:, dc, TT(t)], in0=ps[b][:], scalar=0.5, in1=h[:, dc, TT(t)],
                                op0=ALU.mult, op1=ALU.add),
                                reads=[("ps", b)], writes=[("h", dc, t)])

        def proj_fm(keyw, wv, evac):
            for t in range(4):
                b = rr8()
                for kc in range(8):
                    P.add("pe", lambda e, kc=kc, t=t, b=b: e.matmul(ps[b][:], lhsT=wv[:, kc, :], rhs=xn[:, kc, TT(t)],
                                                                     start=(kc == 0), stop=(kc == 7)),
                          reads=[keyw, ("xn", kc, t)], writes=[("ps", b)])
                evac(t, b)

        def conv_branch(l):
            for blk in ("bigA", "bigB", "bigC", "scr"):
                P.fence(blk)
            glu = [bigB[:, 0:2, :].bitcast(F32), bigB[:, 2:4, :].bitcast(F32),
                   bigC[:, 0:2, :].bitcast(F32), bigC[:, 2:4, :].bitcast(F32)]
            glu = [g.rearrange("p a n -> p (a n)") for g in glu]
            gkey = [("bigB", "g0"), ("bigB", "g1"), ("bigC", "g2"), ("bigC", "g3")]
            sg = [scr[:, 0:512], scr[:, 512:1024]]
            si = 0
            for cc in range(4):
                keys, (wv, wg) = wload([wview(Win, l, 0, D, OFF_CONV + cc * 128, 128),
                                        wview(Win, l, 0, D, OFF_CONV + 512 + cc * 128, 128)])
                for t in range(4):
                    bv, bg = rr8(), rr8()
                    for kc in range(8):
                        P.add("pe", lambda e, kc=kc, t=t, bv=bv, wv=wv: e.matmul(
                            ps[bv][:], lhsT=wv[:, kc, :], rhs=xn[:, kc, TT(t)], start=(kc == 0), stop=(kc == 7)),
                            reads=[keys[0], ("xn", kc, t)], writes=[("ps", bv)])
                    for kc in range(8):
                        P.add("pe", lambda e, kc=kc, t=t, bg=bg, wg=wg: e.matmul(
                            ps[bg][:], lhsT=wg[:, kc, :], rhs=xn[:, kc, TT(t)], start=(kc == 0), stop=(kc == 7)),
                            reads=[keys[1], ("xn", kc, t)], writes=[("ps", bg)])
                    s_ = sg[si % 2]
                    sk = ("scr", "sg", si % 2)
                    si += 1
                    P.add("act", lambda e, s_=s_, bg=bg: e.activation(out=s_, in_=ps[bg][:], func=AF.Sigmoid),
                          reads=[("ps", bg)], writes=[sk])
                    P.add("dve", lambda e, s_=s_, bv=bv, cc=cc, t=t: e.tensor_tensor(
                        out=glu[cc][:, TT(t)], in0=s_, in1=ps[bv][:], op=ALU.mult),
                        reads=[sk, ("ps", bv)], writes=[gkey[cc]])
            dump("glu", glu[0], [gkey[0]])
            for cpair in range(2):
                P.fence("scr")
                accs = [scr[:, 0:2048], scr[:, 2048:4096]]
                tmp = scr[:, 4096:4608]
                tmpb = scr[:, 4608:5120].bitcast(BF16)
                xcb = scr[:, 5120:6144].bitcast(BF16).rearrange("p (k n) -> p k n", k=4)
                rstd = scr[:, 6144:6656]
                for ci in range(2):
                    cc = cpair * 2 + ci
                    P.add("dve", lambda e, ci=ci, cc=cc: e.tensor_scalar(
                        out=accs[ci], in0=glu[cc], scalar1=vcol(l, V_DW, 30 * 4 + cc), scalar2=vcol(l, V_DWB, cc),
                        op0=ALU.mult, op1=ALU.add),
                        reads=[gkey[cc], ("vecs",)], writes=[("scr", "acc", ci)])
                for tap in range(30):
                    dsh = 30 - tap
                    for ci in range(2):
                        cc = cpair * 2 + ci
                        P.add("dve", lambda e, ci=ci, cc=cc, tap=tap, dsh=dsh: e.scalar_tensor_tensor(
                            out=accs[ci][:, dsh:S], in0=glu[cc][:, 0:S - dsh], scalar=vcol(l, V_DW, tap * 4 + cc),
                            in1=accs[ci][:, dsh:S], op0=ALU.mult, op1=ALU.add),
                            reads=[gkey[cc], ("vecs",), ("scr", "acc", ci)], writes=[("scr", "acc", ci)])
                for ci in range(2):
                    cc = cpair * 2 + ci
                    P.add("act", lambda e, ci=ci, cc=cc: e.copy(out=glu[cc], in_=accs[ci]),
                          reads=[("scr", "acc", ci)], writes=[gkey[cc]])
            dump("convout", glu[0], [gkey[0]])
            P.fence("scr")
            xb = scr[:, 0:1024].bitcast(BF16).rearrange("p (k n) -> p k n", k=4)
            xc = scr[:, 1024:3072].rearrange("p (k n) -> p k n", k=4)
            rstd = scr[:, 3072:3584]
            yt = scr[:, 3584:4096]
            for t in range(4):
                for cc in range(4):
                    P.add("act", lambda e, cc=cc, t=t: e.copy(out=xb[:, cc, :], in_=glu[cc][:, TT(t)]),
                          reads=[gkey[cc]], writes=[("scr", "xb", cc)])
                bm = rr8()
                for cc in range(4):
                    P.add("pe", lambda e, cc=cc, bm=bm: e.matmul(ps[bm][:], lhsT=ones_c[:], rhs=xb[:, cc, :],
                                                                 start=(cc == 0), stop=(cc == 3)),
                          reads=[("scr", "xb", cc), ("ones",)], writes=[("ps", bm)])
                for cc in range(4):
                    P.add("dve", lambda e, cc=cc, t=t, bm=bm: e.tensor_tensor(
                        out=xc[:, cc, :], in0=glu[cc][:, TT(t)], in1=ps[bm][:], op=ALU.subtract),
                        reads=[gkey[cc], ("ps", bm)], writes=[("scr", "xc", cc)])
                for cc in range(4):
                    P.add("act", lambda e, cc=cc: e.activation(out=xb[:, cc, :], in_=xc[:, cc, :], func=AF.Square),
                          reads=[("scr", "xc", cc)], writes=[("scr", "xb", cc)])
                bv = rr8()
                for cc in range(4):
                    P.add("pe", lambda e, cc=cc, bv=bv: e.matmul(ps[bv][:], lhsT=ones_c[:], rhs=xb[:, cc, :],
                                                                 start=(cc == 0), stop=(cc == 3)),
                          reads=[("scr", "xb", cc), ("ones",)], writes=[("ps", bv)])
                P.add("act", lambda e, bv=bv: e.activation(out=rstd, in_=ps[bv][:], func=AF.Sqrt, bias=eps_c[:, 0:1]),
                      reads=[("ps", bv), ("ones",)], writes=[("scr", "rstd")])
                P.add("dve", lambda e: e.reciprocal(out=rstd, in_=rstd),
                      reads=[("scr", "rstd")], writes=[("scr", "rstd")])
                for cc in range(4):
                    P.add("dve", lambda e, cc=cc: e.scalar_tensor_tensor(
                        out=xc[:, cc, :], in0=xc[:, cc, :], scalar=vcol(l, V_LNG, cc), in1=rstd,
                        op0=ALU.mult, op1=ALU.mult),
                        reads=[("scr", "xc", cc), ("scr", "rstd"), ("vecs",)], writes=[("scr", "xc", cc)])
                    P.add("act", lambda e, cc=cc, t=t: e.activation(
                        out=bigA[:, cc, TT(t)], in_=xc[:, cc, :], func=AF.Silu, bias=vcol(l, V_LNB, cc)),
                        reads=[("scr", "xc", cc), ("vecs",)], writes=[("bigA", cc, t)])
            dump("c", bigA[:].rearrange("p a n -> p (a n)"), [("bigA", cc, t) for cc in range(4) for t in range(4)])
            P.fence("bigB")
            P.fence("bigC")

        def sb_attention(l):
            P.fence("scr")
            P.fence("bigB")
            qT = scr[:, 0:1024].bitcast(BF16)
            kT = scr[:, 1024:2048].bitcast(BF16)
            vv = scr[:, 2048:3072].bitcast(BF16).rearrange("p (t n) -> p t n", t=16)
            Eb = [scr[:, 3072:3584], scr[:, 3584:4096], scr[:, 6912:7424]]
            Gb = [scr[:, 4096:4608], scr[:, 4608:5120]]
            SPb = [scr[:, 5120:5376].bitcast(BF16), scr[:, 5376:5632].bitcast(BF16)]
            SSb = [scr[:, 5632:5888].bitcast(BF16), scr[:, 5888:6144].bitcast(BF16), scr[:, 6656:6912].bitcast(BF16)]
            Wb = [scr[:, 6144:6400].bitcast(BF16), scr[:, 6400:6656].bitcast(BF16)]
            cnt = {"e": 0, "ss": 0}
            for hp in range(4):
                keys, (wq, wk) = wload([wview(Win, l, 0, D, OFF_SBQ + hp * 128, 128),
                                        wview(Win, l, 0, D, OFF_SBK + hp * 128, 128)])
                keys2, (wv,) = wload([wview(Win, l, 0, D, OFF_SBV + hp * 128, 128)])
                keys = keys + keys2
                proj_fm(keys[0], wq, lambda t, b: P.add(
                    "act", lambda e, t=t, b=b: e.copy(out=qT[:, TT(t)], in_=ps[b][:]),
                    reads=[("ps", b)], writes=[("scr", "q", t)]))
                proj_fm(keys[1], wk, lambda t, b: P.add(
                    "dve", lambda e, t=t, b=b: e.tensor_copy(out=kT[:, TT(t)], in_=ps[b][:]),
                    reads=[("ps", b)], writes=[("scr", "k", t)]))
                for g in range(4):
                    b = rr8()
                    for ti in range(4):
                        t16 = g * 4 + ti
                        for kc in range(8):
                            P.add("pe", lambda e, kc=kc, t16=t16, ti=ti, b=b, wv=wv: e.matmul(
                                ps[b][:, ti * 128:(ti + 1) * 128], lhsT=xn[:, kc, t16 * 128:(t16 + 1) * 128],
                                rhs=wv[:, kc, :], start=(kc == 0), stop=(kc == 7)),
                                reads=[keys[2], ("xn", kc, t16 // 4)], writes=[("ps", b)])
                    P.add("act", lambda e, g=g, b=b: e.copy(
                        out=vv[:, g * 4:(g + 1) * 4, :], in_=ps[b][:].rearrange("p (t n) -> p t n", t=4)),
                        reads=[("ps", b)], writes=[("scr", "v", g)])
                pairs = []
                for j in range(2):
                    for qt in range(4):
                        nkt = (qt + 1) * 4
                        bo = rrO()
                        for idx, kt in enumerate(reversed(range(nkt))):
                            k0, q0 = kt * 128, qt * 512
                            diag = k0 >= q0
                            c0 = k0 - q0 if diag else 0
                            pairs.append(dict(j=j, qt=qt, kt=kt, k0=k0, q0=q0, diag=diag, c0=c0, first=(idx == 0),
                                              last=(kt == 0), bo=bo, pj=slice(64 * j, 64 * j + 64)))
                state = {"prev_ss": None}

                def stA(p, n):
                    i = n % 2
                    c0, q0, k0, pj, kt, qt = p["c0"], p["q0"], p["k0"], p["pj"], p["kt"], p["qt"]
                    cols = slice(c0, 512)
                    qcols = slice(q0 + c0, q0 + 512)
                    if p["first"]:
                        state["prev_ss"] = None
                    bs = rr6()
                    P.add("pe", lambda e: e.matmul(
                        ps[bs][:, cols], lhsT=kT[pj, k0:k0 + 128], rhs=qT[pj, qcols], start=True, stop=not p["diag"]),
                        reads=[("scr", "k", kt // 4), ("scr", "q", qt)], writes=[("ps", bs)])
                    if p["diag"]:
                        P.add("pe", lambda e: e.matmul(ps[bs][:, c0:c0 + 128], lhsT=IDN, rhs=NEGM, start=False, stop=True),
                              reads=[("cb",)], writes=[("ps", bs)])
                    ie = n % 3
                    E, SP = Eb[ie], SPb[i]
                    P.add("act", lambda e: e.activation(out=E[:, cols], in_=ps[bs][:, cols], func=AF.Exp, scale=0.125),
                          reads=[("ps", bs)], writes=[("scr", "E", ie)])
                    P.add("act", lambda e: e.activation(out=SP[:, cols], in_=E[:, cols], func=AF.Ln, bias=1.0),
                          reads=[("scr", "E", ie)], writes=[("scr", "SP", i)])
                    p["pss"] = state["prev_ss"]
                    if kt > 0:
                        si = n % 3
                        SSn = SSb[si]
                        nk = ("scr", "SS", si)
                        if state["prev_ss"] is None:
                            P.add("pool", lambda e: e.tensor_copy(out=SSn[:, cols], in_=SP[:, cols]),
                                  reads=[("scr", "SP", i)], writes=[nk])
                        else:
                            pss, pk, pc0 = state["prev_ss"]
                            if pc0 > c0:
                                P.add("pool", lambda e: e.tensor_copy(out=SSn[:, c0:pc0], in_=SP[:, c0:pc0]),
                                      reads=[("scr", "SP", i)], writes=[nk])
                            P.add("pool", lambda e: e.tensor_tensor(
                                out=SSn[:, pc0:512], in0=SP[:, pc0:512], in1=pss[:, pc0:512], op=ALU.add),
                                reads=[("scr", "SP", i), pk], writes=[nk])
                        state["prev_ss"] = (SSn, nk, c0)

                def stB(p, n):
                    i = n % 2
                    c0 = p["c0"]
                    cols = slice(c0, 512)
                    ie = n % 3
                    E, SP, G, Wt = Eb[ie], SPb[i], Gb[i], Wb[i]
                    bc = rr6()
                    pss = p["pss"]
                    P.add("pe", lambda e: e.matmul(ps[bc][:, cols], lhsT=TRI, rhs=SP[:, cols], start=True,
                                                   stop=(pss is None)),
                          reads=[("scr", "SP", i), ("cb",)], writes=[("ps", bc)])
                    if pss is not None:
                        ssb, pk, pc0 = pss
                        P.add("pe", lambda e: e.matmul(ps[bc][:, pc0:512], lhsT=ones1[:], rhs=ssb[:, pc0:512],
                                                       start=False, stop=True),
                              reads=[pk, ("ones",)], writes=[("ps", bc)])
                    dummies(NDUM["sb"])
                    P.add("act", lambda e: e.activation(out=G[:, cols], in_=ps[bc][:, cols], func=AF.Exp, scale=-1.0),
                          reads=[("ps", bc)], writes=[("scr", "G", i)])
                    P.add("dve", lambda e: e.tensor_tensor(out=Wt[:, cols], in0=E[:, cols], in1=G[:, cols], op=ALU.mult),
                          reads=[("scr", "E", ie), ("scr", "G", i)], writes=[("scr", "W", i)])

                def stC(p, n, hp=hp):
                    i = n % 2
                    c0, bo, pj, kt, qt = p["c0"], p["bo"], p["pj"], p["kt"], p["qt"]
                    cols = slice(c0, 512)
                    Wt = Wb[i]
                    if p["first"]:
                        P.add("pe", lambda e: e.matmul(ps[bo][0:64, :], lhsT=zer[:, 0:64], rhs=cb[:, 0:512],
                                                       start=True, stop=False),
                              reads=[("ones",), ("cb",)], writes=[("ps", bo)])
                    P.add("pe", lambda e: e.matmul(ps[bo][0:64, cols], lhsT=vv[:, kt, pj], rhs=Wt[:, cols],
                                                   start=False, stop=p["last"]),
                          reads=[("scr", "W", i), ("scr", "v", kt // 4)], writes=[("ps", bo)])
                    if p["last"]:
                        P.add("dve", lambda e: e.tensor_copy(out=bigB[pj, hp, TT(qt)], in_=ps[bo][0:64, :]),
                              reads=[("ps", bo)], writes=[("bigB", hp, qt, pj.start)])

                NPR = len(pairs)
                for n in range(NPR + 2):
                    if n < NPR:
                        stA(pairs[n], n)
                    if 1 <= n <= NPR:
                        stB(pairs[n - 1], n - 1)
                    if n >= 2:
                        stC(pairs[n - 2], n - 2)

        def moba_attention(l):
            P.fence("scr")
            P.fence("bigC")
            P.add("dve", lambda e: e.memset(vext, 1.0), writes=[("scr", "vext")])
            for j in range(2):
                P.add("pool", lambda e, j=j: e.dma_start(out=kaug[64:75, j, :], in_=kst_d[64:75, :]),
                      writes=[("scr", "kst", j)], dma="c1")
            ksf = scr[0:64, 0:16].rearrange("p (j n) -> p j n", j=2)
            gm = scr[:, 64:128].rearrange("p (g n) -> p g n", g=8)
            cmp_ = scr[:, 128:640].rearrange("p (g n m) -> p g n m", g=8, n=8)
            cntt = scr[:, 640:704].rearrange("p (g n) -> p g n", g=8)
            t1 = scr[:, 704:768].rearrange("p (g n) -> p g n", g=8)
            rden = scr[0:64, 768:1280]
            Pm = [scr[:, 1280:1536].bitcast(BF16), scr[:, 1536:1792].bitcast(BF16), scr[:, 1792:2048].bitcast(BF16)]
            cnt = {"p": 0}
            GM = cst[:, C_GM:C_GM + 128].rearrange("p (o j n) -> p o j n", o=8, j=2)
            LL = cst[:, C_L:C_L + 64].rearrange("p (o n) -> p o n", o=8)
            for hp in range(4):
                keys, (wq, wk) = wload([wview(Win, l, 0, D, OFF_MBQ + hp * 128, 128),
                                        wview(Win, l, 0, D, OFF_MBK + hp * 128, 128)])
                keys2, (wv,) = wload([wview(Win, l, 0, D, OFF_MBV + hp * 128, 128)])
                keys = keys + keys2

                def evq(t, b):
                    P.add("act", lambda e, t=t, b=b: e.copy(out=qaug[0:64, 0, TT(t)], in_=ps[b][0:64, :]),
                          reads=[("ps", b)], writes=[("scr", "qaug", 0, t)])
                    P.add("dve", lambda e, t=t, b=b: e.tensor_copy(out=qaug[0:64, 1, TT(t)], in_=ps[b][64:128, :]),
                          reads=[("ps", b)], writes=[("scr", "qaug", 1, t)])
                proj_fm(keys[0], wq, evq)

                def evk(t, b):
                    P.add("act", lambda e, t=t, b=b: e.copy(out=kaug[0:64, 0, TT(t)], in_=ps[b][0:64, :]),
                          reads=[("ps", b)], writes=[("scr", "kaug", 0, t)])
                    P.add("dve", lambda e, t=t, b=b: e.tensor_copy(out=kaug[0:64, 1, TT(t)], in_=ps[b][64:128, :]),
                          reads=[("ps", b)], writes=[("scr", "kaug", 1, t)])
                    for j in range(2):
                        P.add("dve", lambda e, t=t, b=b, j=j: e.tensor_reduce(
                            out=ksf[:, j, 2 * t:2 * t + 2], in_=ps[b][64 * j:64 * j + 64, :].rearrange("p (a n) -> p a n", a=2),
                            axis=AX.X, op=ALU.add),
                            reads=[("ps", b)], writes=[("scr", "ksf", j, t)])
                proj_fm(keys[1], wk, evk)
                P.add("dve", lambda e: e.tensor_copy(out=ksumb[:], in_=ksf),
                      reads=[("scr", "ksf", j, t) for j in range(2) for t in range(4)], writes=[("ksumb",)])
                for g in range(4):
                    b = rr8()
                    for ti in range(4):
                        t16 = g * 4 + ti
                        for kc in range(8):
                            P.add("pe", lambda e, kc=kc, t16=t16, ti=ti, b=b, wv=wv: e.matmul(
                                ps[b][:, ti * 128:(ti + 1) * 128], lhsT=xn[:, kc, t16 * 128:(t16 + 1) * 128],
                                rhs=wv[:, kc, :], start=(kc == 0), stop=(kc == 7)),
                                reads=[keys[2], ("xn", kc, t16 // 4)], writes=[("ps", b)])
                    P.add("act", lambda e, g=g, b=b: e.copy(
                        out=vext[:, g * 4:(g + 1) * 4, :, 0:64],
                        in_=ps[b][:].rearrange("p (t j n) -> p t j n", t=4, j=2)),
                        reads=[("ps", b), ("scr", "vext")], writes=[("scr", "vext", g)])
                for qt in range(4):
                    bg = rr8()
                    for ti in range(4):
                        t16 = qt * 4 + ti
                        for j in range(2):
                            g8 = ti * 2 + j
                            P.add("pe", lambda e, bg=bg, g8=g8, j=j, t16=t16: e.matmul(
                                ps[bg][:, g8 * 8:(g8 + 1) * 8], lhsT=qaug[0:64, j, t16 * 128:(t16 + 1) * 128],
                                rhs=ksumb[:, j, :], start=True, stop=True),
                                reads=[("scr", "qaug", j, qt), ("ksumb",)], writes=[("ps", bg)])
                    for ti in range(4):
                        own = (qt * 4 + ti) // 2
                        P.add("dve", lambda e, bg=bg, ti=ti, own=own: e.tensor_tensor(
                            out=gm[:, 2 * ti:2 * ti + 2, :],
                            in0=ps[bg][:, 16 * ti:16 * ti + 16].rearrange("p (j n) -> p j n", j=2),
                            in1=GM[:, own, :, :], op=ALU.add),
                            reads=[("ps", bg), ("cst",)], writes=[("scr", "gm")])
                    gap = [list(a) for a in gm.ap]
                    gm_m = bass.AP(gm.tensor, gm.offset, [gap[0], gap[1], [0, 8], gap[2]])
                    gm_n = bass.AP(gm.tensor, gm.offset, [gap[0], gap[1], gap[2], [0, 8]])
                    P.add("dve", lambda e, gm_m=gm_m, gm_n=gm_n: e.tensor_tensor(
                        out=cmp_, in0=gm_m, in1=gm_n, op=ALU.is_gt),
                        reads=[("scr", "gm")], writes=[("scr", "cmp")])
                    P.add("dve", lambda e: e.tensor_reduce(out=cntt, in_=cmp_, axis=AX.X, op=ALU.add),
                          reads=[("scr", "cmp")], writes=[("scr", "cnt")])
                    P.add("dve", lambda e: e.tensor_scalar(out=t1, in0=cntt, scalar1=2.5, scalar2=BIG,
                                                           op0=ALU.is_lt, op1=ALU.mult),
                          reads=[("scr", "cnt")], writes=[("scr", "t1")])
                    for ti in range(4):
                        own = (qt * 4 + ti) // 2
                        for j in range(2):
                            hh = 2 * hp + j
                            P.add("dve", lambda e, ti=ti, j=j, hh=hh, own=own: e.scalar_tensor_tensor(
                                out=selT[:, hh, ti, 64:72], in0=t1[:, 2 * ti + j, :], scalar=-BIG, in1=LL[:, own, :],
                                op0=ALU.add, op1=ALU.max),
                                reads=[("scr", "t1"), ("cst",), ("selst",)], writes=[("selT", hh, ti)])
                    for j in range(2):
                        hh = 2 * hp + j
                        bt = rr8()
                        for ti in range(4):
                            P.add("pe", lambda e, bt=bt, ti=ti, hh=hh: e.matmul(
                                ps[bt][0:80, ti * 128:(ti + 1) * 128], lhsT=selT[:, hh, ti, :], rhs=IDN,
                                start=True, stop=True),
                                reads=[("selT", hh, ti), ("cb",)], writes=[("ps", bt)])
                        P.add("act", lambda e, bt=bt, j=j, qt=qt: e.copy(out=qaug[64:75, j, TT(qt)], in_=ps[bt][64:75, :]),
                              reads=[("ps", bt)], writes=[("scr", "qst", j, qt)])
                pairs = []
                for j in range(2):
                    for qt in range(4):
                        nkt = (qt + 1) * 4
                        bo = rrO()
                        for kt in range(nkt):
                            k0, q0 = kt * 128, qt * 512
                            diag = k0 >= q0
                            c0 = k0 - q0 if diag else 0
                            pairs.append(dict(j=j, qt=qt, kt=kt, k0=k0, q0=q0, diag=diag, c0=c0, first=(kt == 0),
                                              last=(kt == nkt - 1), bo=bo, hh=2 * hp + j))

                def mA(p, n):
                    c0, q0, k0, j, kt, qt = p["c0"], p["q0"], p["k0"], p["j"], p["kt"], p["qt"]
                    cols = slice(c0, 512)
                    qcols = slice(q0 + c0, q0 + 512)
                    bs = rr6()
                    p["bs"] = bs
                    P.add("pe", lambda e: e.matmul(
                        ps[bs][:, cols], lhsT=kaug[0:75, j, k0:k0 + 128], rhs=qaug[0:75, j, qcols],
                        start=True, stop=not p["diag"]),
                        reads=[("scr", "kaug", j, kt // 4), ("scr", "kst", j), ("scr", "qaug", j, qt), ("scr", "qst", j, qt)],
                        writes=[("ps", bs)])
                    if p["diag"]:
                        P.add("pe", lambda e: e.matmul(ps[bs][:, c0:c0 + 128], lhsT=IDN, rhs=NEGM2, start=False, stop=True),
                              reads=[("cb",)], writes=[("ps", bs)])

                def mB(p, n):
                    i = n % 3
                    c0, bs = p["c0"], p["bs"]
                    cols = slice(c0, 512)
                    pm = Pm[i]
                    biasc = float(-SLOPES[p["hh"]] * (p["q0"] - p["k0"]))
                    P.add("act", lambda e: e.activation(
                        out=pm[:, cols], in_=ps[bs][:, cols], func=AF.Exp, scale=0.125, bias=biasc),
                        reads=[("ps", bs)], writes=[("scr", "Pm", i)])

                def mC(p, n, hp=hp):
                    i = n % 3
                    c0, bo, j, kt, qt = p["c0"], p["bo"], p["j"], p["kt"], p["qt"]
                    cols = slice(c0, 512)
                    pm = Pm[i]
                    pj = slice(64 * j, 64 * j + 64)
                    P.add("pe", lambda e: e.matmul(ps[bo][:, cols], lhsT=vext[:, kt, j, :], rhs=pm[:, cols],
                                                   start=p["first"], stop=p["last"]),
                          reads=[("scr", "Pm", i), ("scr", "vext", kt // 4), ("scr", "vext")], writes=[("ps", bo)])
                    if p["last"]:
                        P.add("dve", lambda e: e.reciprocal(out=rden, in_=ps[bo][64:128, :]),
                              reads=[("ps", bo)], writes=[("scr", "rden")])
                        P.add("dve", lambda e: e.tensor_tensor(
                            out=bigC[pj, hp, TT(qt)], in0=ps[bo][0:64, :], in1=rden, op=ALU.mult),
                            reads=[("ps", bo), ("scr", "rden")], writes=[("bigC", hp, qt, pj.start)])

                NPR = len(pairs)
                for n in range(NPR + 2):
                    if n < NPR:
                        mA(pairs[n], n)
                    if 1 <= n <= NPR:
                        mB(pairs[n - 1], n - 1)
                    if n >= 2:
                        mC(pairs[n - 2], n - 2)

        def mix_out(l, have):
            P.fence("scr")
            mixed = scr[:, 0:4096].bitcast(BF16).rearrange("p (k n) -> p k n", k=8)
            gtb = [scr[:, 4096 + 512 * i:4608 + 512 * i] for i in range(2)]
            ttb = [[scr[:, 5120 + 512 * (3 * a + i):5632 + 512 * (3 * a + i)] for i in range(3)] for a in range(2)]
            gcnt = {"i": 0}
            srcs = [("sb", bigB, WA, "bigB"), ("mb", bigC, WB, "bigC"), ("conv", bigA, WC, "bigA")]
            for half in range(2):
                for dc in range(8):
                    kg1, gw1 = wload([wview(Win, l, 0, D, OFF_GATE + br * D + dc * 128, 128) for br in range(2)])
                    kg2, gw2 = wload([wview(Win, l, 0, D, OFF_GATE + 2 * D + dc * 128, 128),
                                      wview(WA, l, 0, 512, dc * 128, 128), wview(WB, l, 0, 512, dc * 128, 128)])
                    kg3, gw3 = wload([wview(WC, l, 0, 512, dc * 128, 128)])
                    keysg, gw = kg1 + kg2[0:1], gw1 + gw2[0:1]
                    keysy, yw = kg2[1:3] + kg3, gw2[1:3] + gw3
                    for t2 in range(2):
                        t = half * 2 + t2
                        tt_ = ttb[t2]
                        for br, (nm, ob, W_, blk) in enumerate(srcs):
                            if nm not in have:
                                continue
                            bgate = rr8()
                            for kc in range(8):
                                P.add("pe", lambda e, kc=kc, t=t, bgate=bgate, gwb=gw[br]: e.matmul(
                                    ps[bgate][:], lhsT=gwb[:, kc, :], rhs=xn[:, kc, TT(t)],
                                    start=(kc == 0), stop=(kc == 7)),
                                    reads=[keysg[br], ("xn", kc, t)], writes=[("ps", bgate)])
                            by = rr8()
                            for kc in range(4):
                                if nm == "conv":
                                    rk = [("bigA", kc, t)]
                                else:
                                    rk = [(blk, kc, t, 0), (blk, kc, t, 64)]
                                P.add("pe", lambda e, kc=kc, t=t, by=by, ywb=yw[br], ob=ob: e.matmul(
                                    ps[by][:], lhsT=ywb[:, kc, :], rhs=ob[:, kc, TT(t)],
                                    start=(kc == 0), stop=(kc == 3)),
                                    reads=[keysy[br]] + rk, writes=[("ps", by)])
                            gi = gcnt["i"] % 2
                            gcnt["i"] += 1
                            P.add("act", lambda e, br=br, bgate=bgate, dc=dc, gi=gi: e.activation(
                                out=gtb[gi], in_=ps[bgate][:], func=AF.Sigmoid, bias=vcol(l, V_GB, br * 8 + dc)),
                                reads=[("ps", bgate), ("vecs",)], writes=[("scr", "gt", gi)])
                            P.add("dve", lambda e, br=br, by=by, gi=gi, tt_=tt_: e.tensor_tensor(
                                out=tt_[br], in0=gtb[gi], in1=ps[by][:], op=ALU.mult),
                                reads=[("scr", "gt", gi), ("ps", by)], writes=[("scr", "tt", t2, br)])
                        live = [br for br, s_ in enumerate(srcs) if s_[0] in have]
                        mo = mixed[:, dc, t2 * 512:(t2 + 1) * 512]
                        mk_ = ("scr", "mixed", dc, t2)
                        if len(live) == 1:
                            P.add("dve", lambda e, mo=mo, a=live[0], tt_=tt_: e.tensor_copy(out=mo, in_=tt_[a]),
                                  reads=[("scr", "tt", t2, live[0])], writes=[mk_])
                        elif len(live) == 2:
                            P.add("dve", lambda e, mo=mo, a=live[0], b_=live[1], tt_=tt_: e.tensor_tensor(
                                out=mo, in0=tt_[a], in1=tt_[b_], op=ALU.add),
                                reads=[("scr", "tt", t2, live[0]), ("scr", "tt", t2, live[1])], writes=[mk_])
                        else:
                            P.add("dve", lambda e, tt_=tt_: e.tensor_tensor(out=tt_[0], in0=tt_[0], in1=tt_[1], op=ALU.add),
                                  reads=[("scr", "tt", t2, 0), ("scr", "tt", t2, 1)], writes=[("scr", "tt", t2, 0)])
                            P.add("dve", lambda e, mo=mo, tt_=tt_: e.tensor_tensor(out=mo, in0=tt_[0], in1=tt_[2], op=ALU.add),
                                  reads=[("scr", "tt", t2, 0), ("scr", "tt", t2, 2)], writes=[mk_])
                dump("mixed", scr[:, 0:4096].bitcast(BF16), [("scr", "mixed", dc_, t_) for dc_ in range(8) for t_ in range(2)])
                for d0 in range(0, 8, 2):
                    keys, (wo,) = wload([wview(WO, l, 0, D, d0 * 128, 256)])
                    for di in range(2):
                        dc = d0 + di
                        for t2 in range(2):
                            t = half * 2 + t2
                            b = rr8()
                            for kc in range(8):
                                P.add("pe", lambda e, kc=kc, t2=t2, b=b, di=di, wo=wo: e.matmul(
                                    ps[b][:], lhsT=wo[:, kc, di * 128:(di + 1) * 128],
                                    rhs=mixed[:, kc, t2 * 512:(t2 + 1) * 512], start=(kc == 0), stop=(kc == 7)),
                                    reads=[keys[0], ("scr", "mixed", kc, t2)], writes=[("ps", b)])
                            P.add("dve", lambda e, b=b, dc=dc, t=t: e.tensor_tensor(
                                out=h[:, dc, TT(t)], in0=ps[b][:], in1=h[:, dc, TT(t)], op=ALU.add),
                                reads=[("ps", b)], writes=[("h", dc, t)])

        outs = []
        for s_i in range(nseq):
            for dc in range(8):
                P.add("sp", lambda e, dc=dc, s_i=s_i: e.dma_start(out=h[:, dc, :], in_=xT[s_i, dc * 128:(dc + 1) * 128, :]),
                      writes=[("h", dc, t) for t in range(4)], dma="x%d" % dc)
            for l in range(depth):
                if "ffn1" in stages:
                    ffn(l, W1i, W1o, V_FFN1N)
                if "mix" in stages:
                    rmsnorm(l, V_MIXN)
                    if "conv" in mix_parts:
                        conv_branch(l)
                    if "sb" in mix_parts:
                        sb_attention(l)
                    if "mb" in mix_parts:
                        moba_attention(l)
                    mix_out(l, mix_parts)
                if "ffn2" in stages:
                    ffn(l, W2i, W2o, V_FFN2N)
            if "final" in stages:
                rmsnorm(None, depth * NVEC_L, out_f32=h)
            for dc in range(8):
                outs.append(P.add("sp", lambda e, dc=dc, s_i=s_i: e.dma_start(
                    out=outT[s_i, dc * 128:(dc + 1) * 128, :], in_=h[:, dc, :]),
                    reads=[("h", dc, t) for t in range(4)], dma="o%d" % dc))
        P.emit(nc, final_waits=outs[-8:] + dbg_ops)
    return nc


_CACHE = {}


def kernel(**inputs):
    x = np.asarray(inputs["x"], np.float32)
    B = x.shape[0]
    per = B // NCORES
    cst, kst, sel = host_consts()
    vecs = host_vecs(inputs, DEPTH_FULL)
    nc = build(per, DEPTH_FULL)
    shared = {k: np.ascontiguousarray(np.asarray(inputs[k], np.float32)) for k in
              ("ffn1_w_in", "ffn1_w_out", "ffn2_w_in", "ffn2_w_out", "w_in", "sb_w_out", "mb_w_out",
               "conv_w_out", "w_o")}
    shared.update({"vecs": vecs, "cst": cst, "kst": kst, "selst": sel})
    in_maps = []
    for c in range(NCORES):
        xs = x[c * per:(c + 1) * per]
        m = dict(shared)
        m["xT"] = np.ascontiguousarray(xs.transpose(0, 2, 1))
        in_maps.append(m)
    res = run_bass_kernel_spmd(nc, in_maps, core_ids=list(range(NCORES)))
    out = np.empty((B, S, D), np.float32)
    for c in range(NCORES):
        out[c * per:(c + 1) * per] = res.results[c]["outT"].transpose(0, 2, 1)
    return out
```

```python
import contextlib
import numpy as np
import concourse.bass as bass
import concourse.mybir as mybir
from concourse.bass_utils import run_bass_kernel_spmd

F32 = mybir.dt.float32
BF16 = mybir.dt.bfloat16
AF = mybir.ActivationFunctionType
ALU = mybir.AluOpType
AX = mybir.AxisListType

D = 1024
S = 2048
DFF = 2816
NFC = 22
DEPTH_FULL = 2
NCORES = 8
SEQ_PER_CORE = 4
OFF_SBQ, OFF_SBK, OFF_SBV = 0, 512, 1024
OFF_MBQ, OFF_MBK, OFF_MBV = 1536, 2048, 2560
OFF_CONV = 3072
OFF_GATE = 4096
IN_COLS = 7168
EPS = 1e-6
BIG = 29952.0
SLOPES = [2.0 ** (-(h + 1)) for h in range(8)]
ENGS = ("pe", "act", "dve", "pool", "sp")


class Op:
    __slots__ = ("eng", "fn", "pos", "sig", "waits", "dma", "val")

    def __init__(self, eng, fn, dma=None):
        self.eng, self.fn, self.dma = eng, fn, dma
        self.pos, self.sig, self.waits, self.val = -1, False, [], 0


class Prog:
    def __init__(self):
        self.streams = {e: [] for e in ENGS}
        self.last_w, self.readers = {}, {}
        self.seen = {e: {} for e in ENGS}
        self.dma_cnt = {}
        self.fences = {}

    def _need(self, x, y, raw):
        if y is None or y is x:
            return
        if y.dma is not None:
            key = ("d", y.dma)
            val = self.dma_cnt[y.dma] - (16 if x.dma == y.dma else 0)
            if self.seen[x.eng].get(key, 0) >= val:
                return
            self.seen[x.eng][key] = val
            x.waits.append(("d", y.dma, val))
            return
        if y.eng == x.eng and x.dma is None:
            if x.eng == "pe" or not raw:
                return
            if len(self.streams[x.eng]) - y.pos > 3:
                return
        key = ("e", y.eng)
        if self.seen[x.eng].get(key, -1) >= y.pos:
            return
        self.seen[x.eng][key] = y.pos
        y.sig = True
        x.waits.append(("e", y.eng, y))

    def fence(self, block):
        ops = {}
        for k in [k for k in self.last_w if k[0] == block]:
            o = self.last_w.pop(k)
            ops[id(o)] = o
        for k in [k for k in self.readers if k[0] == block]:
            for o in self.readers.pop(k):
                ops[id(o)] = o
        best = {}
        for o in ops.values():
            kk = (o.eng, o.dma)
            rank = o.val if o.dma is not None else o.pos
            if kk not in best or rank > best[kk][0]:
                best[kk] = (rank, o)
        if best:
            self.fences[block] = [v[1] for v in best.values()]

    def add(self, eng, fn, reads=(), writes=(), dma=None):
        x = Op(eng, fn, dma)
        if dma is not None:
            self.dma_cnt[dma] = self.dma_cnt.get(dma, 0) + 16
            x.val = self.dma_cnt[dma]
        reads = list(reads)
        writes = list(writes)
        for r in list(reads):
            if r[0] == "ps":
                reads.remove(r)
                writes.append(r)
        for k in reads + writes:
            if k not in self.last_w and k[0] in self.fences:
                for o in self.fences[k[0]]:
                    self._need(x, o, True)
        for r in reads:
            self._need(x, self.last_w.get(r), True)
        for w in writes:
            self._need(x, self.last_w.get(w), True)
            for rd in self.readers.get(w, ()):
                self._need(x, rd, False)
        x.pos = len(self.streams[eng])
        self.streams[eng].append(x)
        for r in reads:
            self.readers.setdefault(r, []).append(x)
        for w in writes:
            self.last_w[w] = x
            self.readers[w] = []
        return x

    def emit(self, nc, final_waits=()):
        with contextlib.ExitStack() as es:
            esem = {e: es.enter_context(nc.semaphore("s_" + e)) for e in ENGS}
            dsem = {n: es.enter_context(nc.semaphore("d_" + n)) for n in self.dma_cnt}
            block = es.enter_context(nc.Block())
            for e in ENGS:
                c = 0
                for op in self.streams[e]:
                    if op.dma is None:
                        if op.sig:
                            c += 1
                        op.val = c
            hooks = {"pe": block.tensor, "act": block.scalar, "dve": block.vector,
                     "pool": block.gpsimd, "sp": block.sync}

            def mk(e):
                def body(eng):
                    for op in self.streams[e]:
                        for w in op.waits:
                            if w[0] == "d":
                                eng.wait_ge(dsem[w[1]], w[2])
                            else:
                                eng.wait_ge(esem[w[1]], w[2].val)
                        ins = op.fn(eng)
                        if op.dma is not None:
                            ins.then_inc(dsem[op.dma], 16)
                        elif op.sig:
                            ins.then_inc(esem[e], 1)
                    if e == "sp":
                        for op in final_waits:
                            eng.wait_ge(dsem[op.dma], op.val)
                return body
            for e in ENGS:
                hooks[e](mk(e))


NVEC_L = 8 + 8 + 8 + 24 + 4 + 4 + 4 + 124
V_FFN1N, V_MIXN, V_FFN2N, V_GB, V_DWB, V_LNG, V_LNB, V_DW = 0, 8, 16, 24, 48, 52, 56, 60
C_TRI, C_MLT, C_MLE, C_ID, C_GM, C_L = 0, 128, 256, 384, 512, 640
NCST = 704


def host_consts():
    c = np.zeros((128, NCST), np.float32)
    j = np.arange(128)[:, None]
    s = np.arange(128)[None, :]
    c[:, C_TRI:C_TRI + 128] = (j >= s)
    c[:, C_MLT:C_MLT + 128] = np.where(j >= s, -BIG, 0.0)
    c[:, C_MLE:C_MLE + 128] = np.where(j > s, -BIG, 0.0)
    c[:, C_ID:C_ID + 128] = (j == s)
    own = np.arange(8)[:, None]
    n = np.arange(8)[None, :]
    gm = np.where(n < own, 0.0, -BIG).astype(np.float32)
    ll = np.where(n < own, -BIG, 0.0).astype(np.float32)
    c[:, C_GM:C_GM + 128] = np.repeat(gm[:, None, :], 2, axis=1).reshape(1, 128)
    c[:, C_L:C_L + 64] = ll.reshape(1, 64)
    kst = np.zeros((128, S), np.float32)
    pos = np.arange(S)
    for nn in range(8):
        kst[64 + nn] = (pos // 256 == nn)
    kst[72] = 1.0
    kst[73] = 1.0
    kst[74] = pos % 128
    sel = np.zeros((128, 8, 4, 80), np.float32)
    q = np.arange(128)
    for h in range(8):
        for m in range(4):
            i = m * 128 + q
            sel[:, h, m, 72] = -8.0 * SLOPES[h] * (256 * (i // 256))
            sel[:, h, m, 73] = -8.0 * SLOPES[h] * (i % 256)
            sel[:, h, m, 74] = 8.0 * SLOPES[h]
    return c, kst, sel.reshape(128, 8 * 4 * 80)


def host_vecs(inp, depth):
    def col(v):
        return np.ascontiguousarray(np.asarray(v, np.float32).reshape(-1, 128).T)
    cols = []
    for l in range(depth):
        cols += [col(inp["ffn1_norm"][l]), col(inp["mix_norm"][l]), col(inp["ffn2_norm"][l]),
                 col(inp["gate_bias"][l]), col(inp["conv_dw_bias"][l]), col(inp["conv_ln_g"][l]),
                 col(inp["conv_ln_b"][l])]
        dw = np.asarray(inp["conv_dw"][l], np.float32).reshape(31, 512)
        cols.append(np.ascontiguousarray(dw.reshape(31, 4, 128).transpose(2, 0, 1).reshape(128, 124)))
    cols.append(col(inp["final_norm"]))
    return np.ascontiguousarray(np.concatenate(cols, axis=1))


def build(nseq=SEQ_PER_CORE, depth=DEPTH_FULL, stages=("ffn1", "mix", "ffn2", "final"),
          mix_parts=("conv", "sb", "mb"), dbg=None):
    nc = bass.Bass("TRN2", target_bir_lowering=False)
    dram = {}

    def din(name, shape):
        dram[name] = nc.dram_tensor(name, list(shape), F32, kind="ExternalInput").ap()
        return dram[name]

    xT = din("xT", [nseq, D, S])
    W1i = din("ffn1_w_in", [depth, D, 2 * DFF])
    W1o = din("ffn1_w_out", [depth, DFF, D])
    W2i = din("ffn2_w_in", [depth, D, 2 * DFF])
    W2o = din("ffn2_w_out", [depth, DFF, D])
    Win = din("w_in", [depth, D, IN_COLS])
    WA = din("sb_w_out", [depth, 512, D])
    WB = din("mb_w_out", [depth, 512, D])
    WC = din("conv_w_out", [depth, 512, D])
    WO = din("w_o", [depth, D, D])
    NV = depth * NVEC_L + 8
    vecs_d = din("vecs", [128, NV])
    cst_d = din("cst", [128, NCST])
    kst_d = din("kst", [128, S])
    sel_d = din("selst", [128, 8 * 4 * 80])
    outT = nc.dram_tensor("outT", [nseq, D, S], F32, kind="ExternalOutput").ap()
    dbg_d = nc.dram_tensor("dbg", [128, 8192], F32, kind="ExternalOutput").ap() if dbg else None
    dbg_ops = []

    def dump(name, ap, keys):
        if dbg == name and not dbg_ops:
            n = ap.shape[1]
            dbg_ops.append(P.add("pool", lambda e: e.dma_start(out=dbg_d[:, 0:n], in_=ap), reads=keys, dma="dbg"))

    P = Prog()
    es = contextlib.ExitStack()
    with es:
        def sb(name, shape, dt):
            return es.enter_context(nc.sbuf_tensor(name, list(shape), dt))

        h = sb("h", [128, 8, S], F32)
        xn = sb("xn", [128, 8, S], BF16)
        bigA = sb("bigA", [128, 4, S], BF16)
        bigB = sb("bigB", [128, 4, S], BF16)
        bigC = sb("bigC", [128, 4, S], BF16)
        scr = sb("scr", [128, 8192], F32)
        NW = 4
        WSZ = 2048
        wsl = [sb("w%d" % i, [128, WSZ], BF16) for i in range(NW)]
        vecs = sb("vecs_sb", [128, NV], F32)
        cst = sb("cst_sb", [128, NCST], F32)
        cb = sb("cst_bf", [128, 512], BF16)
        ones_d = sb("ones_d", [128, 128], BF16)
        ones_c = sb("ones_c", [128, 128], BF16)
        ones1 = sb("ones1", [128, 128], BF16)
        zer = sb("zer", [128, 128], BF16)
        eps_c = sb("eps_c", [128, 1], F32)
        kaug = scr[:, 2048:4096].bitcast(BF16).rearrange("p (j n) -> p j n", j=2)
        qaug = scr[:, 4096:6144].bitcast(BF16).rearrange("p (j n) -> p j n", j=2)
        vext = scr[:, 6144:8192].bitcast(BF16).rearrange("p (t j n) -> p t j n", t=16, j=2)
        selT = sb("selT", [128, 8, 4, 80], BF16)
        ksumb = sb("ksumb", [64, 2, 8], BF16)
        ps = [es.enter_context(nc.psum_tensor("ps%d" % i, [128, 512], F32)) for i in range(8)]

        class RR:
            def __init__(self, ids):
                self.ids, self.i = list(ids), 0

            def __call__(self):
                b = self.ids[self.i % len(self.ids)]
                self.i += 1
                return b
        rr8 = RR(range(8))
        rr6 = RR(range(5))
        NDUM = {"sb": 3}

        def dummies(n):
            for _ in range(n):
                P.add("pe", lambda e: e.matmul(ps[5][:], lhsT=zer[:], rhs=cb[:, 0:512], start=True, stop=True),
                      reads=[("ones",), ("cb",)], writes=[("ps", 5)])
        rrO = RR([6, 7])
        wstate = {"i": 0}

        def wload(parts, eng="pool"):
            si = wstate["i"] % NW
            wstate["i"] += 1
            P.fence("w%d" % si)
            off = 0
            views, keys = [], []
            for pi, ap in enumerate(parts):
                K, n = ap.shape[1], ap.shape[2]
                v = wsl[si][:, off:off + K * n].rearrange("p (k n) -> p k n", k=K)
                key = ("w%d" % si, pi)
                P.add(eng, lambda e, v=v, ap=ap: e.dma_start(out=v, in_=ap), writes=[key], dma="w%d" % si)
                views.append(v)
                keys.append(key)
                off += K * n
            assert off <= WSZ
            return keys, views

        def wview(Wd, l, r0, nrow, c0, ncol):
            return Wd[l, r0:r0 + nrow, c0:c0 + ncol].rearrange("(k p) n -> p k n", p=128)

        def vcol(l, base, i):
            c = l * NVEC_L + base + i
            return vecs[:, c:c + 1]

        def TT(t):
            return slice(t * 512, (t + 1) * 512)

        P.add("sp", lambda e: e.dma_start(out=vecs[:], in_=vecs_d), writes=[("vecs",)], dma="c0")
        P.add("sp", lambda e: e.dma_start(out=cst[:], in_=cst_d), writes=[("cst",)], dma="c0")
        P.add("dve", lambda e: e.tensor_copy(out=cb[:], in_=cst[:, 0:512]), reads=[("cst",)], writes=[("cb",)])
        P.add("dve", lambda e: e.memset(ones_d[:], 1.0 / 1024), writes=[("ones",)])
        P.add("dve", lambda e: e.memset(ones_c[:], 1.0 / 512), writes=[("ones",)])
        P.add("dve", lambda e: e.memset(ones1[:], 1.0), writes=[("ones",)])
        P.add("dve", lambda e: e.memset(zer[:], 0.0), writes=[("ones",)])
        P.add("dve", lambda e: e.memset(eps_c[:], EPS), writes=[("ones",)])
        P.add("pool", lambda e: e.dma_start(out=selT[:].rearrange("p a b c -> p (a b c)"), in_=sel_d),
              writes=[("selst",)], dma="c1")
        TRI = cb[:, C_TRI:C_TRI + 128]
        NEGM2 = cb[:, C_MLE:C_MLE + 128]
        NEGM = cb[:, C_MLT:C_MLT + 128]
        IDN = cb[:, C_ID:C_ID + 128]
        MLT = cst[:, C_MLT:C_MLT + 128]

        def rmsnorm(l, base, out_bf=True, out_f32=None):
            P.fence("scr")
            sqbs = [scr[:, 0:2048].bitcast(BF16).rearrange("p (k n) -> p k n", k=8),
                    scr[:, 2048:4096].bitcast(BF16).rearrange("p (k n) -> p k n", k=8)]
            rstds = [scr[:, 4096:4608], scr[:, 4608:5120]]
            banks = {}

            def sqs(t):
                sqb = sqbs[t % 2]
                for dc in range(8):
                    P.add("act", lambda e, dc=dc: e.activation(out=sqb[:, dc, :], in_=h[:, dc, TT(t)], func=AF.Square),
                          reads=[("h", dc, t)], writes=[("scr", "sq", t % 2, dc)])
                bnk = rr8()
                banks[t] = bnk
                for dc in range(8):
                    P.add("pe", lambda e, dc=dc: e.matmul(ps[bnk][:], lhsT=ones_d[:], rhs=sqb[:, dc, :],
                                                          start=(dc == 0), stop=(dc == 7)),
                          reads=[("scr", "sq", t % 2, dc), ("ones",)], writes=[("ps", bnk)])

            def fin(t):
                bnk = banks[t]
                rstd = rstds[t % 2]
                rk = ("scr", "rstd", t % 2)
                P.add("act", lambda e: e.activation(out=rstd, in_=ps[bnk][:], func=AF.Sqrt, bias=eps_c[:, 0:1]),
                      reads=[("ps", bnk), ("ones",)], writes=[rk])
                P.add("dve", lambda e: e.reciprocal(out=rstd, in_=rstd), reads=[rk], writes=[rk])
                for dc in range(8):
                    if out_f32 is None:
                        o = xn[:, dc, TT(t)]
                        wk = ("xn", dc, t)
                    else:
                        o = out_f32[:, dc, TT(t)]
                        wk = ("h", dc, t)
                    c = base + dc if l is None else l * NVEC_L + base + dc
                    P.add("dve", lambda e, o=o, dc=dc, c=c: e.scalar_tensor_tensor(
                        out=o, in0=h[:, dc, TT(t)], scalar=vecs[:, c:c + 1], in1=rstd, op0=ALU.mult, op1=ALU.mult),
                        reads=[("h", dc, t), rk, ("vecs",)], writes=[wk])

            sqs(0)
            for t in range(4):
                if t + 1 < 4:
                    sqs(t + 1)
                fin(t)

        def ffn(l, Wi, Wo, nbase):
            rmsnorm(l, nbase)
            P.fence("scr")
            P.fence("bigA")
            P.fence("bigB")
            sil = [scr[:, 5120:5632], scr[:, 5632:6144]]
            groups = [(0, 8), (8, 7), (15, 7)]
            sidx = 0
            for (g0, gn) in groups:
                def hid(c, t):
                    return (bigA if c < 4 else bigB)[:, c % 4, TT(t)]
                for c0 in range(0, gn, 1):
                    ncnk = 1
                    fc = g0 + c0
                    keys, (wa, wb) = wload([wview(Wi, l, 0, D, fc * 128, ncnk * 128),
                                            wview(Wi, l, 0, D, DFF + fc * 128, ncnk * 128)])
                    for ci in range(ncnk):
                        c = c0 + ci
                        for t in range(4):
                            ba, bb = rr8(), rr8()
                            for kc in range(8):
                                P.add("pe", lambda e, kc=kc, t=t, ba=ba, ci=ci, wa=wa: e.matmul(
                                    ps[ba][:], lhsT=wa[:, kc, ci * 128:(ci + 1) * 128], rhs=xn[:, kc, TT(t)],
                                    start=(kc == 0), stop=(kc == 7)),
                                    reads=[keys[0], ("xn", kc, t)], writes=[("ps", ba)])
                            for kc in range(8):
                                P.add("pe", lambda e, kc=kc, t=t, bb=bb, ci=ci, wb=wb: e.matmul(
                                    ps[bb][:], lhsT=wb[:, kc, ci * 128:(ci + 1) * 128], rhs=xn[:, kc, TT(t)],
                                    start=(kc == 0), stop=(kc == 7)),
                                    reads=[keys[1], ("xn", kc, t)], writes=[("ps", bb)])
                            st = sil[sidx % 2]
                            sk = ("scr", "sil", sidx % 2)
                            sidx += 1
                            P.add("act", lambda e, st=st, ba=ba: e.activation(out=st, in_=ps[ba][:], func=AF.Silu),
                                  reads=[("ps", ba)], writes=[sk])
                            P.add("dve", lambda e, st=st, bb=bb, c=c, t=t: e.tensor_tensor(
                                out=hid(c, t), in0=st, in1=ps[bb][:], op=ALU.mult),
                                reads=[sk, ("ps", bb)], writes=[("bigA" if c < 4 else "bigB", c % 4, t)])
                for d0 in range(0, 8, 2):
                    keys, (wo,) = wload([wview(Wo, l, g0 * 128, gn * 128, d0 * 128, 256)])
                    for di in range(2):
                        dc = d0 + di
                        for t in range(4):
                            b = rr8()
                            for c in range(gn):
                                P.add("pe", lambda e, c=c, t=t, b=b, di=di, wo=wo, gn=gn: e.matmul(
                                    ps[b][:], lhsT=wo[:, c, di * 128:(di + 1) * 128], rhs=hid(c, t),
                                    start=(c == 0), stop=(c == gn - 1)),
                                    reads=[keys[0], ("bigA" if c < 4 else "bigB", c % 4, t)], writes=[("ps", b)])
                            P.add("dve", lambda e, b=b, dc=dc, t=t: e.scalar_tensor_tensor(
                                out=h[:, dc, TT(t)], in0=ps[b][:], scalar=0.5, in1=h[:, dc, TT(t)],
                                op0=ALU.mult, op1=ALU.add),
                                reads=[("ps", b)], writes=[("h", dc, t)])

        def proj_fm(keyw, wv, evac):
            for t in range(4):
                b = rr8()
                for kc in range(8):
                    P.add("pe", lambda e, kc=kc, t=t, b=b: e.matmul(ps[b][:], lhsT=wv[:, kc, :], rhs=xn[:, kc, TT(t)],
                                                                     start=(kc == 0), stop=(kc == 7)),
                          reads=[keyw, ("xn", kc, t)], writes=[("ps", b)])
                evac(t, b)

        def conv_branch(l):
            for blk in ("bigA", "bigB", "bigC", "scr"):
                P.fence(blk)
            gpad = [bigB[:, 0:2, :].rearrange("p a n -> p (a n)"), bigB[:, 2:4, :].rearrange("p a n -> p (a n)"),
                    bigC[:, 0:2, :].rearrange("p a n -> p (a n)"), bigC[:, 2:4, :].rearrange("p a n -> p (a n)")]
            gkey = [("bigB", "g0"), ("bigB", "g1"), ("bigC", "g2"), ("bigC", "g3")]
            sg = [scr[:, 0:512], scr[:, 512:1024]]
            si = 0
            for cc in range(4):
                P.add("dve", lambda e, cc=cc: e.memset(gpad[cc][:, 0:30], 0.0), writes=[gkey[cc]])
                keys, (wv, wg) = wload([wview(Win, l, 0, D, OFF_CONV + cc * 128, 128),
                                        wview(Win, l, 0, D, OFF_CONV + 512 + cc * 128, 128)])
                for t in range(4):
                    bv, bg = rr8(), rr8()
                    for kc in range(8):
                        P.add("pe", lambda e, kc=kc, t=t, bv=bv, wv=wv: e.matmul(
                            ps[bv][:], lhsT=wv[:, kc, :], rhs=xn[:, kc, TT(t)], start=(kc == 0), stop=(kc == 7)),
                            reads=[keys[0], ("xn", kc, t)], writes=[("ps", bv)])
                    for kc in range(8):
                        P.add("pe", lambda e, kc=kc, t=t, bg=bg, wg=wg: e.matmul(
                            ps[bg][:], lhsT=wg[:, kc, :], rhs=xn[:, kc, TT(t)], start=(kc == 0), stop=(kc == 7)),
                            reads=[keys[1], ("xn", kc, t)], writes=[("ps", bg)])
                    s_ = sg[si % 2]
                    sk = ("scr", "sg", si % 2)
                    si += 1
                    P.add("act", lambda e, s_=s_, bg=bg: e.activation(out=s_, in_=ps[bg][:], func=AF.Sigmoid),
                          reads=[("ps", bg)], writes=[sk])
                    P.add("dve", lambda e, s_=s_, bv=bv, cc=cc, t=t: e.tensor_tensor(
                        out=gpad[cc][:, 30 + t * 512:30 + (t + 1) * 512], in0=s_, in1=ps[bv][:], op=ALU.mult),
                        reads=[sk, ("ps", bv)], writes=[gkey[cc]])
            P.fence("scr")
            Dgs = [scr[:, 0:1984].bitcast(BF16).rearrange("p (k n) -> p k n", k=31),
                   scr[:, 1984:3968].bitcast(BF16).rearrange("p (k n) -> p k n", k=31)]
            for cc in range(4):
                Dg = Dgs[cc % 2]
                for tap in range(31):
                    P.add("dve", lambda e, Dg=Dg, tap=tap, cc=cc: e.tensor_scalar(
                        out=Dg[:, tap, :], in0=IDN, scalar1=vcol(l, V_DW, tap * 4 + cc), scalar2=None, op0=ALU.mult),
                        reads=[("cb",), ("vecs",)], writes=[("scr", "Dg", cc % 2, tap)])
                for t in range(4):
                    bk = rr8()
                    for tap in range(31):
                        P.add("pe", lambda e, Dg=Dg, tap=tap, cc=cc, t=t, bk=bk: e.matmul(
                            ps[bk][:], lhsT=Dg[:, tap, :], rhs=gpad[cc][:, t * 512 + tap:t * 512 + tap + 512],
                            start=(tap == 0), stop=(tap == 30)),
                            reads=[("scr", "Dg", cc % 2, tap), gkey[cc]], writes=[("ps", bk)])
                    P.add("act", lambda e, cc=cc, t=t, bk=bk: e.activation(
                        out=bigA[:, cc, TT(t)], in_=ps[bk][:], func=AF.Identity, bias=vcol(l, V_DWB, cc)),
                        reads=[("ps", bk), ("vecs",)], writes=[("bigA", cc, t)])
            P.fence("scr")
            sqb = scr[:, 0:1024].bitcast(BF16).rearrange("p (k n) -> p k n", k=4)
            xc = scr[:, 1024:3072].rearrange("p (k n) -> p k n", k=4)
            rstd = scr[:, 3072:3584]
            for t in range(4):
                bm = rr8()
                for cc in range(4):
                    P.add("pe", lambda e, cc=cc, bm=bm, t=t: e.matmul(ps[bm][:], lhsT=ones_c[:], rhs=bigA[:, cc, TT(t)],
                                                                      start=(cc == 0), stop=(cc == 3)),
                          reads=[("bigA", cc, t), ("ones",)], writes=[("ps", bm)])
                for cc in range(4):
                    P.add("dve", lambda e, cc=cc, t=t, bm=bm: e.tensor_tensor(
                        out=xc[:, cc, :], in0=bigA[:, cc, TT(t)], in1=ps[bm][:], op=ALU.subtract),
                        reads=[("bigA", cc, t), ("ps", bm)], writes=[("scr", "xc", cc)])
                for cc in range(4):
                    P.add("act", lambda e, cc=cc: e.activation(out=sqb[:, cc, :], in_=xc[:, cc, :], func=AF.Square),
                          reads=[("scr", "xc", cc)], writes=[("scr", "xb", cc)])
                bv = rr8()
                for cc in range(4):
                    P.add("pe", lambda e, cc=cc, bv=bv: e.matmul(ps[bv][:], lhsT=ones_c[:], rhs=sqb[:, cc, :],
                                                                 start=(cc == 0), stop=(cc == 3)),
                          reads=[("scr", "xb", cc), ("ones",)], writes=[("ps", bv)])
                P.add("act", lambda e, bv=bv: e.activation(out=rstd, in_=ps[bv][:], func=AF.Sqrt, bias=eps_c[:, 0:1]),
                      reads=[("ps", bv), ("ones",)], writes=[("scr", "rstd")])
                P.add("dve", lambda e: e.reciprocal(out=rstd, in_=rstd),
                      reads=[("scr", "rstd")], writes=[("scr", "rstd")])
                for cc in range(4):
                    P.add("dve", lambda e, cc=cc: e.scalar_tensor_tensor(
                        out=xc[:, cc, :], in0=xc[:, cc, :], scalar=vcol(l, V_LNG, cc), in1=rstd,
                        op0=ALU.mult, op1=ALU.mult),
                        reads=[("scr", "xc", cc), ("scr", "rstd"), ("vecs",)], writes=[("scr", "xc", cc)])
                    P.add("act", lambda e, cc=cc, t=t: e.activation(
                        out=bigA[:, cc, TT(t)], in_=xc[:, cc, :], func=AF.Silu, bias=vcol(l, V_LNB, cc)),
                        reads=[("scr", "xc", cc), ("vecs",)], writes=[("bigA", cc, t)])
            dump("c", bigA[:].rearrange("p a n -> p (a n)"), [("bigA", cc, t) for cc in range(4) for t in range(4)])
            P.fence("bigB")
            P.fence("bigC")

        def sb_attention(l):
            P.fence("scr")
            P.fence("bigB")
            qT = scr[:, 0:1024].bitcast(BF16)
            kT = scr[:, 1024:2048].bitcast(BF16)
            vv = scr[:, 2048:3072].bitcast(BF16).rearrange("p (t n) -> p t n", t=16)
            Eb = [scr[:, 3072:3584], scr[:, 3584:4096], scr[:, 6912:7424]]
            Gb = [scr[:, 4096:4608], scr[:, 4608:5120]]
            SPb = [scr[:, 5120:5376].bitcast(BF16), scr[:, 5376:5632].bitcast(BF16)]
            SSb = [scr[:, 5632:5888].bitcast(BF16), scr[:, 5888:6144].bitcast(BF16), scr[:, 6656:6912].bitcast(BF16)]
            Wb = [scr[:, 6144:6400].bitcast(BF16), scr[:, 6400:6656].bitcast(BF16)]
            cnt = {"e": 0, "ss": 0}
            for hp in range(4):
                keys, (wq, wk) = wload([wview(Win, l, 0, D, OFF_SBQ + hp * 128, 128),
                                        wview(Win, l, 0, D, OFF_SBK + hp * 128, 128)])
                keys2, (wv,) = wload([wview(Win, l, 0, D, OFF_SBV + hp * 128, 128)])
                keys = keys + keys2
                proj_fm(keys[0], wq, lambda t, b: P.add(
                    "act", lambda e, t=t, b=b: e.copy(out=qT[:, TT(t)], in_=ps[b][:]),
                    reads=[("ps", b)], writes=[("scr", "q", t)]))
                proj_fm(keys[1], wk, lambda t, b: P.add(
                    "dve", lambda e, t=t, b=b: e.tensor_copy(out=kT[:, TT(t)], in_=ps[b][:]),
                    reads=[("ps", b)], writes=[("scr", "k", t)]))
                for g in range(4):
                    b = rr8()
                    for ti in range(4):
                        t16 = g * 4 + ti
                        for kc in range(8):
                            P.add("pe", lambda e, kc=kc, t16=t16, ti=ti, b=b, wv=wv: e.matmul(
                                ps[b][:, ti * 128:(ti + 1) * 128], lhsT=xn[:, kc, t16 * 128:(t16 + 1) * 128],
                                rhs=wv[:, kc, :], start=(kc == 0), stop=(kc == 7)),
                                reads=[keys[2], ("xn", kc, t16 // 4)], writes=[("ps", b)])
                    P.add("act", lambda e, g=g, b=b: e.copy(
                        out=vv[:, g * 4:(g + 1) * 4, :], in_=ps[b][:].rearrange("p (t n) -> p t n", t=4)),
                        reads=[("ps", b)], writes=[("scr", "v", g)])
                pairs = []
                for j in range(2):
                    for qt in range(4):
                        nkt = (qt + 1) * 4
                        bo = rrO()
                        for idx, kt in enumerate(reversed(range(nkt))):
                            k0, q0 = kt * 128, qt * 512
                            diag = k0 >= q0
                            c0 = k0 - q0 if diag else 0
                            pairs.append(dict(j=j, qt=qt, kt=kt, k0=k0, q0=q0, diag=diag, c0=c0, first=(idx == 0),
                                              last=(kt == 0), bo=bo, pj=slice(64 * j, 64 * j + 64)))
                state = {"prev_ss": None}

                def stA(p, n):
                    i = n % 2
                    c0, q0, k0, pj, kt, qt = p["c0"], p["q0"], p["k0"], p["pj"], p["kt"], p["qt"]
                    cols = slice(c0, 512)
                    qcols = slice(q0 + c0, q0 + 512)
                    if p["first"]:
                        state["prev_ss"] = None
                    bs = rr6()
                    P.add("pe", lambda e: e.matmul(
                        ps[bs][:, cols], lhsT=kT[pj, k0:k0 + 128], rhs=qT[pj, qcols], start=True, stop=not p["diag"]),
                        reads=[("scr", "k", kt // 4), ("scr", "q", qt)], writes=[("ps", bs)])
                    if p["diag"]:
                        P.add("pe", lambda e: e.matmul(ps[bs][:, c0:c0 + 128], lhsT=IDN, rhs=NEGM, start=False, stop=True),
                              reads=[("cb",)], writes=[("ps", bs)])
                    ie = n % 3
                    E, SP = Eb[ie], SPb[i]
                    P.add("act", lambda e: e.activation(out=E[:, cols], in_=ps[bs][:, cols], func=AF.Exp, scale=0.125),
                          reads=[("ps", bs)], writes=[("scr", "E", ie)])
                    P.add("act", lambda e: e.activation(out=SP[:, cols], in_=E[:, cols], func=AF.Ln, bias=1.0),
                          reads=[("scr", "E", ie)], writes=[("scr", "SP", i)])
                    p["pss"] = state["prev_ss"]
                    if kt > 0:
                        si = n % 3
                        SSn = SSb[si]
                        nk = ("scr", "SS", si)
                        if state["prev_ss"] is None:
                            P.add("pool", lambda e: e.tensor_copy(out=SSn[:, cols], in_=SP[:, cols]),
                                  reads=[("scr", "SP", i)], writes=[nk])
                        else:
                            pss, pk, pc0 = state["prev_ss"]
                            if pc0 > c0:
                                P.add("pool", lambda e: e.tensor_copy(out=SSn[:, c0:pc0], in_=SP[:, c0:pc0]),
                                      reads=[("scr", "SP", i)], writes=[nk])
                            P.add("pool", lambda e: e.tensor_tensor(
                                out=SSn[:, pc0:512], in0=SP[:, pc0:512], in1=pss[:, pc0:512], op=ALU.add),
                                reads=[("scr", "SP", i), pk], writes=[nk])
                        state["prev_ss"] = (SSn, nk, c0)

                def stB(p, n):
                    i = n % 2
                    c0 = p["c0"]
                    cols = slice(c0, 512)
                    ie = n % 3
                    E, SP, G, Wt = Eb[ie], SPb[i], Gb[i], Wb[i]
                    bc = rr6()
                    pss = p["pss"]
                    P.add("pe", lambda e: e.matmul(ps[bc][:, cols], lhsT=TRI, rhs=SP[:, cols], start=True,
                                                   stop=(pss is None)),
                          reads=[("scr", "SP", i), ("cb",)], writes=[("ps", bc)])
                    if pss is not None:
                        ssb, pk, pc0 = pss
                        P.add("pe", lambda e: e.matmul(ps[bc][:, pc0:512], lhsT=ones1[:], rhs=ssb[:, pc0:512],
                                                       start=False, stop=True),
                              reads=[pk, ("ones",)], writes=[("ps", bc)])
                    dummies(NDUM["sb"])
                    P.add("act", lambda e: e.activation(out=G[:, cols], in_=ps[bc][:, cols], func=AF.Exp, scale=-1.0),
                          reads=[("ps", bc)], writes=[("scr", "G", i)])
                    P.add("dve", lambda e: e.tensor_tensor(out=Wt[:, cols], in0=E[:, cols], in1=G[:, cols], op=ALU.mult),
                          reads=[("scr", "E", ie), ("scr", "G", i)], writes=[("scr", "W", i)])

                def stC(p, n, hp=hp):
                    i = n % 2
                    c0, bo, pj, kt, qt = p["c0"], p["bo"], p["pj"], p["kt"], p["qt"]
                    cols = slice(c0, 512)
                    Wt = Wb[i]
                    if p["first"]:
                        P.add("pe", lambda e: e.matmul(ps[bo][0:64, :], lhsT=zer[:, 0:64], rhs=cb[:, 0:512],
                                                       start=True, stop=False),
                              reads=[("ones",), ("cb",)], writes=[("ps", bo)])
                    P.add("pe", lambda e: e.matmul(ps[bo][0:64, cols], lhsT=vv[:, kt, pj], rhs=Wt[:, cols],
                                                   start=False, stop=p["last"]),
                          reads=[("scr", "W", i), ("scr", "v", kt // 4)], writes=[("ps", bo)])
                    if p["last"]:
                        P.add("dve", lambda e: e.tensor_copy(out=bigB[pj, hp, TT(qt)], in_=ps[bo][0:64, :]),
                              reads=[("ps", bo)], writes=[("bigB", hp, qt, pj.start)])

                NPR = len(pairs)
                for n in range(NPR + 2):
                    if n < NPR:
                        stA(pairs[n], n)
                    if 1 <= n <= NPR:
                        stB(pairs[n - 1], n - 1)
                    if n >= 2:
                        stC(pairs[n - 2], n - 2)

        def moba_attention(l):
            P.fence("scr")
            P.fence("bigC")
            P.add("dve", lambda e: e.memset(vext, 1.0), writes=[("scr", "vext")])
            for j in range(2):
                P.add("pool", lambda e, j=j: e.dma_start(out=kaug[64:75, j, :], in_=kst_d[64:75, :]),
                      writes=[("scr", "kst", j)], dma="c1")
            ksf = scr[0:64, 0:16].rearrange("p (j n) -> p j n", j=2)
            gm = scr[:, 64:128].rearrange("p (g n) -> p g n", g=8)
            cmp_ = scr[:, 128:640].rearrange("p (g n m) -> p g n m", g=8, n=8)
            cntt = scr[:, 640:704].rearrange("p (g n) -> p g n", g=8)
            t1 = scr[:, 704:768].rearrange("p (g n) -> p g n", g=8)
            rden = scr[0:64, 768:1280]
            Pm = [scr[:, 1280:1536].bitcast(BF16), scr[:, 1536:1792].bitcast(BF16), scr[:, 1792:2048].bitcast(BF16)]
            cnt = {"p": 0}
            GM = cst[:, C_GM:C_GM + 128].rearrange("p (o j n) -> p o j n", o=8, j=2)
            LL = cst[:, C_L:C_L + 64].rearrange("p (o n) -> p o n", o=8)
            for hp in range(4):
                keys, (wq, wk) = wload([wview(Win, l, 0, D, OFF_MBQ + hp * 128, 128),
                                        wview(Win, l, 0, D, OFF_MBK + hp * 128, 128)])
                keys2, (wv,) = wload([wview(Win, l, 0, D, OFF_MBV + hp * 128, 128)])
                keys = keys + keys2

                def evq(t, b):
                    P.add("act", lambda e, t=t, b=b: e.copy(out=qaug[0:64, 0, TT(t)], in_=ps[b][0:64, :]),
                          reads=[("ps", b)], writes=[("scr", "qaug", 0, t)])
                    P.add("dve", lambda e, t=t, b=b: e.tensor_copy(out=qaug[0:64, 1, TT(t)], in_=ps[b][64:128, :]),
                          reads=[("ps", b)], writes=[("scr", "qaug", 1, t)])
                proj_fm(keys[0], wq, evq)

                def evk(t, b):
                    P.add("act", lambda e, t=t, b=b: e.copy(out=kaug[0:64, 0, TT(t)], in_=ps[b][0:64, :]),
                          reads=[("ps", b)], writes=[("scr", "kaug", 0, t)])
                    P.add("dve", lambda e, t=t, b=b: e.tensor_copy(out=kaug[0:64, 1, TT(t)], in_=ps[b][64:128, :]),
                          reads=[("ps", b)], writes=[("scr", "kaug", 1, t)])
                    for j in range(2):
                        P.add("dve", lambda e, t=t, b=b, j=j: e.tensor_reduce(
                            out=ksf[:, j, 2 * t:2 * t + 2], in_=ps[b][64 * j:64 * j + 64, :].rearrange("p (a n) -> p a n", a=2),
                            axis=AX.X, op=ALU.add),
                            reads=[("ps", b)], writes=[("scr", "ksf", j, t)])
                proj_fm(keys[1], wk, evk)
                P.add("dve", lambda e: e.tensor_copy(out=ksumb[:], in_=ksf),
                      reads=[("scr", "ksf", j, t) for j in range(2) for t in range(4)], writes=[("ksumb",)])
                for g in range(4):
                    b = rr8()
                    for ti in range(4):
                        t16 = g * 4 + ti
                        for kc in range(8):
                            P.add("pe", lambda e, kc=kc, t16=t16, ti=ti, b=b, wv=wv: e.matmul(
                                ps[b][:, ti * 128:(ti + 1) * 128], lhsT=xn[:, kc, t16 * 128:(t16 + 1) * 128],
                                rhs=wv[:, kc, :], start=(kc == 0), stop=(kc == 7)),
                                reads=[keys[2], ("xn", kc, t16 // 4)], writes=[("ps", b)])
                    P.add("act", lambda e, g=g, b=b: e.copy(
                        out=vext[:, g * 4:(g + 1) * 4, :, 0:64],
                        in_=ps[b][:].rearrange("p (t j n) -> p t j n", t=4, j=2)),
                        reads=[("ps", b), ("scr", "vext")], writes=[("scr", "vext", g)])
                for qt in range(4):
                    bg = rr8()
                    for ti in range(4):
                        t16 = qt * 4 + ti
                        for j in range(2):
                            g8 = ti * 2 + j
                            P.add("pe", lambda e, bg=bg, g8=g8, j=j, t16=t16: e.matmul(
                                ps[bg][:, g8 * 8:(g8 + 1) * 8], lhsT=qaug[0:64, j, t16 * 128:(t16 + 1) * 128],
                                rhs=ksumb[:, j, :], start=True, stop=True),
                                reads=[("scr", "qaug", j, qt), ("ksumb",)], writes=[("ps", bg)])
                    for ti in range(4):
                        own = (qt * 4 + ti) // 2
                        P.add("dve", lambda e, bg=bg, ti=ti, own=own: e.tensor_tensor(
                            out=gm[:, 2 * ti:2 * ti + 2, :],
                            in0=ps[bg][:, 16 * ti:16 * ti + 16].rearrange("p (j n) -> p j n", j=2),
                            in1=GM[:, own, :, :], op=ALU.add),
                            reads=[("ps", bg), ("cst",)], writes=[("scr", "gm")])
                    gap = [list(a) for a in gm.ap]
                    gm_m = bass.AP(gm.tensor, gm.offset, [gap[0], gap[1], [0, 8], gap[2]])
                    gm_n = bass.AP(gm.tensor, gm.offset, [gap[0], gap[1], gap[2], [0, 8]])
                    P.add("dve", lambda e, gm_m=gm_m, gm_n=gm_n: e.tensor_tensor(
                        out=cmp_, in0=gm_m, in1=gm_n, op=ALU.is_gt),
                        reads=[("scr", "gm")], writes=[("scr", "cmp")])
                    P.add("dve", lambda e: e.tensor_reduce(out=cntt, in_=cmp_, axis=AX.X, op=ALU.add),
                          reads=[("scr", "cmp")], writes=[("scr", "cnt")])
                    P.add("dve", lambda e: e.tensor_scalar(out=t1, in0=cntt, scalar1=2.5, scalar2=BIG,
                                                           op0=ALU.is_lt, op1=ALU.mult),
                          reads=[("scr", "cnt")], writes=[("scr", "t1")])
                    for ti in range(4):
                        own = (qt * 4 + ti) // 2
                        for j in range(2):
                            hh = 2 * hp + j
                            P.add("dve", lambda e, ti=ti, j=j, hh=hh, own=own: e.scalar_tensor_tensor(
                                out=selT[:, hh, ti, 64:72], in0=t1[:, 2 * ti + j, :], scalar=-BIG, in1=LL[:, own, :],
                                op0=ALU.add, op1=ALU.max),
                                reads=[("scr", "t1"), ("cst",), ("selst",)], writes=[("selT", hh, ti)])
                    for j in range(2):
                        hh = 2 * hp + j
                        bt = rr8()
                        for ti in range(4):
                            P.add("pe", lambda e, bt=bt, ti=ti, hh=hh: e.matmul(
                                ps[bt][0:80, ti * 128:(ti + 1) * 128], lhsT=selT[:, hh, ti, :], rhs=IDN,
                                start=True, stop=True),
                                reads=[("selT", hh, ti), ("cb",)], writes=[("ps", bt)])
                        P.add("act", lambda e, bt=bt, j=j, qt=qt: e.copy(out=qaug[64:75, j, TT(qt)], in_=ps[bt][64:75, :]),
                              reads=[("ps", bt)], writes=[("scr", "qst", j, qt)])
                pairs = []
                for j in range(2):
                    for qt in range(4):
                        nkt = (qt + 1) * 4
                        bo = rrO()
                        for kt in range(nkt):
                            k0, q0 = kt * 128, qt * 512
                            diag = k0 >= q0
                            c0 = k0 - q0 if diag else 0
                            pairs.append(dict(j=j, qt=qt, kt=kt, k0=k0, q0=q0, diag=diag, c0=c0, first=(kt == 0),
                                              last=(kt == nkt - 1), bo=bo, hh=2 * hp + j))

                def mA(p, n):
                    c0, q0, k0, j, kt, qt = p["c0"], p["q0"], p["k0"], p["j"], p["kt"], p["qt"]
                    cols = slice(c0, 512)
                    qcols = slice(q0 + c0, q0 + 512)
                    bs = rr6()
                    p["bs"] = bs
                    P.add("pe", lambda e: e.matmul(
                        ps[bs][:, cols], lhsT=kaug[0:75, j, k0:k0 + 128], rhs=qaug[0:75, j, qcols],
                        start=True, stop=not p["diag"]),
                        reads=[("scr", "kaug", j, kt // 4), ("scr", "kst", j), ("scr", "qaug", j, qt), ("scr", "qst", j, qt)],
                        writes=[("ps", bs)])
                    if p["diag"]:
                        P.add("pe", lambda e: e.matmul(ps[bs][:, c0:c0 + 128], lhsT=IDN, rhs=NEGM2, start=False, stop=True),
                              reads=[("cb",)], writes=[("ps", bs)])

                def mB(p, n):
                    i = n % 3
                    c0, bs = p["c0"], p["bs"]
                    cols = slice(c0, 512)
                    pm = Pm[i]
                    biasc = float(-SLOPES[p["hh"]] * (p["q0"] - p["k0"]))
                    P.add("act", lambda e: e.activation(
                        out=pm[:, cols], in_=ps[bs][:, cols], func=AF.Exp, scale=0.125, bias=biasc),
                        reads=[("ps", bs)], writes=[("scr", "Pm", i)])

                def mC(p, n, hp=hp):
                    i = n % 3
                    c0, bo, j, kt, qt = p["c0"], p["bo"], p["j"], p["kt"], p["qt"]
                    cols = slice(c0, 512)
                    pm = Pm[i]
                    pj = slice(64 * j, 64 * j + 64)
                    P.add("pe", lambda e: e.matmul(ps[bo][:, cols], lhsT=vext[:, kt, j, :], rhs=pm[:, cols],
                                                   start=p["first"], stop=p["last"]),
                          reads=[("scr", "Pm", i), ("scr", "vext", kt // 4), ("scr", "vext")], writes=[("ps", bo)])
                    if p["last"]:
                        P.add("dve", lambda e: e.reciprocal(out=rden, in_=ps[bo][64:128, :]),
                              reads=[("ps", bo)], writes=[("scr", "rden")])
                        P.add("dve", lambda e: e.tensor_tensor(
                            out=bigC[pj, hp, TT(qt)], in0=ps[bo][0:64, :], in1=rden, op=ALU.mult),
                            reads=[("ps", bo), ("scr", "rden")], writes=[("bigC", hp, qt, pj.start)])

                NPR = len(pairs)
                for n in range(NPR + 2):
                    if n < NPR:
                        mA(pairs[n], n)
                    if 1 <= n <= NPR:
                        mB(pairs[n - 1], n - 1)
                    if n >= 2:
                        mC(pairs[n - 2], n - 2)

        def mix_out(l, have):
            P.fence("scr")
            mixed = scr[:, 0:4096].bitcast(BF16).rearrange("p (k n) -> p k n", k=8)
            gtb = [scr[:, 4096 + 512 * i:4608 + 512 * i] for i in range(2)]
            ttb = [[scr[:, 5120 + 512 * (3 * a + i):5632 + 512 * (3 * a + i)] for i in range(3)] for a in range(2)]
            gcnt = {"i": 0}
            srcs = [("sb", bigB, WA, "bigB"), ("mb", bigC, WB, "bigC"), ("conv", bigA, WC, "bigA")]
            for half in range(2):
                for dc in range(8):
                    kg1, gw1 = wload([wview(Win, l, 0, D, OFF_GATE + br * D + dc * 128, 128) for br in range(2)])
                    kg2, gw2 = wload([wview(Win, l, 0, D, OFF_GATE + 2 * D + dc * 128, 128),
                                      wview(WA, l, 0, 512, dc * 128, 128), wview(WB, l, 0, 512, dc * 128, 128)])
                    kg3, gw3 = wload([wview(WC, l, 0, 512, dc * 128, 128)])
                    keysg, gw = kg1 + kg2[0:1], gw1 + gw2[0:1]
                    keysy, yw = kg2[1:3] + kg3, gw2[1:3] + gw3
                    for t2 in range(2):
                        t = half * 2 + t2
                        tt_ = ttb[t2]
                        for br, (nm, ob, W_, blk) in enumerate(srcs):
                            if nm not in have:
                                continue
                            bgate = rr8()
                            for kc in range(8):
                                P.add("pe", lambda e, kc=kc, t=t, bgate=bgate, gwb=gw[br]: e.matmul(
                                    ps[bgate][:], lhsT=gwb[:, kc, :], rhs=xn[:, kc, TT(t)],
                                    start=(kc == 0), stop=(kc == 7)),
                                    reads=[keysg[br], ("xn", kc, t)], writes=[("ps", bgate)])
                            by = rr8()
                            for kc in range(4):
                                if nm == "conv":
                                    rk = [("bigA", kc, t)]
                                else:
                                    rk = [(blk, kc, t, 0), (blk, kc, t, 64)]
                                P.add("pe", lambda e, kc=kc, t=t, by=by, ywb=yw[br], ob=ob: e.matmul(
                                    ps[by][:], lhsT=ywb[:, kc, :], rhs=ob[:, kc, TT(t)],
                                    start=(kc == 0), stop=(kc == 3)),
                                    reads=[keysy[br]] + rk, writes=[("ps", by)])
                            gi = gcnt["i"] % 2
                            gcnt["i"] += 1
                            P.add("act", lambda e, br=br, bgate=bgate, dc=dc, gi=gi: e.activation(
                                out=gtb[gi], in_=ps[bgate][:], func=AF.Sigmoid, bias=vcol(l, V_GB, br * 8 + dc)),
                                reads=[("ps", bgate), ("vecs",)], writes=[("scr", "gt", gi)])
                            P.add("dve", lambda e, br=br, by=by, gi=gi, tt_=tt_: e.tensor_tensor(
                                out=tt_[br], in0=gtb[gi], in1=ps[by][:], op=ALU.mult),
                                reads=[("scr", "gt", gi), ("ps", by)], writes=[("scr", "tt", t2, br)])
                        live = [br for br, s_ in enumerate(srcs) if s_[0] in have]
                        mo = mixed[:, dc, t2 * 512:(t2 + 1) * 512]
                        mk_ = ("scr", "mixed", dc, t2)
                        if len(live) == 1:
                            P.add("dve", lambda e, mo=mo, a=live[0], tt_=tt_: e.tensor_copy(out=mo, in_=tt_[a]),
                                  reads=[("scr", "tt", t2, live[0])], writes=[mk_])
                        elif len(live) == 2:
                            P.add("dve", lambda e, mo=mo, a=live[0], b_=live[1], tt_=tt_: e.tensor_tensor(
                                out=mo, in0=tt_[a], in1=tt_[b_], op=ALU.add),
                                reads=[("scr", "tt", t2, live[0]), ("scr", "tt", t2, live[1])], writes=[mk_])
                        else:
                            P.add("dve", lambda e, tt_=tt_: e.tensor_tensor(out=tt_[0], in0=tt_[0], in1=tt_[1], op=ALU.add),
                                  reads=[("scr", "tt", t2, 0), ("scr", "tt", t2, 1)], writes=[("scr", "tt", t2, 0)])
                            P.add("dve", lambda e, mo=mo, tt_=tt_: e.tensor_tensor(out=mo, in0=tt_[0], in1=tt_[2], op=ALU.add),
                                  reads=[("scr", "tt", t2, 0), ("scr", "tt", t2, 2)], writes=[mk_])
                dump("mixed", scr[:, 0:4096].bitcast(BF16), [("scr", "mixed", dc_, t_) for dc_ in range(8) for t_ in range(2)])
                for d0 in range(0, 8, 2):
                    keys, (wo,) = wload([wview(WO, l, 0, D, d0 * 128, 256)])
                    for di in range(2):
                        dc = d0 + di
                        for t2 in range(2):
                            t = half * 2 + t2
                            b = rr8()
                            for kc in range(8):
                                P.add("pe", lambda e, kc=kc, t2=t2, b=b, di=di, wo=wo: e.matmul(
                                    ps[b][:], lhsT=wo[:, kc, di * 128:(di + 1) * 128],
                                    rhs=mixed[:, kc, t2 * 512:(t2 + 1) * 512], start=(kc == 0), stop=(kc == 7)),
                                    reads=[keys[0], ("scr", "mixed", kc, t2)], writes=[("ps", b)])
                            P.add("dve", lambda e, b=b, dc=dc, t=t: e.tensor_tensor(
                                out=h[:, dc, TT(t)], in0=ps[b][:], in1=h[:, dc, TT(t)], op=ALU.add),
                                reads=[("ps", b)], writes=[("h", dc, t)])

        outs = []
        for s_i in range(nseq):
            for dc in range(8):
                P.add("sp", lambda e, dc=dc, s_i=s_i: e.dma_start(out=h[:, dc, :], in_=xT[s_i, dc * 128:(dc + 1) * 128, :]),
                      writes=[("h", dc, t) for t in range(4)], dma="x%d" % dc)
            for l in range(depth):
                if "ffn1" in stages:
                    ffn(l, W1i, W1o, V_FFN1N)
                if "mix" in stages:
                    rmsnorm(l, V_MIXN)
                    if "conv" in mix_parts:
                        conv_branch(l)
                    if "sb" in mix_parts:
                        sb_attention(l)
                    if "mb" in mix_parts:
                        moba_attention(l)
                    mix_out(l, mix_parts)
                if "ffn2" in stages:
                    ffn(l, W2i, W2o, V_FFN2N)
            if "final" in stages:
                rmsnorm(None, depth * NVEC_L, out_f32=h)
            for dc in range(8):
                outs.append(P.add("sp", lambda e, dc=dc, s_i=s_i: e.dma_start(
                    out=outT[s_i, dc * 128:(dc + 1) * 128, :], in_=h[:, dc, :]),
                    reads=[("h", dc, t) for t in range(4)], dma="o%d" % dc))
        P.emit(nc, final_waits=outs[-8:] + dbg_ops)
    return nc


_CACHE = {}


def kernel(**inputs):
    x = np.asarray(inputs["x"], np.float32)
    B = x.shape[0]
    per = B // NCORES
    cst, kst, sel = host_consts()
    vecs = host_vecs(inputs, DEPTH_FULL)
    nc = build(per, DEPTH_FULL)
    shared = {k: np.ascontiguousarray(np.asarray(inputs[k], np.float32)) for k in
              ("ffn1_w_in", "ffn1_w_out", "ffn2_w_in", "ffn2_w_out", "w_in", "sb_w_out", "mb_w_out",
               "conv_w_out", "w_o")}
    shared.update({"vecs": vecs, "cst": cst, "kst": kst, "selst": sel})
    in_maps = []
    for c in range(NCORES):
        xs = x[c * per:(c + 1) * per]
        m = dict(shared)
        m["xT"] = np.ascontiguousarray(xs.transpose(0, 2, 1))
        in_maps.append(m)
    res = run_bass_kernel_spmd(nc, in_maps, core_ids=list(range(NCORES)))
    out = np.empty((B, S, D), np.float32)
    for c in range(NCORES):
        out[c * per:(c + 1) * per] = res.results[c]["outT"].transpose(0, 2, 1)
    return out
```

```python
import contextlib
import numpy as np
import concourse.bass as bass
import concourse.mybir as mybir
from concourse.bass_utils import run_bass_kernel_spmd

F32 = mybir.dt.float32
BF16 = mybir.dt.bfloat16
AF = mybir.ActivationFunctionType
ALU = mybir.AluOpType
AX = mybir.AxisListType

D = 1024
S = 2048
DFF = 2816
NFC = 22
DEPTH_FULL = 2
NCORES = 8
SEQ_PER_CORE = 4
OFF_SBQ, OFF_SBK, OFF_SBV = 0, 512, 1024
OFF_MBQ, OFF_MBK, OFF_MBV = 1536, 2048, 2560
OFF_CONV = 3072
OFF_GATE = 4096
IN_COLS = 7168
EPS = 1e-6
BIG = 29952.0
SLOPES = [2.0 ** (-(h + 1)) for h in range(8)]
ENGS = ("pe", "act", "dve", "pool", "sp")


class Op:
    __slots__ = ("eng", "fn", "pos", "sig", "waits", "dma", "val")

    def __init__(self, eng, fn, dma=None):
        self.eng, self.fn, self.dma = eng, fn, dma
        self.pos, self.sig, self.waits, self.val = -1, False, [], 0


class Prog:
    def __init__(self):
        self.streams = {e: [] for e in ENGS}
        self.last_w, self.readers = {}, {}
        self.seen = {e: {} for e in ENGS}
        self.dma_cnt = {}
        self.fences = {}

    def _need(self, x, y, raw):
        if y is None or y is x:
            return
        if y.dma is not None:
            key = ("d", y.dma)
            val = self.dma_cnt[y.dma] - (16 if x.dma == y.dma else 0)
            if self.seen[x.eng].get(key, 0) >= val:
                return
            self.seen[x.eng][key] = val
            x.waits.append(("d", y.dma, val))
            return
        if y.eng == x.eng and x.dma is None:
            if x.eng == "pe" or not raw:
                return
            if len(self.streams[x.eng]) - y.pos > 3:
                return
        key = ("e", y.eng)
        if self.seen[x.eng].get(key, -1) >= y.pos:
            return
        self.seen[x.eng][key] = y.pos
        y.sig = True
        x.waits.append(("e", y.eng, y))

    def fence(self, block):
        ops = {}
        for k in [k for k in self.last_w if k[0] == block]:
            o = self.last_w.pop(k)
            ops[id(o)] = o
        for k in [k for k in self.readers if k[0] == block]:
            for o in self.readers.pop(k):
                ops[id(o)] = o
        best = {}
        for o in ops.values():
            kk = (o.eng, o.dma)
            rank = o.val if o.dma is not None else o.pos
            if kk not in best or rank > best[kk][0]:
                best[kk] = (rank, o)
        if best:
            self.fences[block] = [v[1] for v in best.values()]

    def add(self, eng, fn, reads=(), writes=(), dma=None):
        x = Op(eng, fn, dma)
        if dma is not None:
            self.dma_cnt[dma] = self.dma_cnt.get(dma, 0) + 16
            x.val = self.dma_cnt[dma]
        reads = list(reads)
        writes = list(writes)
        for r in list(reads):
            if r[0] == "ps":
                reads.remove(r)
                writes.append(r)
        for k in reads + writes:
            if k not in self.last_w and k[0] in self.fences:
                for o in self.fences[k[0]]:
                    self._need(x, o, True)
        for r in reads:
            self._need(x, self.last_w.get(r), True)
        for w in writes:
            self._need(x, self.last_w.get(w), True)
            for rd in self.readers.get(w, ()):
                self._need(x, rd, False)
        x.pos = len(self.streams[eng])
        self.streams[eng].append(x)
        for r in reads:
            self.readers.setdefault(r, []).append(x)
        for w in writes:
            self.last_w[w] = x
            self.readers[w] = []
        return x

    def emit(self, nc, final_waits=()):
        with contextlib.ExitStack() as es:
            esem = {e: es.enter_context(nc.semaphore("s_" + e)) for e in ENGS}
            dsem = {n: es.enter_context(nc.semaphore("d_" + n)) for n in self.dma_cnt}
            block = es.enter_context(nc.Block())
            for e in ENGS:
                c = 0
                for op in self.streams[e]:
                    if op.dma is None:
                        if op.sig:
                            c += 1
                        op.val = c
            hooks = {"pe": block.tensor, "act": block.scalar, "dve": block.vector,
                     "pool": block.gpsimd, "sp": block.sync}

            def mk(e):
                def body(eng):
                    for op in self.streams[e]:
                        for w in op.waits:
                            if w[0] == "d":
                                eng.wait_ge(dsem[w[1]], w[2])
                            else:
                                eng.wait_ge(esem[w[1]], w[2].val)
                        ins = op.fn(eng)
                        if op.dma is not None:
                            ins.then_inc(dsem[op.dma], 16)
                        elif op.sig:
                            ins.then_inc(esem[e], 1)
                    if e == "sp":
                        for op in final_waits:
                            eng.wait_ge(dsem[op.dma], op.val)
                return body
            for e in ENGS:
                hooks[e](mk(e))


NVEC_L = 8 + 8 + 8 + 24 + 4 + 4 + 4 + 124
V_FFN1N, V_MIXN, V_FFN2N, V_GB, V_DWB, V_LNG, V_LNB, V_DW = 0, 8, 16, 24, 48, 52, 56, 60
C_TRI, C_MLT, C_MLE, C_ID, C_GM, C_L = 0, 128, 256, 384, 512, 640
NCST = 704


def host_consts():
    c = np.zeros((128, NCST), np.float32)
    j = np.arange(128)[:, None]
    s = np.arange(128)[None, :]
    c[:, C_TRI:C_TRI + 128] = (j >= s)
    c[:, C_MLT:C_MLT + 128] = np.where(j >= s, -BIG, 0.0)
    c[:, C_MLE:C_MLE + 128] = np.where(j > s, -BIG, 0.0)
    c[:, C_ID:C_ID + 128] = (j == s)
    own = np.arange(8)[:, None]
    n = np.arange(8)[None, :]
    gm = np.where(n < own, 0.0, -BIG).astype(np.float32)
    ll = np.where(n < own, -BIG, 0.0).astype(np.float32)
    c[:, C_GM:C_GM + 128] = np.repeat(gm[:, None, :], 2, axis=1).reshape(1, 128)
    c[:, C_L:C_L + 64] = ll.reshape(1, 64)
    kst = np.zeros((128, S), np.float32)
    pos = np.arange(S)
    for nn in range(8):
        kst[64 + nn] = (pos // 256 == nn)
    kst[72] = 1.0
    kst[73] = 1.0
    kst[74] = pos % 128
    sel = np.zeros((128, 8, 4, 80), np.float32)
    q = np.arange(128)
    for h in range(8):
        for m in range(4):
            i = m * 128 + q
            sel[:, h, m, 72] = -8.0 * SLOPES[h] * (256 * (i // 256))
            sel[:, h, m, 73] = -8.0 * SLOPES[h] * (i % 256)
            sel[:, h, m, 74] = 8.0 * SLOPES[h]
    return c, kst, sel.reshape(128, 8 * 4 * 80)


def host_vecs(inp, depth):
    def col(v):
        return np.ascontiguousarray(np.asarray(v, np.float32).reshape(-1, 128).T)
    cols = []
    for l in range(depth):
        cols += [col(inp["ffn1_norm"][l]), col(inp["mix_norm"][l]), col(inp["ffn2_norm"][l]),
                 col(inp["gate_bias"][l]), col(inp["conv_dw_bias"][l]), col(inp["conv_ln_g"][l]),
                 col(inp["conv_ln_b"][l])]
        dw = np.asarray(inp["conv_dw"][l], np.float32).reshape(31, 512)
        cols.append(np.ascontiguousarray(dw.reshape(31, 4, 128).transpose(2, 0, 1).reshape(128, 124)))
    cols.append(col(inp["final_norm"]))
    return np.ascontiguousarray(np.concatenate(cols, axis=1))


def build(nseq=SEQ_PER_CORE, depth=DEPTH_FULL, stages=("ffn1", "mix", "ffn2", "final"),
          mix_parts=("conv", "sb", "mb"), dbg=None):
    nc = bass.Bass("TRN2", target_bir_lowering=False)
    dram = {}

    def din(name, shape):
        dram[name] = nc.dram_tensor(name, list(shape), F32, kind="ExternalInput").ap()
        return dram[name]

    xT = din("xT", [nseq, D, S])
    W1i = din("ffn1_w_in", [depth, D, 2 * DFF])
    W1o = din("ffn1_w_out", [depth, DFF, D])
    W2i = din("ffn2_w_in", [depth, D, 2 * DFF])
    W2o = din("ffn2_w_out", [depth, DFF, D])
    Win = din("w_in", [depth, D, IN_COLS])
    WA = din("sb_w_out", [depth, 512, D])
    WB = din("mb_w_out", [depth, 512, D])
    WC = din("conv_w_out", [depth, 512, D])
    WO = din("w_o", [depth, D, D])
    NV = depth * NVEC_L + 8
    vecs_d = din("vecs", [128, NV])
    cst_d = din("cst", [128, NCST])
    kst_d = din("kst", [128, S])
    sel_d = din("selst", [128, 8 * 4 * 80])
    outT = nc.dram_tensor("outT", [nseq, D, S], F32, kind="ExternalOutput").ap()
    dbg_d = nc.dram_tensor("dbg", [128, 8192], F32, kind="ExternalOutput").ap() if dbg else None
    dbg_ops = []

    def dump(name, ap, keys):
        if dbg == name and not dbg_ops:
            n = ap.shape[1]
            dbg_ops.append(P.add("pool", lambda e: e.dma_start(out=dbg_d[:, 0:n], in_=ap), reads=keys, dma="dbg"))

    P = Prog()
    es = contextlib.ExitStack()
    with es:
        def sb(name, shape, dt):
            return es.enter_context(nc.sbuf_tensor(name, list(shape), dt))

        h = sb("h", [128, 8, S], F32)
        xn = sb("xn", [128, 8, S], BF16)
        bigA = sb("bigA", [128, 4, S], BF16)
        bigB = sb("bigB", [128, 4, S], BF16)
        bigC = sb("bigC", [128, 4, S], BF16)
        scr = sb("scr", [128, 8192], F32)
        NW = 5
        WSZ = 2048
        wsl = [sb("w%d" % i, [128, WSZ], BF16) for i in range(NW)]
        vecs = sb("vecs_sb", [128, NV], F32)
        cst = sb("cst_sb", [128, NCST], F32)
        cb = sb("cst_bf", [128, 512], BF16)
        ones_d = sb("ones_d", [128, 128], BF16)
        ones_c = sb("ones_c", [128, 128], BF16)
        ones1 = sb("ones1", [128, 128], BF16)
        zer = sb("zer", [128, 128], BF16)
        eps_c = sb("eps_c", [128, 1], F32)
        kaug = scr[:, 2048:4096].bitcast(BF16).rearrange("p (j n) -> p j n", j=2)
        qaug = scr[:, 4096:6144].bitcast(BF16).rearrange("p (j n) -> p j n", j=2)
        vext = scr[:, 6144:8192].bitcast(BF16).rearrange("p (t j n) -> p t j n", t=16, j=2)
        selT = sb("selT", [128, 8, 4, 80], BF16)
        ksumb = sb("ksumb", [64, 2, 8], BF16)
        ps = [es.enter_context(nc.psum_tensor("ps%d" % i, [128, 512], F32)) for i in range(8)]

        class RR:
            def __init__(self, ids):
                self.ids, self.i = list(ids), 0

            def __call__(self):
                b = self.ids[self.i % len(self.ids)]
                self.i += 1
                return b
        rr8 = RR(range(8))
        rr6 = RR(range(5))
        NDUM = {"sb": 3}

        def dummies(n):
            for _ in range(n):
                P.add("pe", lambda e: e.matmul(ps[5][:], lhsT=zer[:], rhs=cb[:, 0:512], start=True, stop=True),
                      reads=[("ones",), ("cb",)], writes=[("ps", 5)])
        rrO = RR([6, 7])
        wstate = {"i": 0}

        def wload(parts, eng="pool"):
            si = wstate["i"] % NW
            wstate["i"] += 1
            P.fence("w%d" % si)
            off = 0
            views, keys = [], []
            for pi, ap in enumerate(parts):
                K, n = ap.shape[1], ap.shape[2]
                v = wsl[si][:, off:off + K * n].rearrange("p (k n) -> p k n", k=K)
                key = ("w%d" % si, pi)
                P.add(eng, lambda e, v=v, ap=ap: e.dma_start(out=v, in_=ap), writes=[key], dma="w%d" % si)
                views.append(v)
                keys.append(key)
                off += K * n
            assert off <= WSZ
            return keys, views

        def wview(Wd, l, r0, nrow, c0, ncol):
            return Wd[l, r0:r0 + nrow, c0:c0 + ncol].rearrange("(k p) n -> p k n", p=128)

        def vcol(l, base, i):
            c = l * NVEC_L + base + i
            return vecs[:, c:c + 1]

        def TT(t):
            return slice(t * 512, (t + 1) * 512)

        P.add("sp", lambda e: e.dma_start(out=vecs[:], in_=vecs_d), writes=[("vecs",)], dma="c0")
        P.add("sp", lambda e: e.dma_start(out=cst[:], in_=cst_d), writes=[("cst",)], dma="c0")
        P.add("dve", lambda e: e.tensor_copy(out=cb[:], in_=cst[:, 0:512]), reads=[("cst",)], writes=[("cb",)])
        P.add("dve", lambda e: e.memset(ones_d[:], 1.0 / 1024), writes=[("ones",)])
        P.add("dve", lambda e: e.memset(ones_c[:], 1.0 / 512), writes=[("ones",)])
        P.add("dve", lambda e: e.memset(ones1[:], 1.0), writes=[("ones",)])
        P.add("dve", lambda e: e.memset(zer[:], 0.0), writes=[("ones",)])
        P.add("dve", lambda e: e.memset(eps_c[:], EPS), writes=[("ones",)])
        P.add("pool", lambda e: e.dma_start(out=selT[:].rearrange("p a b c -> p (a b c)"), in_=sel_d),
              writes=[("selst",)], dma="c1")
        TRI = cb[:, C_TRI:C_TRI + 128]
        NEGM2 = cb[:, C_MLE:C_MLE + 128]
        NEGM = cb[:, C_MLT:C_MLT + 128]
        IDN = cb[:, C_ID:C_ID + 128]
        MLT = cst[:, C_MLT:C_MLT + 128]

        def rmsnorm(l, base, out_bf=True, out_f32=None):
            P.fence("scr")
            sqbs = [scr[:, 0:2048].bitcast(BF16).rearrange("p (k n) -> p k n", k=8),
                    scr[:, 2048:4096].bitcast(BF16).rearrange("p (k n) -> p k n", k=8)]
            rstds = [scr[:, 4096:4608], scr[:, 4608:5120]]
            banks = {}

            def sqs(t):
                sqb = sqbs[t % 2]
                for dc in range(8):
                    P.add("act", lambda e, dc=dc: e.activation(out=sqb[:, dc, :], in_=h[:, dc, TT(t)], func=AF.Square),
                          reads=[("h", dc, t)], writes=[("scr", "sq", t % 2, dc)])
                bnk = rr8()
                banks[t] = bnk
                for dc in range(8):
                    P.add("pe", lambda e, dc=dc: e.matmul(ps[bnk][:], lhsT=ones_d[:], rhs=sqb[:, dc, :],
                                                          start=(dc == 0), stop=(dc == 7)),
                          reads=[("scr", "sq", t % 2, dc), ("ones",)], writes=[("ps", bnk)])

            def fin(t):
                bnk = banks[t]
                rstd = rstds[t % 2]
                rk = ("scr", "rstd", t % 2)
                P.add("act", lambda e: e.activation(out=rstd, in_=ps[bnk][:], func=AF.Sqrt, bias=eps_c[:, 0:1]),
                      reads=[("ps", bnk), ("ones",)], writes=[rk])
                P.add("dve", lambda e: e.reciprocal(out=rstd, in_=rstd), reads=[rk], writes=[rk])
                for dc in range(8):
                    if out_f32 is None:
                        o = xn[:, dc, TT(t)]
                        wk = ("xn", dc, t)
                    else:
                        o = out_f32[:, dc, TT(t)]
                        wk = ("h", dc, t)
                    c = base + dc if l is None else l * NVEC_L + base + dc
                    P.add("dve", lambda e, o=o, dc=dc, c=c: e.scalar_tensor_tensor(
                        out=o, in0=h[:, dc, TT(t)], scalar=vecs[:, c:c + 1], in1=rstd, op0=ALU.mult, op1=ALU.mult),
                        reads=[("h", dc, t), rk, ("vecs",)], writes=[wk])

            sqs(0)
            for t in range(4):
                if t + 1 < 4:
                    sqs(t + 1)
                fin(t)

        def ffn(l, Wi, Wo, nbase):
            rmsnorm(l, nbase)
            P.fence("scr")
            P.fence("bigA")
            P.fence("bigB")
            sil = [scr[:, 5120:5632], scr[:, 5632:6144]]
            groups = [(0, 8), (8, 7), (15, 7)]
            sidx = 0
            for (g0, gn) in groups:
                def hid(c, t):
                    return (bigA if c < 4 else bigB)[:, c % 4, TT(t)]
                for c0 in range(0, gn, 1):
                    ncnk = 1
                    fc = g0 + c0
                    keys, (wa, wb) = wload([wview(Wi, l, 0, D, fc * 128, ncnk * 128),
                                            wview(Wi, l, 0, D, DFF + fc * 128, ncnk * 128)])
                    for ci in range(ncnk):
                        c = c0 + ci
                        for t in range(4):
                            ba, bb = rr8(), rr8()
                            for kc in range(8):
                                P.add("pe", lambda e, kc=kc, t=t, ba=ba, ci=ci, wa=wa: e.matmul(
                                    ps[ba][:], lhsT=wa[:, kc, ci * 128:(ci + 1) * 128], rhs=xn[:, kc, TT(t)],
                                    start=(kc == 0), stop=(kc == 7)),
                                    reads=[keys[0], ("xn", kc, t)], writes=[("ps", ba)])
                            for kc in range(8):
                                P.add("pe", lambda e, kc=kc, t=t, bb=bb, ci=ci, wb=wb: e.matmul(
                                    ps[bb][:], lhsT=wb[:, kc, ci * 128:(ci + 1) * 128], rhs=xn[:, kc, TT(t)],
                                    start=(kc == 0), stop=(kc == 7)),
                                    reads=[keys[1], ("xn", kc, t)], writes=[("ps", bb)])
                            st = sil[sidx % 2]
                            sk = ("scr", "sil", sidx % 2)
                            sidx += 1
                            P.add("act", lambda e, st=st, ba=ba: e.activation(out=st, in_=ps[ba][:], func=AF.Silu),
                                  reads=[("ps", ba)], writes=[sk])
                            P.add("dve", lambda e, st=st, bb=bb, c=c, t=t: e.tensor_tensor(
                                out=hid(c, t), in0=st, in1=ps[bb][:], op=ALU.mult),
                                reads=[sk, ("ps", bb)], writes=[("bigA" if c < 4 else "bigB", c % 4, t)])
                for d0 in range(0, 8, 2):
                    keys, (wo,) = wload([wview(Wo, l, g0 * 128, gn * 128, d0 * 128, 256)])
                    for di in range(2):
                        dc = d0 + di
                        for t in range(4):
                            b = rr8()
                            for c in range(gn):
                                P.add("pe", lambda e, c=c, t=t, b=b, di=di, wo=wo, gn=gn: e.matmul(
                                    ps[b][:], lhsT=wo[:, c, di * 128:(di + 1) * 128], rhs=hid(c, t),
                                    start=(c == 0), stop=(c == gn - 1)),
                                    reads=[keys[0], ("bigA" if c < 4 else "bigB", c % 4, t)], writes=[("ps", b)])
                            P.add("dve", lambda e, b=b, dc=dc, t=t: e.scalar_tensor_tensor(
                                out=h[:, dc, TT(t)], in0=ps[b][:], scalar=0.5, in1=h[:, dc, TT(t)],
                                op0=ALU.mult, op1=ALU.add),
                                reads=[("ps", b)], writes=[("h", dc, t)])

        def proj_fm(keyw, wv, evac):
            for t in range(4):
                b = rr8()
                for kc in range(8):
                    P.add("pe", lambda e, kc=kc, t=t, b=b: e.matmul(ps[b][:], lhsT=wv[:, kc, :], rhs=xn[:, kc, TT(t)],
                                                                     start=(kc == 0), stop=(kc == 7)),
                          reads=[keyw, ("xn", kc, t)], writes=[("ps", b)])
                evac(t, b)

        def conv_branch(l):
            for blk in ("bigA", "bigB", "bigC", "scr"):
                P.fence(blk)
            gpad = [bigB[:, 0:2, :].rearrange("p a n -> p (a n)"), bigB[:, 2:4, :].rearrange("p a n -> p (a n)"),
                    bigC[:, 0:2, :].rearrange("p a n -> p (a n)"), bigC[:, 2:4, :].rearrange("p a n -> p (a n)")]
            gkey = [("bigB", "g0"), ("bigB", "g1"), ("bigC", "g2"), ("bigC", "g3")]
            sg = [scr[:, 0:512], scr[:, 512:1024]]
            si = 0
            for cc in range(4):
                P.add("dve", lambda e, cc=cc: e.memset(gpad[cc][:, 0:30], 0.0), writes=[gkey[cc]])
                keys, (wv, wg) = wload([wview(Win, l, 0, D, OFF_CONV + cc * 128, 128),
                                        wview(Win, l, 0, D, OFF_CONV + 512 + cc * 128, 128)])
                for t in range(4):
                    bv, bg = rr8(), rr8()
                    for kc in range(8):
                        P.add("pe", lambda e, kc=kc, t=t, bv=bv, wv=wv: e.matmul(
                            ps[bv][:], lhsT=wv[:, kc, :], rhs=xn[:, kc, TT(t)], start=(kc == 0), stop=(kc == 7)),
                            reads=[keys[0], ("xn", kc, t)], writes=[("ps", bv)])
                    for kc in range(8):
                        P.add("pe", lambda e, kc=kc, t=t, bg=bg, wg=wg: e.matmul(
                            ps[bg][:], lhsT=wg[:, kc, :], rhs=xn[:, kc, TT(t)], start=(kc == 0), stop=(kc == 7)),
                            reads=[keys[1], ("xn", kc, t)], writes=[("ps", bg)])
                    s_ = sg[si % 2]
                    sk = ("scr", "sg", si % 2)
                    si += 1
                    P.add("act", lambda e, s_=s_, bg=bg: e.activation(out=s_, in_=ps[bg][:], func=AF.Sigmoid),
                          reads=[("ps", bg)], writes=[sk])
                    P.add("dve", lambda e, s_=s_, bv=bv, cc=cc, t=t: e.tensor_tensor(
                        out=gpad[cc][:, 30 + t * 512:30 + (t + 1) * 512], in0=s_, in1=ps[bv][:], op=ALU.mult),
                        reads=[sk, ("ps", bv)], writes=[gkey[cc]])
            P.fence("scr")
            Dgs = [scr[:, 0:1984].bitcast(BF16).rearrange("p (k n) -> p k n", k=31),
                   scr[:, 1984:3968].bitcast(BF16).rearrange("p (k n) -> p k n", k=31)]
            for cc in range(4):
                Dg = Dgs[cc % 2]
                for tap in range(31):
                    P.add("dve", lambda e, Dg=Dg, tap=tap, cc=cc: e.tensor_scalar(
                        out=Dg[:, tap, :], in0=IDN, scalar1=vcol(l, V_DW, tap * 4 + cc), scalar2=None, op0=ALU.mult),
                        reads=[("cb",), ("vecs",)], writes=[("scr", "Dg", cc % 2, tap)])
                for t in range(4):
                    bk = rr8()
                    for tap in range(31):
                        P.add("pe", lambda e, Dg=Dg, tap=tap, cc=cc, t=t, bk=bk: e.matmul(
                            ps[bk][:], lhsT=Dg[:, tap, :], rhs=gpad[cc][:, t * 512 + tap:t * 512 + tap + 512],
                            start=(tap == 0), stop=(tap == 30)),
                            reads=[("scr", "Dg", cc % 2, tap), gkey[cc]], writes=[("ps", bk)])
                    P.add("act", lambda e, cc=cc, t=t, bk=bk: e.activation(
                        out=bigA[:, cc, TT(t)], in_=ps[bk][:], func=AF.Identity, bias=vcol(l, V_DWB, cc)),
                        reads=[("ps", bk), ("vecs",)], writes=[("bigA", cc, t)])
            P.fence("scr")
            sqb = scr[:, 0:1024].bitcast(BF16).rearrange("p (k n) -> p k n", k=4)
            xc = scr[:, 1024:3072].rearrange("p (k n) -> p k n", k=4)
            rstd = scr[:, 3072:3584]
            for t in range(4):
                bm = rr8()
                for cc in range(4):
                    P.add("pe", lambda e, cc=cc, bm=bm, t=t: e.matmul(ps[bm][:], lhsT=ones_c[:], rhs=bigA[:, cc, TT(t)],
                                                                      start=(cc == 0), stop=(cc == 3)),
                          reads=[("bigA", cc, t), ("ones",)], writes=[("ps", bm)])
                for cc in range(4):
                    P.add("dve", lambda e, cc=cc, t=t, bm=bm: e.tensor_tensor(
                        out=xc[:, cc, :], in0=bigA[:, cc, TT(t)], in1=ps[bm][:], op=ALU.subtract),
                        reads=[("bigA", cc, t), ("ps", bm)], writes=[("scr", "xc", cc)])
                for cc in range(4):
                    P.add("act", lambda e, cc=cc: e.activation(out=sqb[:, cc, :], in_=xc[:, cc, :], func=AF.Square),
                          reads=[("scr", "xc", cc)], writes=[("scr", "xb", cc)])
                bv = rr8()
                for cc in range(4):
                    P.add("pe", lambda e, cc=cc, bv=bv: e.matmul(ps[bv][:], lhsT=ones_c[:], rhs=sqb[:, cc, :],
                                                                 start=(cc == 0), stop=(cc == 3)),
                          reads=[("scr", "xb", cc), ("ones",)], writes=[("ps", bv)])
                P.add("act", lambda e, bv=bv: e.activation(out=rstd, in_=ps[bv][:], func=AF.Sqrt, bias=eps_c[:, 0:1]),
                      reads=[("ps", bv), ("ones",)], writes=[("scr", "rstd")])
                P.add("dve", lambda e: e.reciprocal(out=rstd, in_=rstd),
                      reads=[("scr", "rstd")], writes=[("scr", "rstd")])
                for cc in range(4):
                    P.add("dve", lambda e, cc=cc: e.scalar_tensor_tensor(
                        out=xc[:, cc, :], in0=xc[:, cc, :], scalar=vcol(l, V_LNG, cc), in1=rstd,
                        op0=ALU.mult, op1=ALU.mult),
                        reads=[("scr", "xc", cc), ("scr", "rstd"), ("vecs",)], writes=[("scr", "xc", cc)])
                    P.add("act", lambda e, cc=cc, t=t: e.activation(
                        out=bigA[:, cc, TT(t)], in_=xc[:, cc, :], func=AF.Silu, bias=vcol(l, V_LNB, cc)),
                        reads=[("scr", "xc", cc), ("vecs",)], writes=[("bigA", cc, t)])
            dump("c", bigA[:].rearrange("p a n -> p (a n)"), [("bigA", cc, t) for cc in range(4) for t in range(4)])
            P.fence("bigB")
            P.fence("bigC")

        def sb_attention(l):
            P.fence("scr")
            P.fence("bigB")
            qT = scr[:, 0:1024].bitcast(BF16)
            kT = scr[:, 1024:2048].bitcast(BF16)
            vv = scr[:, 2048:3072].bitcast(BF16).rearrange("p (t n) -> p t n", t=16)
            Eb = [scr[:, 3072:3584], scr[:, 3584:4096], scr[:, 6912:7424]]
            Gb = [scr[:, 4096:4608], scr[:, 4608:5120]]
            SPb = [scr[:, 5120:5376].bitcast(BF16), scr[:, 5376:5632].bitcast(BF16)]
            SSb = [scr[:, 5632:5888].bitcast(BF16), scr[:, 5888:6144].bitcast(BF16), scr[:, 6656:6912].bitcast(BF16)]
            Wb = [scr[:, 6144:6400].bitcast(BF16), scr[:, 6400:6656].bitcast(BF16)]
            cnt = {"e": 0, "ss": 0}
            for hp in range(4):
                keys, (wq, wk) = wload([wview(Win, l, 0, D, OFF_SBQ + hp * 128, 128),
                                        wview(Win, l, 0, D, OFF_SBK + hp * 128, 128)])
                keys2, (wv,) = wload([wview(Win, l, 0, D, OFF_SBV + hp * 128, 128)])
                keys = keys + keys2
                proj_fm(keys[0], wq, lambda t, b: P.add(
                    "act", lambda e, t=t, b=b: e.copy(out=qT[:, TT(t)], in_=ps[b][:]),
                    reads=[("ps", b)], writes=[("scr", "q", t)]))
                proj_fm(keys[1], wk, lambda t, b: P.add(
                    "dve", lambda e, t=t, b=b: e.tensor_copy(out=kT[:, TT(t)], in_=ps[b][:]),
                    reads=[("ps", b)], writes=[("scr", "k", t)]))
                for g in range(4):
                    b = rr8()
                    for ti in range(4):
                        t16 = g * 4 + ti
                        for kc in range(8):
                            P.add("pe", lambda e, kc=kc, t16=t16, ti=ti, b=b, wv=wv: e.matmul(
                                ps[b][:, ti * 128:(ti + 1) * 128], lhsT=xn[:, kc, t16 * 128:(t16 + 1) * 128],
                                rhs=wv[:, kc, :], start=(kc == 0), stop=(kc == 7)),
                                reads=[keys[2], ("xn", kc, t16 // 4)], writes=[("ps", b)])
                    P.add("act", lambda e, g=g, b=b: e.copy(
                        out=vv[:, g * 4:(g + 1) * 4, :], in_=ps[b][:].rearrange("p (t n) -> p t n", t=4)),
                        reads=[("ps", b)], writes=[("scr", "v", g)])
                pairs = []
                for j in range(2):
                    for qt in range(4):
                        nkt = (qt + 1) * 4
                        bo = rrO()
                        for idx, kt in enumerate(reversed(range(nkt))):
                            k0, q0 = kt * 128, qt * 512
                            diag = k0 >= q0
                            c0 = k0 - q0 if diag else 0
                            pairs.append(dict(j=j, qt=qt, kt=kt, k0=k0, q0=q0, diag=diag, c0=c0, first=(idx == 0),
                                              last=(kt == 0), bo=bo, pj=slice(64 * j, 64 * j + 64)))
                state = {"prev_ss": None}

                def stA(p, n):
                    i = n % 2
                    c0, q0, k0, pj, kt, qt = p["c0"], p["q0"], p["k0"], p["pj"], p["kt"], p["qt"]
                    cols = slice(c0, 512)
                    qcols = slice(q0 + c0, q0 + 512)
                    if p["first"]:
                        state["prev_ss"] = None
                    bs = rr6()
                    P.add("pe", lambda e: e.matmul(
                        ps[bs][:, cols], lhsT=kT[pj, k0:k0 + 128], rhs=qT[pj, qcols], start=True, stop=not p["diag"]),
                        reads=[("scr", "k", kt // 4), ("scr", "q", qt)], writes=[("ps", bs)])
                    if p["diag"]:
                        P.add("pe", lambda e: e.matmul(ps[bs][:, c0:c0 + 128], lhsT=IDN, rhs=NEGM, start=False, stop=True),
                              reads=[("cb",)], writes=[("ps", bs)])
                    ie = n % 3
                    E, SP = Eb[ie], SPb[i]
                    P.add("act", lambda e: e.activation(out=E[:, cols], in_=ps[bs][:, cols], func=AF.Exp, scale=0.125),
                          reads=[("ps", bs)], writes=[("scr", "E", ie)])
                    P.add("act", lambda e: e.activation(out=SP[:, cols], in_=E[:, cols], func=AF.Ln, bias=1.0),
                          reads=[("scr", "E", ie)], writes=[("scr", "SP", i)])
                    p["pss"] = state["prev_ss"]
                    if kt > 0:
                        si = n % 3
                        SSn = SSb[si]
                        nk = ("scr", "SS", si)
                        if state["prev_ss"] is None:
                            P.add("pool", lambda e: e.tensor_copy(out=SSn[:, cols], in_=SP[:, cols]),
                                  reads=[("scr", "SP", i)], writes=[nk])
                        else:
                            pss, pk, pc0 = state["prev_ss"]
                            if pc0 > c0:
                                P.add("pool", lambda e: e.tensor_copy(out=SSn[:, c0:pc0], in_=SP[:, c0:pc0]),
                                      reads=[("scr", "SP", i)], writes=[nk])
                            P.add("pool", lambda e: e.tensor_tensor(
                                out=SSn[:, pc0:512], in0=SP[:, pc0:512], in1=pss[:, pc0:512], op=ALU.add),
                                reads=[("scr", "SP", i), pk], writes=[nk])
                        state["prev_ss"] = (SSn, nk, c0)

                def stB(p, n):
                    i = n % 2
                    c0 = p["c0"]
                    cols = slice(c0, 512)
                    ie = n % 3
                    E, SP, G, Wt = Eb[ie], SPb[i], Gb[i], Wb[i]
                    bc = rr6()
                    pss = p["pss"]
                    P.add("pe", lambda e: e.matmul(ps[bc][:, cols], lhsT=TRI, rhs=SP[:, cols], start=True,
                                                   stop=(pss is None)),
                          reads=[("scr", "SP", i), ("cb",)], writes=[("ps", bc)])
                    if pss is not None:
                        ssb, pk, pc0 = pss
                        P.add("pe", lambda e: e.matmul(ps[bc][:, pc0:512], lhsT=ones1[:], rhs=ssb[:, pc0:512],
                                                       start=False, stop=True),
                              reads=[pk, ("ones",)], writes=[("ps", bc)])
                    dummies(NDUM["sb"])
                    P.add("act", lambda e: e.activation(out=G[:, cols], in_=ps[bc][:, cols], func=AF.Exp, scale=-1.0),
                          reads=[("ps", bc)], writes=[("scr", "G", i)])
                    P.add("dve", lambda e: e.tensor_tensor(out=Wt[:, cols], in0=E[:, cols], in1=G[:, cols], op=ALU.mult),
                          reads=[("scr", "E", ie), ("scr", "G", i)], writes=[("scr", "W", i)])

                def stC(p, n, hp=hp):
                    i = n % 2
                    c0, bo, pj, kt, qt = p["c0"], p["bo"], p["pj"], p["kt"], p["qt"]
                    cols = slice(c0, 512)
                    Wt = Wb[i]
                    if p["first"]:
                        P.add("pe", lambda e: e.matmul(ps[bo][0:64, :], lhsT=zer[:, 0:64], rhs=cb[:, 0:512],
                                                       start=True, stop=False),
                              reads=[("ones",), ("cb",)], writes=[("ps", bo)])
                    P.add("pe", lambda e: e.matmul(ps[bo][0:64, cols], lhsT=vv[:, kt, pj], rhs=Wt[:, cols],
                                                   start=False, stop=p["last"]),
                          reads=[("scr", "W", i), ("scr", "v", kt // 4)], writes=[("ps", bo)])
                    if p["last"]:
                        P.add("dve", lambda e: e.tensor_copy(out=bigB[pj, hp, TT(qt)], in_=ps[bo][0:64, :]),
                              reads=[("ps", bo)], writes=[("bigB", hp, qt, pj.start)])

                NPR = len(pairs)
                for n in range(NPR + 2):
                    if n < NPR:
                        stA(pairs[n], n)
                    if 1 <= n <= NPR:
                        stB(pairs[n - 1], n - 1)
                    if n >= 2:
                        stC(pairs[n - 2], n - 2)

        def moba_attention(l):
            P.fence("scr")
            P.fence("bigC")
            P.add("dve", lambda e: e.memset(vext, 1.0), writes=[("scr", "vext")])
            for j in range(2):
                P.add("pool", lambda e, j=j: e.dma_start(out=kaug[64:75, j, :], in_=kst_d[64:75, :]),
                      writes=[("scr", "kst", j)], dma="c1")
            ksf = scr[0:64, 0:16].rearrange("p (j n) -> p j n", j=2)
            gm = scr[:, 64:128].rearrange("p (g n) -> p g n", g=8)
            cmp_ = scr[:, 128:640].rearrange("p (g n m) -> p g n m", g=8, n=8)
            cntt = scr[:, 640:704].rearrange("p (g n) -> p g n", g=8)
            t1 = scr[:, 704:768].rearrange("p (g n) -> p g n", g=8)
            rden = scr[0:64, 768:1280]
            Pm = [scr[:, 1280:1536].bitcast(BF16), scr[:, 1536:1792].bitcast(BF16), scr[:, 1792:2048].bitcast(BF16)]
            cnt = {"p": 0}
            GM = cst[:, C_GM:C_GM + 128].rearrange("p (o j n) -> p o j n", o=8, j=2)
            LL = cst[:, C_L:C_L + 64].rearrange("p (o n) -> p o n", o=8)
            for hp in range(4):
                keys, (wq, wk) = wload([wview(Win, l, 0, D, OFF_MBQ + hp * 128, 128),
                                        wview(Win, l, 0, D, OFF_MBK + hp * 128, 128)])
                keys2, (wv,) = wload([wview(Win, l, 0, D, OFF_MBV + hp * 128, 128)])
                keys = keys + keys2

                def evq(t, b):
                    P.add("act", lambda e, t=t, b=b: e.copy(out=qaug[0:64, 0, TT(t)], in_=ps[b][0:64, :]),
                          reads=[("ps", b)], writes=[("scr", "qaug", 0, t)])
                    P.add("dve", lambda e, t=t, b=b: e.tensor_copy(out=qaug[0:64, 1, TT(t)], in_=ps[b][64:128, :]),
                          reads=[("ps", b)], writes=[("scr", "qaug", 1, t)])
                proj_fm(keys[0], wq, evq)

                def evk(t, b):
                    P.add("act", lambda e, t=t, b=b: e.copy(out=kaug[0:64, 0, TT(t)], in_=ps[b][0:64, :]),
                          reads=[("ps", b)], writes=[("scr", "kaug", 0, t)])
                    P.add("dve", lambda e, t=t, b=b: e.tensor_copy(out=kaug[0:64, 1, TT(t)], in_=ps[b][64:128, :]),
                          reads=[("ps", b)], writes=[("scr", "kaug", 1, t)])
                    for j in range(2):
                        P.add("dve", lambda e, t=t, b=b, j=j: e.tensor_reduce(
                            out=ksf[:, j, 2 * t:2 * t + 2], in_=ps[b][64 * j:64 * j + 64, :].rearrange("p (a n) -> p a n", a=2),
                            axis=AX.X, op=ALU.add),
                            reads=[("ps", b)], writes=[("scr", "ksf", j, t)])
                proj_fm(keys[1], wk, evk)
                P.add("dve", lambda e: e.tensor_copy(out=ksumb[:], in_=ksf),
                      reads=[("scr", "ksf", j, t) for j in range(2) for t in range(4)], writes=[("ksumb",)])
                for g in range(4):
                    b = rr8()
                    for ti in range(4):
                        t16 = g * 4 + ti
                        for kc in range(8):
                            P.add("pe", lambda e, kc=kc, t16=t16, ti=ti, b=b, wv=wv: e.matmul(
                                ps[b][:, ti * 128:(ti + 1) * 128], lhsT=xn[:, kc, t16 * 128:(t16 + 1) * 128],
                                rhs=wv[:, kc, :], start=(kc == 0), stop=(kc == 7)),
                                reads=[keys[2], ("xn", kc, t16 // 4)], writes=[("ps", b)])
                    P.add("act", lambda e, g=g, b=b: e.copy(
                        out=vext[:, g * 4:(g + 1) * 4, :, 0:64],
                        in_=ps[b][:].rearrange("p (t j n) -> p t j n", t=4, j=2)),
                        reads=[("ps", b), ("scr", "vext")], writes=[("scr", "vext", g)])
                for qt in range(4):
                    bg = rr8()
                    for ti in range(4):
                        t16 = qt * 4 + ti
                        for j in range(2):
                            g8 = ti * 2 + j
                            P.add("pe", lambda e, bg=bg, g8=g8, j=j, t16=t16: e.matmul(
                                ps[bg][:, g8 * 8:(g8 + 1) * 8], lhsT=qaug[0:64, j, t16 * 128:(t16 + 1) * 128],
                                rhs=ksumb[:, j, :], start=True, stop=True),
                                reads=[("scr", "qaug", j, qt), ("ksumb",)], writes=[("ps", bg)])
                    for ti in range(4):
                        own = (qt * 4 + ti) // 2
                        P.add("dve", lambda e, bg=bg, ti=ti, own=own: e.tensor_tensor(
                            out=gm[:, 2 * ti:2 * ti + 2, :],
                            in0=ps[bg][:, 16 * ti:16 * ti + 16].rearrange("p (j n) -> p j n", j=2),
                            in1=GM[:, own, :, :], op=ALU.add),
                            reads=[("ps", bg), ("cst",)], writes=[("scr", "gm")])
                    gap = [list(a) for a in gm.ap]
                    gm_m = bass.AP(gm.tensor, gm.offset, [gap[0], gap[1], [0, 8], gap[2]])
                    gm_n = bass.AP(gm.tensor, gm.offset, [gap[0], gap[1], gap[2], [0, 8]])
                    P.add("dve", lambda e, gm_m=gm_m, gm_n=gm_n: e.tensor_tensor(
                        out=cmp_, in0=gm_m, in1=gm_n, op=ALU.is_gt),
                        reads=[("scr", "gm")], writes=[("scr", "cmp")])
                    P.add("dve", lambda e: e.tensor_reduce(out=cntt, in_=cmp_, axis=AX.X, op=ALU.add),
                          reads=[("scr", "cmp")], writes=[("scr", "cnt")])
                    P.add("dve", lambda e: e.tensor_scalar(out=t1, in0=cntt, scalar1=2.5, scalar2=BIG,
                                                           op0=ALU.is_lt, op1=ALU.mult),
                          reads=[("scr", "cnt")], writes=[("scr", "t1")])
                    for ti in range(4):
                        own = (qt * 4 + ti) // 2
                        for j in range(2):
                            hh = 2 * hp + j
                            P.add("dve", lambda e, ti=ti, j=j, hh=hh, own=own: e.scalar_tensor_tensor(
                                out=selT[:, hh, ti, 64:72], in0=t1[:, 2 * ti + j, :], scalar=-BIG, in1=LL[:, own, :],
                                op0=ALU.add, op1=ALU.max),
                                reads=[("scr", "t1"), ("cst",), ("selst",)], writes=[("selT", hh, ti)])
                    for j in range(2):
                        hh = 2 * hp + j
                        bt = rr8()
                        for ti in range(4):
                            P.add("pe", lambda e, bt=bt, ti=ti, hh=hh: e.matmul(
                                ps[bt][0:80, ti * 128:(ti + 1) * 128], lhsT=selT[:, hh, ti, :], rhs=IDN,
                                start=True, stop=True),
                                reads=[("selT", hh, ti), ("cb",)], writes=[("ps", bt)])
                        P.add("act", lambda e, bt=bt, j=j, qt=qt: e.copy(out=qaug[64:75, j, TT(qt)], in_=ps[bt][64:75, :]),
                              reads=[("ps", bt)], writes=[("scr", "qst", j, qt)])
                pairs = []
                for j in range(2):
                    for qt in range(4):
                        nkt = (qt + 1) * 4
                        bo = rrO()
                        for kt in range(nkt):
                            k0, q0 = kt * 128, qt * 512
                            diag = k0 >= q0
                            c0 = k0 - q0 if diag else 0
                            pairs.append(dict(j=j, qt=qt, kt=kt, k0=k0, q0=q0, diag=diag, c0=c0, first=(kt == 0),
                                              last=(kt == nkt - 1), bo=bo, hh=2 * hp + j))

                def mA(p, n):
                    c0, q0, k0, j, kt, qt = p["c0"], p["q0"], p["k0"], p["j"], p["kt"], p["qt"]
                    cols = slice(c0, 512)
                    qcols = slice(q0 + c0, q0 + 512)
                    bs = rr6()
                    p["bs"] = bs
                    P.add("pe", lambda e: e.matmul(
                        ps[bs][:, cols], lhsT=kaug[0:75, j, k0:k0 + 128], rhs=qaug[0:75, j, qcols],
                        start=True, stop=not p["diag"]),
                        reads=[("scr", "kaug", j, kt // 4), ("scr", "kst", j), ("scr", "qaug", j, qt), ("scr", "qst", j, qt)],
                        writes=[("ps", bs)])
                    if p["diag"]:
                        P.add("pe", lambda e: e.matmul(ps[bs][:, c0:c0 + 128], lhsT=IDN, rhs=NEGM2, start=False, stop=True),
                              reads=[("cb",)], writes=[("ps", bs)])

                def mB(p, n):
                    i = n % 3
                    c0, bs = p["c0"], p["bs"]
                    cols = slice(c0, 512)
                    pm = Pm[i]
                    biasc = float(-SLOPES[p["hh"]] * (p["q0"] - p["k0"]))
                    P.add("act", lambda e: e.activation(
                        out=pm[:, cols], in_=ps[bs][:, cols], func=AF.Exp, scale=0.125, bias=biasc),
                        reads=[("ps", bs)], writes=[("scr", "Pm", i)])

                def mC(p, n, hp=hp):
                    i = n % 3
                    c0, bo, j, kt, qt = p["c0"], p["bo"], p["j"], p["kt"], p["qt"]
                    cols = slice(c0, 512)
                    pm = Pm[i]
                    pj = slice(64 * j, 64 * j + 64)
                    P.add("pe", lambda e: e.matmul(ps[bo][:, cols], lhsT=vext[:, kt, j, :], rhs=pm[:, cols],
                                                   start=p["first"], stop=p["last"]),
                          reads=[("scr", "Pm", i), ("scr", "vext", kt // 4), ("scr", "vext")], writes=[("ps", bo)])
                    if p["last"]:
                        P.add("dve", lambda e: e.reciprocal(out=rden, in_=ps[bo][64:128, :]),
                              reads=[("ps", bo)], writes=[("scr", "rden")])
                        P.add("dve", lambda e: e.tensor_tensor(
                            out=bigC[pj, hp, TT(qt)], in0=ps[bo][0:64, :], in1=rden, op=ALU.mult),
                            reads=[("ps", bo), ("scr", "rden")], writes=[("bigC", hp, qt, pj.start)])

                NPR = len(pairs)
                for n in range(NPR + 2):
                    if n < NPR:
                        mA(pairs[n], n)
                    if 1 <= n <= NPR:
                        mB(pairs[n - 1], n - 1)
                    if n >= 2:
                        mC(pairs[n - 2], n - 2)

        def mix_out(l, have):
            P.fence("scr")
            mixed = scr[:, 0:4096].bitcast(BF16).rearrange("p (k n) -> p k n", k=8)
            gtb = [scr[:, 4096 + 512 * i:4608 + 512 * i] for i in range(2)]
            ttb = [[scr[:, 5120 + 512 * (3 * a + i):5632 + 512 * (3 * a + i)] for i in range(3)] for a in range(2)]
            gcnt = {"i": 0}
            srcs = [("sb", bigB, WA, "bigB"), ("mb", bigC, WB, "bigC"), ("conv", bigA, WC, "bigA")]
            for half in range(2):
                for dc in range(8):
                    kg1, gw1 = wload([wview(Win, l, 0, D, OFF_GATE + br * D + dc * 128, 128) for br in range(2)])
                    kg2, gw2 = wload([wview(Win, l, 0, D, OFF_GATE + 2 * D + dc * 128, 128),
                                      wview(WA, l, 0, 512, dc * 128, 128), wview(WB, l, 0, 512, dc * 128, 128)])
                    kg3, gw3 = wload([wview(WC, l, 0, 512, dc * 128, 128)])
                    keysg, gw = kg1 + kg2[0:1], gw1 + gw2[0:1]
                    keysy, yw = kg2[1:3] + kg3, gw2[1:3] + gw3
                    for t2 in range(2):
                        t = half * 2 + t2
                        tt_ = ttb[t2]
                        for br, (nm, ob, W_, blk) in enumerate(srcs):
                            if nm not in have:
                                continue
                            bgate = rr8()
                            for kc in range(8):
                                P.add("pe", lambda e, kc=kc, t=t, bgate=bgate, gwb=gw[br]: e.matmul(
                                    ps[bgate][:], lhsT=gwb[:, kc, :], rhs=xn[:, kc, TT(t)],
                                    start=(kc == 0), stop=(kc == 7)),
                                    reads=[keysg[br], ("xn", kc, t)], writes=[("ps", bgate)])
                            by = rr8()
                            for kc in range(4):
                                if nm == "conv":
                                    rk = [("bigA", kc, t)]
                                else:
                                    rk = [(blk, kc, t, 0), (blk, kc, t, 64)]
                                P.add("pe", lambda e, kc=kc, t=t, by=by, ywb=yw[br], ob=ob: e.matmul(
                                    ps[by][:], lhsT=ywb[:, kc, :], rhs=ob[:, kc, TT(t)],
                                    start=(kc == 0), stop=(kc == 3)),
                                    reads=[keysy[br]] + rk, writes=[("ps", by)])
                            gi = gcnt["i"] % 2
                            gcnt["i"] += 1
                            P.add("act", lambda e, br=br, bgate=bgate, dc=dc, gi=gi: e.activation(
                                out=gtb[gi], in_=ps[bgate][:], func=AF.Sigmoid, bias=vcol(l, V_GB, br * 8 + dc)),
                                reads=[("ps", bgate), ("vecs",)], writes=[("scr", "gt", gi)])
                            P.add("dve", lambda e, br=br, by=by, gi=gi, tt_=tt_: e.tensor_tensor(
                                out=tt_[br], in0=gtb[gi], in1=ps[by][:], op=ALU.mult),
                                reads=[("scr", "gt", gi), ("ps", by)], writes=[("scr", "tt", t2, br)])
                        live = [br for br, s_ in enumerate(srcs) if s_[0] in have]
                        mo = mixed[:, dc, t2 * 512:(t2 + 1) * 512]
                        mk_ = ("scr", "mixed", dc, t2)
                        if len(live) == 1:
                            P.add("dve", lambda e, mo=mo, a=live[0], tt_=tt_: e.tensor_copy(out=mo, in_=tt_[a]),
                                  reads=[("scr", "tt", t2, live[0])], writes=[mk_])
                        elif len(live) == 2:
                            P.add("dve", lambda e, mo=mo, a=live[0], b_=live[1], tt_=tt_: e.tensor_tensor(
                                out=mo, in0=tt_[a], in1=tt_[b_], op=ALU.add),
                                reads=[("scr", "tt", t2, live[0]), ("scr", "tt", t2, live[1])], writes=[mk_])
                        else:
                            P.add("dve", lambda e, tt_=tt_: e.tensor_tensor(out=tt_[0], in0=tt_[0], in1=tt_[1], op=ALU.add),
                                  reads=[("scr", "tt", t2, 0), ("scr", "tt", t2, 1)], writes=[("scr", "tt", t2, 0)])
                            P.add("dve", lambda e, mo=mo, tt_=tt_: e.tensor_tensor(out=mo, in0=tt_[0], in1=tt_[2], op=ALU.add),
                                  reads=[("scr", "tt", t2, 0), ("scr", "tt", t2, 2)], writes=[mk_])
                dump("mixed", scr[:, 0:4096].bitcast(BF16), [("scr", "mixed", dc_, t_) for dc_ in range(8) for t_ in range(2)])
                for d0 in range(0, 8, 2):
                    keys, (wo,) = wload([wview(WO, l, 0, D, d0 * 128, 256)])
                    for di in range(2):
                        dc = d0 + di
                        for t2 in range(2):
                            t = half * 2 + t2
                            b = rr8()
                            for kc in range(8):
                                P.add("pe", lambda e, kc=kc, t2=t2, b=b, di=di, wo=wo: e.matmul(
                                    ps[b][:], lhsT=wo[:, kc, di * 128:(di + 1) * 128],
                                    rhs=mixed[:, kc, t2 * 512:(t2 + 1) * 512], start=(kc == 0), stop=(kc == 7)),
                                    reads=[keys[0], ("scr", "mixed", kc, t2)], writes=[("ps", b)])
                            P.add("dve", lambda e, b=b, dc=dc, t=t: e.tensor_tensor(
                                out=h[:, dc, TT(t)], in0=ps[b][:], in1=h[:, dc, TT(t)], op=ALU.add),
                                reads=[("ps", b)], writes=[("h", dc, t)])

        outs = []
        for s_i in range(nseq):
            for dc in range(8):
                P.add("sp", lambda e, dc=dc, s_i=s_i: e.dma_start(out=h[:, dc, :], in_=xT[s_i, dc * 128:(dc + 1) * 128, :]),
                      writes=[("h", dc, t) for t in range(4)], dma="x%d" % dc)
            for l in range(depth):
                if "ffn1" in stages:
                    ffn(l, W1i, W1o, V_FFN1N)
                if "mix" in stages:
                    rmsnorm(l, V_MIXN)
                    if "conv" in mix_parts:
                        conv_branch(l)
                    if "sb" in mix_parts:
                        sb_attention(l)
                    if "mb" in mix_parts:
                        moba_attention(l)
                    mix_out(l, mix_parts)
                if "ffn2" in stages:
                    ffn(l, W2i, W2o, V_FFN2N)
            if "final" in stages:
                rmsnorm(None, depth * NVEC_L, out_f32=h)
            for dc in range(8):
                outs.append(P.add("sp", lambda e, dc=dc, s_i=s_i: e.dma_start(
                    out=outT[s_i, dc * 128:(dc + 1) * 128, :], in_=h[:, dc, :]),
                    reads=[("h", dc, t) for t in range(4)], dma="o%d" % dc))
        P.emit(nc, final_waits=outs[-8:] + dbg_ops)
    return nc


_CACHE = {}


def kernel(**inputs):
    x = np.asarray(inputs["x"], np.float32)
    B = x.shape[0]
    per = B // NCORES
    cst, kst, sel = host_consts()
    vecs = host_vecs(inputs, DEPTH_FULL)
    nc = build(per, DEPTH_FULL)
    shared = {k: np.ascontiguousarray(np.asarray(inputs[k], np.float32)) for k in
              ("ffn1_w_in", "ffn1_w_out", "ffn2_w_in", "ffn2_w_out", "w_in", "sb_w_out", "mb_w_out",
               "conv_w_out", "w_o")}
    shared.update({"vecs": vecs, "cst": cst, "kst": kst, "selst": sel})
    in_maps = []
    for c in range(NCORES):
        xs = x[c * per:(c + 1) * per]
        m = dict(shared)
        m["xT"] = np.ascontiguousarray(xs.transpose(0, 2, 1))
        in_maps.append(m)
    res = run_bass_kernel_spmd(nc, in_maps, core_ids=list(range(NCORES)))
    out = np.empty((B, S, D), np.float32)
    for c in range(NCORES):
        out[c * per:(c + 1) * per] = res.results[c]["outT"].transpose(0, 2, 1)
    return out
```

```python
import contextlib
import numpy as np
import concourse.bass as bass
import concourse.mybir as mybir
from concourse.bass_utils import run_bass_kernel_spmd

F32 = mybir.dt.float32
BF16 = mybir.dt.bfloat16
AF = mybir.ActivationFunctionType
ALU = mybir.AluOpType
AX = mybir.AxisListType

D = 1024
S = 2048
DFF = 2816
NFC = 22
DEPTH_FULL = 2
NCORES = 8
SEQ_PER_CORE = 4
OFF_SBQ, OFF_SBK, OFF_SBV = 0, 512, 1024
OFF_MBQ, OFF_MBK, OFF_MBV = 1536, 2048, 2560
OFF_CONV = 3072
OFF_GATE = 4096
IN_COLS = 7168
EPS = 1e-6
BIG = 29952.0
SLOPES = [2.0 ** (-(h + 1)) for h in range(8)]
ENGS = ("pe", "act", "dve", "pool", "sp")


class Op:
    __slots__ = ("eng", "fn", "pos", "sig", "waits", "dma", "val")

    def __init__(self, eng, fn, dma=None):
        self.eng, self.fn, self.dma = eng, fn, dma
        self.pos, self.sig, self.waits, self.val = -1, False, [], 0


class Prog:
    def __init__(self):
        self.streams = {e: [] for e in ENGS}
        self.last_w, self.readers = {}, {}
        self.seen = {e: {} for e in ENGS}
        self.dma_cnt = {}
        self.fences = {}

    def _need(self, x, y, raw):
        if y is None or y is x:
            return
        if y.dma is not None:
            key = ("d", y.dma)
            val = self.dma_cnt[y.dma] - (16 if x.dma == y.dma else 0)
            if self.seen[x.eng].get(key, 0) >= val:
                return
            self.seen[x.eng][key] = val
            x.waits.append(("d", y.dma, val))
            return
        if y.eng == x.eng and x.dma is None:
            if x.eng == "pe":
                return
        key = ("e", y.eng)
        if self.seen[x.eng].get(key, -1) >= y.pos:
            return
        self.seen[x.eng][key] = y.pos
        y.sig = True
        x.waits.append(("e", y.eng, y))

    def fence(self, block):
        ops = {}
        for k in [k for k in self.last_w if k[0] == block]:
            o = self.last_w.pop(k)
            ops[id(o)] = o
        for k in [k for k in self.readers if k[0] == block]:
            for o in self.readers.pop(k):
                ops[id(o)] = o
        best = {}
        for o in ops.values():
            kk = (o.eng, o.dma)
            rank = o.val if o.dma is not None else o.pos
            if kk not in best or rank > best[kk][0]:
                best[kk] = (rank, o)
        if best:
            self.fences[block] = [v[1] for v in best.values()]

    def add(self, eng, fn, reads=(), writes=(), dma=None):
        x = Op(eng, fn, dma)
        if dma is not None:
            self.dma_cnt[dma] = self.dma_cnt.get(dma, 0) + 16
            x.val = self.dma_cnt[dma]
        reads = list(reads)
        writes = list(writes)
        for r in list(reads):
            if r[0] == "ps":
                reads.remove(r)
                writes.append(r)
        for k in reads + writes:
            if k not in self.last_w and k[0] in self.fences:
                for o in self.fences[k[0]]:
                    self._need(x, o, True)
        for r in reads:
            self._need(x, self.last_w.get(r), True)
        for w in writes:
            self._need(x, self.last_w.get(w), True)
            for rd in self.readers.get(w, ()):
                self._need(x, rd, False)
        x.pos = len(self.streams[eng])
        self.streams[eng].append(x)
        for r in reads:
            self.readers.setdefault(r, []).append(x)
        for w in writes:
            self.last_w[w] = x
            self.readers[w] = []
        return x

    def emit(self, nc, final_waits=()):
        with contextlib.ExitStack() as es:
            esem = {e: es.enter_context(nc.semaphore("s_" + e)) for e in ENGS}
            dsem = {n: es.enter_context(nc.semaphore("d_" + n)) for n in self.dma_cnt}
            block = es.enter_context(nc.Block())
            for e in ENGS:
                c = 0
                for op in self.streams[e]:
                    if op.dma is None:
                        if op.sig:
                            c += 1
                        op.val = c
            hooks = {"pe": block.tensor, "act": block.scalar, "dve": block.vector,
                     "pool": block.gpsimd, "sp": block.sync}

            def mk(e):
                def body(eng):
                    for op in self.streams[e]:
                        for w in op.waits:
                            if w[0] == "d":
                                eng.wait_ge(dsem[w[1]], w[2])
                            else:
                                eng.wait_ge(esem[w[1]], w[2].val)
                        ins = op.fn(eng)
                        if op.dma is not None:
                            ins.then_inc(dsem[op.dma], 16)
                        elif op.sig:
                            ins.then_inc(esem[e], 1)
                    if e == "sp":
                        for op in final_waits:
                            eng.wait_ge(dsem[op.dma], op.val)
                return body
            for e in ENGS:
                hooks[e](mk(e))


NVEC_L = 8 + 8 + 8 + 24 + 4 + 4 + 4 + 124
V_FFN1N, V_MIXN, V_FFN2N, V_GB, V_DWB, V_LNG, V_LNB, V_DW = 0, 8, 16, 24, 48, 52, 56, 60
C_TRI, C_MLT, C_MLE, C_ID, C_GM, C_L = 0, 128, 256, 384, 512, 640
NCST = 704


def host_consts():
    c = np.zeros((128, NCST), np.float32)
    j = np.arange(128)[:, None]
    s = np.arange(128)[None, :]
    c[:, C_TRI:C_TRI + 128] = (j >= s)
    c[:, C_MLT:C_MLT + 128] = np.where(j >= s, -BIG, 0.0)
    c[:, C_MLE:C_MLE + 128] = np.where(j > s, -BIG, 0.0)
    c[:, C_ID:C_ID + 128] = (j == s)
    own = np.arange(8)[:, None]
    n = np.arange(8)[None, :]
    gm = np.where(n < own, 0.0, -BIG).astype(np.float32)
    ll = np.where(n < own, -BIG, 0.0).astype(np.float32)
    c[:, C_GM:C_GM + 128] = np.repeat(gm[:, None, :], 2, axis=1).reshape(1, 128)
    c[:, C_L:C_L + 64] = ll.reshape(1, 64)
    kst = np.zeros((128, S), np.float32)
    pos = np.arange(S)
    for nn in range(8):
        kst[64 + nn] = (pos // 256 == nn)
    kst[72] = 1.0
    kst[73] = 1.0
    kst[74] = pos % 128
    sel = np.zeros((128, 8, 4, 80), np.float32)
    q = np.arange(128)
    for h in range(8):
        for m in range(4):
            i = m * 128 + q
            sel[:, h, m, 72] = -8.0 * SLOPES[h] * (256 * (i // 256))
            sel[:, h, m, 73] = -8.0 * SLOPES[h] * (i % 256)
            sel[:, h, m, 74] = 8.0 * SLOPES[h]
    return c, kst, sel.reshape(128, 8 * 4 * 80)


def host_vecs(inp, depth):
    def col(v):
        return np.ascontiguousarray(np.asarray(v, np.float32).reshape(-1, 128).T)
    cols = []
    for l in range(depth):
        cols += [col(inp["ffn1_norm"][l]), col(inp["mix_norm"][l]), col(inp["ffn2_norm"][l]),
                 col(inp["gate_bias"][l]), col(inp["conv_dw_bias"][l]), col(inp["conv_ln_g"][l]),
                 col(inp["conv_ln_b"][l])]
        dw = np.asarray(inp["conv_dw"][l], np.float32).reshape(31, 512)
        cols.append(np.ascontiguousarray(dw.reshape(31, 4, 128).transpose(2, 0, 1).reshape(128, 124)))
    cols.append(col(inp["final_norm"]))
    return np.ascontiguousarray(np.concatenate(cols, axis=1))


def build(nseq=SEQ_PER_CORE, depth=DEPTH_FULL, stages=("ffn1", "mix", "ffn2", "final"),
          mix_parts=("conv", "sb", "mb"), dbg=None):
    nc = bass.Bass("TRN2", target_bir_lowering=False)
    dram = {}

    def din(name, shape):
        dram[name] = nc.dram_tensor(name, list(shape), F32, kind="ExternalInput").ap()
        return dram[name]

    xT = din("xT", [nseq, D, S])
    W1i = din("ffn1_w_in", [depth, D, 2 * DFF])
    W1o = din("ffn1_w_out", [depth, DFF, D])
    W2i = din("ffn2_w_in", [depth, D, 2 * DFF])
    W2o = din("ffn2_w_out", [depth, DFF, D])
    Win = din("w_in", [depth, D, IN_COLS])
    WA = din("sb_w_out", [depth, 512, D])
    WB = din("mb_w_out", [depth, 512, D])
    WC = din("conv_w_out", [depth, 512, D])
    WO = din("w_o", [depth, D, D])
    NV = depth * NVEC_L + 8
    vecs_d = din("vecs", [128, NV])
    cst_d = din("cst", [128, NCST])
    kst_d = din("kst", [128, S])
    sel_d = din("selst", [128, 8 * 4 * 80])
    outT = nc.dram_tensor("outT", [nseq, D, S], F32, kind="ExternalOutput").ap()
    dbg_d = nc.dram_tensor("dbg", [128, 8192], F32, kind="ExternalOutput").ap() if dbg else None
    dbg_ops = []

    def dump(name, ap, keys):
        if dbg == name and not dbg_ops:
            n = ap.shape[1]
            dbg_ops.append(P.add("pool", lambda e: e.dma_start(out=dbg_d[:, 0:n], in_=ap), reads=keys, dma="dbg"))

    P = Prog()
    es = contextlib.ExitStack()
    with es:
        def sb(name, shape, dt):
            return es.enter_context(nc.sbuf_tensor(name, list(shape), dt))

        h = sb("h", [128, 8, S], F32)
        xn = sb("xn", [128, 8, S], BF16)
        bigA = sb("bigA", [128, 4, S], BF16)
        bigB = sb("bigB", [128, 4, S], BF16)
        bigC = sb("bigC", [128, 4, S], BF16)
        scr = sb("scr", [128, 8192], F32)
        NW = 5
        WSZ = 2048
        wsl = [sb("w%d" % i, [128, WSZ], BF16) for i in range(NW)]
        vecs = sb("vecs_sb", [128, NV], F32)
        cst = sb("cst_sb", [128, NCST], F32)
        cb = sb("cst_bf", [128, 512], BF16)
        ones_d = sb("ones_d", [128, 128], BF16)
        ones_c = sb("ones_c", [128, 128], BF16)
        ones1 = sb("ones1", [128, 128], BF16)
        zer = sb("zer", [128, 128], BF16)
        eps_c = sb("eps_c", [128, 1], F32)
        kaug = scr[:, 2048:4096].bitcast(BF16).rearrange("p (j n) -> p j n", j=2)
        qaug = scr[:, 4096:6144].bitcast(BF16).rearrange("p (j n) -> p j n", j=2)
        vext = scr[:, 6144:8192].bitcast(BF16).rearrange("p (t j n) -> p t j n", t=16, j=2)
        selT = sb("selT", [128, 8, 4, 80], BF16)
        ksumb = sb("ksumb", [64, 2, 8], BF16)
        ps = [es.enter_context(nc.psum_tensor("ps%d" % i, [128, 512], F32)) for i in range(8)]

        class RR:
            def __init__(self, ids):
                self.ids, self.i = list(ids), 0

            def __call__(self):
                b = self.ids[self.i % len(self.ids)]
                self.i += 1
                return b
        rr8 = RR(range(8))
        rr6 = RR(range(5))
        NDUM = {"sb": 3}

        def dummies(n):
            for _ in range(n):
                P.add("pe", lambda e: e.matmul(ps[5][:], lhsT=zer[:], rhs=cb[:, 0:512], start=True, stop=True),
                      reads=[("ones",), ("cb",)], writes=[("ps", 5)])
        rrO = RR([6, 7])
        wstate = {"i": 0}

        def wload(parts, eng="pool"):
            si = wstate["i"] % NW
            wstate["i"] += 1
            P.fence("w%d" % si)
            off = 0
            views, keys = [], []
            for pi, ap in enumerate(parts):
                K, n = ap.shape[1], ap.shape[2]
                v = wsl[si][:, off:off + K * n].rearrange("p (k n) -> p k n", k=K)
                key = ("w%d" % si, pi)
                P.add(eng, lambda e, v=v, ap=ap: e.dma_start(out=v, in_=ap), writes=[key], dma="w%d" % si)
                views.append(v)
                keys.append(key)
                off += K * n
            assert off <= WSZ
            return keys, views

        def wview(Wd, l, r0, nrow, c0, ncol):
            return Wd[l, r0:r0 + nrow, c0:c0 + ncol].rearrange("(k p) n -> p k n", p=128)

        def vcol(l, base, i):
            c = l * NVEC_L + base + i
            return vecs[:, c:c + 1]

        def TT(t):
            return slice(t * 512, (t + 1) * 512)

        P.add("sp", lambda e: e.dma_start(out=vecs[:], in_=vecs_d), writes=[("vecs",)], dma="c0")
        P.add("sp", lambda e: e.dma_start(out=cst[:], in_=cst_d), writes=[("cst",)], dma="c0")
        P.add("dve", lambda e: e.tensor_copy(out=cb[:], in_=cst[:, 0:512]), reads=[("cst",)], writes=[("cb",)])
        P.add("dve", lambda e: e.memset(ones_d[:], 1.0 / 1024), writes=[("ones",)])
        P.add("dve", lambda e: e.memset(ones_c[:], 1.0 / 512), writes=[("ones",)])
        P.add("dve", lambda e: e.memset(ones1[:], 1.0), writes=[("ones",)])
        P.add("dve", lambda e: e.memset(zer[:], 0.0), writes=[("ones",)])
        P.add("dve", lambda e: e.memset(eps_c[:], EPS), writes=[("ones",)])
        P.add("pool", lambda e: e.dma_start(out=selT[:].rearrange("p a b c -> p (a b c)"), in_=sel_d),
              writes=[("selst",)], dma="c1")
        TRI = cb[:, C_TRI:C_TRI + 128]
        NEGM2 = cb[:, C_MLE:C_MLE + 128]
        NEGM = cb[:, C_MLT:C_MLT + 128]
        IDN = cb[:, C_ID:C_ID + 128]
        MLT = cst[:, C_MLT:C_MLT + 128]

        def rmsnorm(l, base, out_bf=True, out_f32=None):
            P.fence("scr")
            sqbs = [scr[:, 0:2048].bitcast(BF16).rearrange("p (k n) -> p k n", k=8),
                    scr[:, 2048:4096].bitcast(BF16).rearrange("p (k n) -> p k n", k=8)]
            rstds = [scr[:, 4096:4608], scr[:, 4608:5120]]
            banks = {}

            def sqs(t):
                sqb = sqbs[t % 2]
                for dc in range(8):
                    P.add("act", lambda e, dc=dc: e.activation(out=sqb[:, dc, :], in_=h[:, dc, TT(t)], func=AF.Square),
                          reads=[("h", dc, t)], writes=[("scr", "sq", t % 2, dc)])
                bnk = rr8()
                banks[t] = bnk
                for dc in range(8):
                    P.add("pe", lambda e, dc=dc: e.matmul(ps[bnk][:], lhsT=ones_d[:], rhs=sqb[:, dc, :],
                                                          start=(dc == 0), stop=(dc == 7)),
                          reads=[("scr", "sq", t % 2, dc), ("ones",)], writes=[("ps", bnk)])

            def fin(t):
                bnk = banks[t]
                rstd = rstds[t % 2]
                rk = ("scr", "rstd", t % 2)
                P.add("act", lambda e: e.activation(out=rstd, in_=ps[bnk][:], func=AF.Ln, bias=eps_c[:, 0:1]),
                      reads=[("ps", bnk), ("ones",)], writes=[rk])
                P.add("act", lambda e: e.activation(out=rstd, in_=rstd, func=AF.Exp, scale=-0.5), reads=[rk], writes=[rk])
                for dc in range(8):
                    if out_f32 is None:
                        o = xn[:, dc, TT(t)]
                        wk = ("xn", dc, t)
                    else:
                        o = out_f32[:, dc, TT(t)]
                        wk = ("h", dc, t)
                    c = base + dc if l is None else l * NVEC_L + base + dc
                    P.add("dve", lambda e, o=o, dc=dc, c=c: e.scalar_tensor_tensor(
                        out=o, in0=h[:, dc, TT(t)], scalar=vecs[:, c:c + 1], in1=rstd, op0=ALU.mult, op1=ALU.mult),
                        reads=[("h", dc, t), rk, ("vecs",)], writes=[wk])

            sqs(0)
            for t in range(4):
                if t + 1 < 4:
                    sqs(t + 1)
                fin(t)

        def ffn(l, Wi, Wo, nbase):
            rmsnorm(l, nbase)
            P.fence("scr")
            P.fence("bigA")
            P.fence("bigB")
            sil = [scr[:, 5120:5632], scr[:, 5632:6144]]
            groups = [(0, 8), (8, 7), (15, 7)]
            sidx = 0
            for (g0, gn) in groups:
                def hid(c, t):
                    return (bigA if c < 4 else bigB)[:, c % 4, TT(t)]
                for c0 in range(0, gn, 1):
                    ncnk = 1
                    fc = g0 + c0
                    keys, (wa, wb) = wload([wview(Wi, l, 0, D, fc * 128, ncnk * 128),
                                            wview(Wi, l, 0, D, DFF + fc * 128, ncnk * 128)])
                    for ci in range(ncnk):
                        c = c0 + ci
                        for t in range(4):
                            ba, bb = rr8(), rr8()
                            for kc in range(8):
                                P.add("pe", lambda e, kc=kc, t=t, ba=ba, ci=ci, wa=wa: e.matmul(
                                    ps[ba][:], lhsT=wa[:, kc, ci * 128:(ci + 1) * 128], rhs=xn[:, kc, TT(t)],
                                    start=(kc == 0), stop=(kc == 7)),
                                    reads=[keys[0], ("xn", kc, t)], writes=[("ps", ba)])
                            for kc in range(8):
                                P.add("pe", lambda e, kc=kc, t=t, bb=bb, ci=ci, wb=wb: e.matmul(
                                    ps[bb][:], lhsT=wb[:, kc, ci * 128:(ci + 1) * 128], rhs=xn[:, kc, TT(t)],
                                    start=(kc == 0), stop=(kc == 7)),
                                    reads=[keys[1], ("xn", kc, t)], writes=[("ps", bb)])
                            st = sil[sidx % 2]
                            sk = ("scr", "sil", sidx % 2)
                            sidx += 1
                            P.add("act", lambda e, st=st, ba=ba: e.activation(out=st, in_=ps[ba][:], func=AF.Silu),
                                  reads=[("ps", ba)], writes=[sk])
                            P.add("dve", lambda e, st=st, bb=bb, c=c, t=t: e.tensor_tensor(
                                out=hid(c, t), in0=st, in1=ps[bb][:], op=ALU.mult),
                                reads=[sk, ("ps", bb)], writes=[("bigA" if c < 4 else "bigB", c % 4, t)])
                for d0 in range(0, 8, 2):
                    keys, (wo,) = wload([wview(Wo, l, g0 * 128, gn * 128, d0 * 128, 256)])
                    for di in range(2):
                        dc = d0 + di
                        for t in range(4):
                            b = rr8()
                            for c in range(gn):
                                P.add("pe", lambda e, c=c, t=t, b=b, di=di, wo=wo, gn=gn: e.matmul(
                                    ps[b][:], lhsT=wo[:, c, di * 128:(di + 1) * 128], rhs=hid(c, t),
                                    start=(c == 0), stop=(c == gn - 1)),
                                    reads=[keys[0], ("bigA" if c < 4 else "bigB", c % 4, t)], writes=[("ps", b)])
                            P.add("dve", lambda e, b=b, dc=dc, t=t: e.scalar_tensor_tensor(
                                out=h[:, dc, TT(t)], in0=ps[b][:], scalar=0.5, in1=h[:, dc, TT(t)],
                                op0=ALU.mult, op1=ALU.add),
                                reads=[("ps", b)], writes=[("h", dc, t)])

        def proj_fm(keyw, wv, evac):
            for t in range(4):
                b = rr8()
                for kc in range(8):
                    P.add("pe", lambda e, kc=kc, t=t, b=b: e.matmul(ps[b][:], lhsT=wv[:, kc, :], rhs=xn[:, kc, TT(t)],
                                                                     start=(kc == 0), stop=(kc == 7)),
                          reads=[keyw, ("xn", kc, t)], writes=[("ps", b)])
                evac(t, b)

        def conv_branch(l):
            for blk in ("bigA", "bigB", "bigC", "scr"):
                P.fence(blk)
            gpad = [bigB[:, 0:2, :].rearrange("p a n -> p (a n)"), bigB[:, 2:4, :].rearrange("p a n -> p (a n)"),
                    bigC[:, 0:2, :].rearrange("p a n -> p (a n)"), bigC[:, 2:4, :].rearrange("p a n -> p (a n)")]
            gkey = [("bigB", "g0"), ("bigB", "g1"), ("bigC", "g2"), ("bigC", "g3")]
            sg = [scr[:, 0:512], scr[:, 512:1024]]
            si = 0
            for cc in range(4):
                P.add("dve", lambda e, cc=cc: e.memset(gpad[cc][:, 0:30], 0.0), writes=[gkey[cc]])
                keys, (wv, wg) = wload([wview(Win, l, 0, D, OFF_CONV + cc * 128, 128),
                                        wview(Win, l, 0, D, OFF_CONV + 512 + cc * 128, 128)])
                for t in range(4):
                    bv, bg = rr8(), rr8()
                    for kc in range(8):
                        P.add("pe", lambda e, kc=kc, t=t, bv=bv, wv=wv: e.matmul(
                            ps[bv][:], lhsT=wv[:, kc, :], rhs=xn[:, kc, TT(t)], start=(kc == 0), stop=(kc == 7)),
                            reads=[keys[0], ("xn", kc, t)], writes=[("ps", bv)])
                    for kc in range(8):
                        P.add("pe", lambda e, kc=kc, t=t, bg=bg, wg=wg: e.matmul(
                            ps[bg][:], lhsT=wg[:, kc, :], rhs=xn[:, kc, TT(t)], start=(kc == 0), stop=(kc == 7)),
                            reads=[keys[1], ("xn", kc, t)], writes=[("ps", bg)])
                    s_ = sg[si % 2]
                    sk = ("scr", "sg", si % 2)
                    si += 1
                    P.add("act", lambda e, s_=s_, bg=bg: e.activation(out=s_, in_=ps[bg][:], func=AF.Sigmoid),
                          reads=[("ps", bg)], writes=[sk])
                    P.add("dve", lambda e, s_=s_, bv=bv, cc=cc, t=t: e.tensor_tensor(
                        out=gpad[cc][:, 30 + t * 512:30 + (t + 1) * 512], in0=s_, in1=ps[bv][:], op=ALU.mult),
                        reads=[sk, ("ps", bv)], writes=[gkey[cc]])
            P.fence("scr")
            Dgs = [scr[:, 0:1984].bitcast(BF16).rearrange("p (k n) -> p k n", k=31),
                   scr[:, 1984:3968].bitcast(BF16).rearrange("p (k n) -> p k n", k=31)]
            for cc in range(4):
                Dg = Dgs[cc % 2]
                for tap in range(31):
                    P.add("dve", lambda e, Dg=Dg, tap=tap, cc=cc: e.tensor_scalar(
                        out=Dg[:, tap, :], in0=IDN, scalar1=vcol(l, V_DW, tap * 4 + cc), scalar2=None, op0=ALU.mult),
                        reads=[("cb",), ("vecs",)], writes=[("scr", "Dg", cc % 2, tap)])
                for t in range(4):
                    bk = rr8()
                    for tap in range(31):
                        P.add("pe", lambda e, Dg=Dg, tap=tap, cc=cc, t=t, bk=bk: e.matmul(
                            ps[bk][:], lhsT=Dg[:, tap, :], rhs=gpad[cc][:, t * 512 + tap:t * 512 + tap + 512],
                            start=(tap == 0), stop=(tap == 30)),
                            reads=[("scr", "Dg", cc % 2, tap), gkey[cc]], writes=[("ps", bk)])
                    P.add("act", lambda e, cc=cc, t=t, bk=bk: e.activation(
                        out=bigA[:, cc, TT(t)], in_=ps[bk][:], func=AF.Identity, bias=vcol(l, V_DWB, cc)),
                        reads=[("ps", bk), ("vecs",)], writes=[("bigA", cc, t)])
            P.fence("scr")
            sqb = scr[:, 0:1024].bitcast(BF16).rearrange("p (k n) -> p k n", k=4)
            xc = scr[:, 1024:3072].rearrange("p (k n) -> p k n", k=4)
            rstd = scr[:, 3072:3584]
            for t in range(4):
                bm = rr8()
                for cc in range(4):
                    P.add("pe", lambda e, cc=cc, bm=bm, t=t: e.matmul(ps[bm][:], lhsT=ones_c[:], rhs=bigA[:, cc, TT(t)],
                                                                      start=(cc == 0), stop=(cc == 3)),
                          reads=[("bigA", cc, t), ("ones",)], writes=[("ps", bm)])
                for cc in range(4):
                    P.add("dve", lambda e, cc=cc, t=t, bm=bm: e.tensor_tensor(
                        out=xc[:, cc, :], in0=bigA[:, cc, TT(t)], in1=ps[bm][:], op=ALU.subtract),
                        reads=[("bigA", cc, t), ("ps", bm)], writes=[("scr", "xc", cc)])
                for cc in range(4):
                    P.add("act", lambda e, cc=cc: e.activation(out=sqb[:, cc, :], in_=xc[:, cc, :], func=AF.Square),
                          reads=[("scr", "xc", cc)], writes=[("scr", "xb", cc)])
                bv = rr8()
                for cc in range(4):
                    P.add("pe", lambda e, cc=cc, bv=bv: e.matmul(ps[bv][:], lhsT=ones_c[:], rhs=sqb[:, cc, :],
                                                                 start=(cc == 0), stop=(cc == 3)),
                          reads=[("scr", "xb", cc), ("ones",)], writes=[("ps", bv)])
                P.add("act", lambda e, bv=bv: e.activation(out=rstd, in_=ps[bv][:], func=AF.Ln, bias=eps_c[:, 0:1]),
                      reads=[("ps", bv), ("ones",)], writes=[("scr", "rstd")])
                P.add("act", lambda e: e.activation(out=rstd, in_=rstd, func=AF.Exp, scale=-0.5),
                      reads=[("scr", "rstd")], writes=[("scr", "rstd")])
                for cc in range(4):
                    P.add("dve", lambda e, cc=cc: e.scalar_tensor_tensor(
                        out=xc[:, cc, :], in0=xc[:, cc, :], scalar=vcol(l, V_LNG, cc), in1=rstd,
                        op0=ALU.mult, op1=ALU.mult),
                        reads=[("scr", "xc", cc), ("scr", "rstd"), ("vecs",)], writes=[("scr", "xc", cc)])
                    P.add("act", lambda e, cc=cc, t=t: e.activation(
                        out=bigA[:, cc, TT(t)], in_=xc[:, cc, :], func=AF.Silu, bias=vcol(l, V_LNB, cc)),
                        reads=[("scr", "xc", cc), ("vecs",)], writes=[("bigA", cc, t)])
            dump("c", bigA[:].rearrange("p a n -> p (a n)"), [("bigA", cc, t) for cc in range(4) for t in range(4)])
            P.fence("bigB")
            P.fence("bigC")

        def sb_attention(l):
            P.fence("scr")
            P.fence("bigB")
            qT = scr[:, 0:1024].bitcast(BF16)
            kT = scr[:, 1024:2048].bitcast(BF16)
            vv = scr[:, 2048:3072].bitcast(BF16).rearrange("p (t n) -> p t n", t=16)
            Eb = [scr[:, 3072:3584], scr[:, 3584:4096], scr[:, 6912:7424]]
            Gb = [scr[:, 4096:4608], scr[:, 4608:5120]]
            SPb = [scr[:, 5120:5376].bitcast(BF16), scr[:, 5376:5632].bitcast(BF16)]
            SSb = [scr[:, 5632:5888].bitcast(BF16), scr[:, 5888:6144].bitcast(BF16), scr[:, 6656:6912].bitcast(BF16)]
            Wb = [scr[:, 6144:6400].bitcast(BF16), scr[:, 6400:6656].bitcast(BF16)]
            cnt = {"e": 0, "ss": 0}
            for hp in range(4):
                keys, (wq, wk) = wload([wview(Win, l, 0, D, OFF_SBQ + hp * 128, 128),
                                        wview(Win, l, 0, D, OFF_SBK + hp * 128, 128)])
                keys2, (wv,) = wload([wview(Win, l, 0, D, OFF_SBV + hp * 128, 128)])
                keys = keys + keys2
                proj_fm(keys[0], wq, lambda t, b: P.add(
                    "act", lambda e, t=t, b=b: e.copy(out=qT[:, TT(t)], in_=ps[b][:]),
                    reads=[("ps", b)], writes=[("scr", "q", t)]))
                proj_fm(keys[1], wk, lambda t, b: P.add(
                    "dve", lambda e, t=t, b=b: e.tensor_copy(out=kT[:, TT(t)], in_=ps[b][:]),
                    reads=[("ps", b)], writes=[("scr", "k", t)]))
                for g in range(4):
                    b = rr8()
                    for ti in range(4):
                        t16 = g * 4 + ti
                        for kc in range(8):
                            P.add("pe", lambda e, kc=kc, t16=t16, ti=ti, b=b, wv=wv: e.matmul(
                                ps[b][:, ti * 128:(ti + 1) * 128], lhsT=xn[:, kc, t16 * 128:(t16 + 1) * 128],
                                rhs=wv[:, kc, :], start=(kc == 0), stop=(kc == 7)),
                                reads=[keys[2], ("xn", kc, t16 // 4)], writes=[("ps", b)])
                    P.add("act", lambda e, g=g, b=b: e.copy(
                        out=vv[:, g * 4:(g + 1) * 4, :], in_=ps[b][:].rearrange("p (t n) -> p t n", t=4)),
                        reads=[("ps", b)], writes=[("scr", "v", g)])
                pairs = []
                for j in range(2):
                    for qt in range(4):
                        nkt = (qt + 1) * 4
                        bo = rrO()
                        for idx, kt in enumerate(reversed(range(nkt))):
                            k0, q0 = kt * 128, qt * 512
                            diag = k0 >= q0
                            c0 = k0 - q0 if diag else 0
                            pairs.append(dict(j=j, qt=qt, kt=kt, k0=k0, q0=q0, diag=diag, c0=c0, first=(idx == 0),
                                              last=(kt == 0), bo=bo, pj=slice(64 * j, 64 * j + 64)))
                state = {"prev_ss": None}

                def stA(p, n):
                    i = n % 2
                    c0, q0, k0, pj, kt, qt = p["c0"], p["q0"], p["k0"], p["pj"], p["kt"], p["qt"]
                    cols = slice(c0, 512)
                    qcols = slice(q0 + c0, q0 + 512)
                    if p["first"]:
                        state["prev_ss"] = None
                    bs = rr6()
                    P.add("pe", lambda e: e.matmul(
                        ps[bs][:, cols], lhsT=kT[pj, k0:k0 + 128], rhs=qT[pj, qcols], start=True, stop=not p["diag"]),
                        reads=[("scr", "k", kt // 4), ("scr", "q", qt)], writes=[("ps", bs)])
                    if p["diag"]:
                        P.add("pe", lambda e: e.matmul(ps[bs][:, c0:c0 + 128], lhsT=IDN, rhs=NEGM, start=False, stop=True),
                              reads=[("cb",)], writes=[("ps", bs)])
                    ie = n % 3
                    E, SP = Eb[ie], SPb[i]
                    P.add("act", lambda e: e.activation(out=E[:, cols], in_=ps[bs][:, cols], func=AF.Exp, scale=0.125),
                          reads=[("ps", bs)], writes=[("scr", "E", ie)])
                    P.add("act", lambda e: e.activation(out=SP[:, cols], in_=E[:, cols], func=AF.Ln, bias=1.0),
                          reads=[("scr", "E", ie)], writes=[("scr", "SP", i)])
                    p["pss"] = state["prev_ss"]
                    if kt > 0:
                        si = n % 3
                        SSn = SSb[si]
                        nk = ("scr", "SS", si)
                        if state["prev_ss"] is None:
                            P.add("pool", lambda e: e.tensor_copy(out=SSn[:, cols], in_=SP[:, cols]),
                                  reads=[("scr", "SP", i)], writes=[nk])
                        else:
                            pss, pk, pc0 = state["prev_ss"]
                            if pc0 > c0:
                                P.add("pool", lambda e: e.tensor_copy(out=SSn[:, c0:pc0], in_=SP[:, c0:pc0]),
                                      reads=[("scr", "SP", i)], writes=[nk])
                            P.add("pool", lambda e: e.tensor_tensor(
                                out=SSn[:, pc0:512], in0=SP[:, pc0:512], in1=pss[:, pc0:512], op=ALU.add),
                                reads=[("scr", "SP", i), pk], writes=[nk])
                        state["prev_ss"] = (SSn, nk, c0)

                def stB(p, n):
                    i = n % 2
                    c0 = p["c0"]
                    cols = slice(c0, 512)
                    ie = n % 3
                    E, SP, G, Wt = Eb[ie], SPb[i], Gb[i], Wb[i]
                    bc = rr6()
                    pss = p["pss"]
                    P.add("pe", lambda e: e.matmul(ps[bc][:, cols], lhsT=TRI, rhs=SP[:, cols], start=True,
                                                   stop=(pss is None)),
                          reads=[("scr", "SP", i), ("cb",)], writes=[("ps", bc)])
                    if pss is not None:
                        ssb, pk, pc0 = pss
                        P.add("pe", lambda e: e.matmul(ps[bc][:, pc0:512], lhsT=ones1[:], rhs=ssb[:, pc0:512],
                                                       start=False, stop=True),
                              reads=[pk, ("ones",)], writes=[("ps", bc)])
                    dummies(NDUM["sb"])
                    P.add("act", lambda e: e.activation(out=G[:, cols], in_=ps[bc][:, cols], func=AF.Exp, scale=-1.0),
                          reads=[("ps", bc)], writes=[("scr", "G", i)])
                    P.add("dve", lambda e: e.tensor_tensor(out=Wt[:, cols], in0=E[:, cols], in1=G[:, cols], op=ALU.mult),
                          reads=[("scr", "E", ie), ("scr", "G", i)], writes=[("scr", "W", i)])

                def stC(p, n, hp=hp):
                    i = n % 2
                    c0, bo, pj, kt, qt = p["c0"], p["bo"], p["pj"], p["kt"], p["qt"]
                    cols = slice(c0, 512)
                    Wt = Wb[i]
                    if p["first"]:
                        P.add("pe", lambda e: e.matmul(ps[bo][0:64, :], lhsT=zer[:, 0:64], rhs=cb[:, 0:512],
                                                       start=True, stop=False),
                              reads=[("ones",), ("cb",)], writes=[("ps", bo)])
                    P.add("pe", lambda e: e.matmul(ps[bo][0:64, cols], lhsT=vv[:, kt, pj], rhs=Wt[:, cols],
                                                   start=False, stop=p["last"]),
                          reads=[("scr", "W", i), ("scr", "v", kt // 4)], writes=[("ps", bo)])
                    if p["last"]:
                        P.add("dve", lambda e: e.tensor_copy(out=bigB[pj, hp, TT(qt)], in_=ps[bo][0:64, :]),
                              reads=[("ps", bo)], writes=[("bigB", hp, qt, pj.start)])

                NPR = len(pairs)
                for n in range(NPR + 2):
                    if n < NPR:
                        stA(pairs[n], n)
                    if 1 <= n <= NPR:
                        stB(pairs[n - 1], n - 1)
                    if n >= 2:
                        stC(pairs[n - 2], n - 2)

        def moba_attention(l):
            P.fence("scr")
            P.fence("bigC")
            P.add("dve", lambda e: e.memset(vext, 1.0), writes=[("scr", "vext")])
            for j in range(2):
                P.add("pool", lambda e, j=j: e.dma_start(out=kaug[64:75, j, :], in_=kst_d[64:75, :]),
                      writes=[("scr", "kst", j)], dma="c1")
            ksf = scr[0:64, 0:16].rearrange("p (j n) -> p j n", j=2)
            gm = scr[:, 64:128].rearrange("p (g n) -> p g n", g=8)
            cmp_ = scr[:, 128:640].rearrange("p (g n m) -> p g n m", g=8, n=8)
            cntt = scr[:, 640:704].rearrange("p (g n) -> p g n", g=8)
            t1 = scr[:, 704:768].rearrange("p (g n) -> p g n", g=8)
            rden = scr[0:64, 768:1280]
            Pm = [scr[:, 1280:1536].bitcast(BF16), scr[:, 1536:1792].bitcast(BF16), scr[:, 1792:2048].bitcast(BF16)]
            cnt = {"p": 0}
            GM = cst[:, C_GM:C_GM + 128].rearrange("p (o j n) -> p o j n", o=8, j=2)
            LL = cst[:, C_L:C_L + 64].rearrange("p (o n) -> p o n", o=8)
            for hp in range(4):
                keys, (wq, wk) = wload([wview(Win, l, 0, D, OFF_MBQ + hp * 128, 128),
                                        wview(Win, l, 0, D, OFF_MBK + hp * 128, 128)])
                keys2, (wv,) = wload([wview(Win, l, 0, D, OFF_MBV + hp * 128, 128)])
                keys = keys + keys2

                def evq(t, b):
                    P.add("act", lambda e, t=t, b=b: e.copy(out=qaug[0:64, 0, TT(t)], in_=ps[b][0:64, :]),
                          reads=[("ps", b)], writes=[("scr", "qaug", 0, t)])
                    P.add("dve", lambda e, t=t, b=b: e.tensor_copy(out=qaug[0:64, 1, TT(t)], in_=ps[b][64:128, :]),
                          reads=[("ps", b)], writes=[("scr", "qaug", 1, t)])
                proj_fm(keys[0], wq, evq)

                def evk(t, b):
                    P.add("act", lambda e, t=t, b=b: e.copy(out=kaug[0:64, 0, TT(t)], in_=ps[b][0:64, :]),
                          reads=[("ps", b)], writes=[("scr", "kaug", 0, t)])
                    P.add("dve", lambda e, t=t, b=b: e.tensor_copy(out=kaug[0:64, 1, TT(t)], in_=ps[b][64:128, :]),
                          reads=[("ps", b)], writes=[("scr", "kaug", 1, t)])
                    for j in range(2):
                        P.add("dve", lambda e, t=t, b=b, j=j: e.tensor_reduce(
                            out=ksf[:, j, 2 * t:2 * t + 2], in_=ps[b][64 * j:64 * j + 64, :].rearrange("p (a n) -> p a n", a=2),
                            axis=AX.X, op=ALU.add),
                            reads=[("ps", b)], writes=[("scr", "ksf", j, t)])
                proj_fm(keys[1], wk, evk)
                P.add("dve", lambda e: e.tensor_copy(out=ksumb[:], in_=ksf),
                      reads=[("scr", "ksf", j, t) for j in range(2) for t in range(4)], writes=[("ksumb",)])
                for g in range(4):
                    b = rr8()
                    for ti in range(4):
                        t16 = g * 4 + ti
                        for kc in range(8):
                            P.add("pe", lambda e, kc=kc, t16=t16, ti=ti, b=b, wv=wv: e.matmul(
                                ps[b][:, ti * 128:(ti + 1) * 128], lhsT=xn[:, kc, t16 * 128:(t16 + 1) * 128],
                                rhs=wv[:, kc, :], start=(kc == 0), stop=(kc == 7)),
                                reads=[keys[2], ("xn", kc, t16 // 4)], writes=[("ps", b)])
                    P.add("act", lambda e, g=g, b=b: e.copy(
                        out=vext[:, g * 4:(g + 1) * 4, :, 0:64],
                        in_=ps[b][:].rearrange("p (t j n) -> p t j n", t=4, j=2)),
                        reads=[("ps", b), ("scr", "vext")], writes=[("scr", "vext", g)])
                for qt in range(4):
                    bg = rr8()
                    for ti in range(4):
                        t16 = qt * 4 + ti
                        for j in range(2):
                            g8 = ti * 2 + j
                            P.add("pe", lambda e, bg=bg, g8=g8, j=j, t16=t16: e.matmul(
                                ps[bg][:, g8 * 8:(g8 + 1) * 8], lhsT=qaug[0:64, j, t16 * 128:(t16 + 1) * 128],
                                rhs=ksumb[:, j, :], start=True, stop=True),
                                reads=[("scr", "qaug", j, qt), ("ksumb",)], writes=[("ps", bg)])
                    for ti in range(4):
                        own = (qt * 4 + ti) // 2
                        P.add("dve", lambda e, bg=bg, ti=ti, own=own: e.tensor_tensor(
                            out=gm[:, 2 * ti:2 * ti + 2, :],
                            in0=ps[bg][:, 16 * ti:16 * ti + 16].rearrange("p (j n) -> p j n", j=2),
                            in1=GM[:, own, :, :], op=ALU.add),
                            reads=[("ps", bg), ("cst",)], writes=[("scr", "gm")])
                    gap = [list(a) for a in gm.ap]
                    gm_m = bass.AP(gm.tensor, gm.offset, [gap[0], gap[1], [0, 8], gap[2]])
                    gm_n = bass.AP(gm.tensor, gm.offset, [gap[0], gap[1], gap[2], [0, 8]])
                    P.add("dve", lambda e, gm_m=gm_m, gm_n=gm_n: e.tensor_tensor(
                        out=cmp_, in0=gm_m, in1=gm_n, op=ALU.is_gt),
                        reads=[("scr", "gm")], writes=[("scr", "cmp")])
                    P.add("dve", lambda e: e.tensor_reduce(out=cntt, in_=cmp_, axis=AX.X, op=ALU.add),
                          reads=[("scr", "cmp")], writes=[("scr", "cnt")])
                    P.add("dve", lambda e: e.tensor_scalar(out=t1, in0=cntt, scalar1=2.5, scalar2=BIG,
                                                           op0=ALU.is_lt, op1=ALU.mult),
                          reads=[("scr", "cnt")], writes=[("scr", "t1")])
                    for ti in range(4):
                        own = (qt * 4 + ti) // 2
                        for j in range(2):
                            hh = 2 * hp + j
                            P.add("dve", lambda e, ti=ti, j=j, hh=hh, own=own: e.scalar_tensor_tensor(
                                out=selT[:, hh, ti, 64:72], in0=t1[:, 2 * ti + j, :], scalar=-BIG, in1=LL[:, own, :],
                                op0=ALU.add, op1=ALU.max),
                                reads=[("scr", "t1"), ("cst",), ("selst",)], writes=[("selT", hh, ti)])
                    for j in range(2):
                        hh = 2 * hp + j
                        bt = rr8()
                        for ti in range(4):
                            P.add("pe", lambda e, bt=bt, ti=ti, hh=hh: e.matmul(
                                ps[bt][0:80, ti * 128:(ti + 1) * 128], lhsT=selT[:, hh, ti, :], rhs=IDN,
                                start=True, stop=True),
                                reads=[("selT", hh, ti), ("cb",)], writes=[("ps", bt)])
                        P.add("act", lambda e, bt=bt, j=j, qt=qt: e.copy(out=qaug[64:75, j, TT(qt)], in_=ps[bt][64:75, :]),
                              reads=[("ps", bt)], writes=[("scr", "qst", j, qt)])
                pairs = []
                for j in range(2):
                    for qt in range(4):
                        nkt = (qt + 1) * 4
                        bo = rrO()
                        for kt in range(nkt):
                            k0, q0 = kt * 128, qt * 512
                            diag = k0 >= q0
                            c0 = k0 - q0 if diag else 0
                            pairs.append(dict(j=j, qt=qt, kt=kt, k0=k0, q0=q0, diag=diag, c0=c0, first=(kt == 0),
                                              last=(kt == nkt - 1), bo=bo, hh=2 * hp + j))

                def mA(p, n):
                    c0, q0, k0, j, kt, qt = p["c0"], p["q0"], p["k0"], p["j"], p["kt"], p["qt"]
                    cols = slice(c0, 512)
                    qcols = slice(q0 + c0, q0 + 512)
                    bs = rr6()
                    p["bs"] = bs
                    P.add("pe", lambda e: e.matmul(
                        ps[bs][:, cols], lhsT=kaug[0:75, j, k0:k0 + 128], rhs=qaug[0:75, j, qcols],
                        start=True, stop=not p["diag"]),
                        reads=[("scr", "kaug", j, kt // 4), ("scr", "kst", j), ("scr", "qaug", j, qt), ("scr", "qst", j, qt)],
                        writes=[("ps", bs)])
                    if p["diag"]:
                        P.add("pe", lambda e: e.matmul(ps[bs][:, c0:c0 + 128], lhsT=IDN, rhs=NEGM2, start=False, stop=True),
                              reads=[("cb",)], writes=[("ps", bs)])

                def mB(p, n):
                    i = n % 3
                    c0, bs = p["c0"], p["bs"]
                    cols = slice(c0, 512)
                    pm = Pm[i]
                    biasc = float(-SLOPES[p["hh"]] * (p["q0"] - p["k0"]))
                    P.add("act", lambda e: e.activation(
                        out=pm[:, cols], in_=ps[bs][:, cols], func=AF.Exp, scale=0.125, bias=biasc),
                        reads=[("ps", bs)], writes=[("scr", "Pm", i)])

                def mC(p, n, hp=hp):
                    i = n % 3
                    c0, bo, j, kt, qt = p["c0"], p["bo"], p["j"], p["kt"], p["qt"]
                    cols = slice(c0, 512)
                    pm = Pm[i]
                    pj = slice(64 * j, 64 * j + 64)
                    P.add("pe", lambda e: e.matmul(ps[bo][:, cols], lhsT=vext[:, kt, j, :], rhs=pm[:, cols],
                                                   start=p["first"], stop=p["last"]),
                          reads=[("scr", "Pm", i), ("scr", "vext", kt // 4), ("scr", "vext")], writes=[("ps", bo)])
                    if p["last"]:
                        P.add("dve", lambda e: e.reciprocal(out=rden, in_=ps[bo][64:128, :]),
                              reads=[("ps", bo)], writes=[("scr", "rden")])
                        P.add("dve", lambda e: e.tensor_tensor(
                            out=bigC[pj, hp, TT(qt)], in0=ps[bo][0:64, :], in1=rden, op=ALU.mult),
                            reads=[("ps", bo), ("scr", "rden")], writes=[("bigC", hp, qt, pj.start)])

                NPR = len(pairs)
                for n in range(NPR + 2):
                    if n < NPR:
                        mA(pairs[n], n)
                    if 1 <= n <= NPR:
                        mB(pairs[n - 1], n - 1)
                    if n >= 2:
                        mC(pairs[n - 2], n - 2)

        def mix_out(l, have):
            P.fence("scr")
            mixed = scr[:, 0:4096].bitcast(BF16).rearrange("p (k n) -> p k n", k=8)
            gtb = [scr[:, 4096 + 512 * i:4608 + 512 * i] for i in range(2)]
            ttb = [[scr[:, 5120 + 512 * (3 * a + i):5632 + 512 * (3 * a + i)] for i in range(3)] for a in range(2)]
            gcnt = {"i": 0}
            srcs = [("sb", bigB, WA, "bigB"), ("mb", bigC, WB, "bigC"), ("conv", bigA, WC, "bigA")]
            for half in range(2):
                for dc in range(8):
                    kg1, gw1 = wload([wview(Win, l, 0, D, OFF_GATE + br * D + dc * 128, 128) for br in range(2)])
                    kg2, gw2 = wload([wview(Win, l, 0, D, OFF_GATE + 2 * D + dc * 128, 128),
                                      wview(WA, l, 0, 512, dc * 128, 128), wview(WB, l, 0, 512, dc * 128, 128)])
                    kg3, gw3 = wload([wview(WC, l, 0, 512, dc * 128, 128)])
                    keysg, gw = kg1 + kg2[0:1], gw1 + gw2[0:1]
                    keysy, yw = kg2[1:3] + kg3, gw2[1:3] + gw3
                    for t2 in range(2):
                        t = half * 2 + t2
                        tt_ = ttb[t2]
                        for br, (nm, ob, W_, blk) in enumerate(srcs):
                            if nm not in have:
                                continue
                            bgate = rr8()
                            for kc in range(8):
                                P.add("pe", lambda e, kc=kc, t=t, bgate=bgate, gwb=gw[br]: e.matmul(
                                    ps[bgate][:], lhsT=gwb[:, kc, :], rhs=xn[:, kc, TT(t)],
                                    start=(kc == 0), stop=(kc == 7)),
                                    reads=[keysg[br], ("xn", kc, t)], writes=[("ps", bgate)])
                            by = rr8()
                            for kc in range(4):
                                if nm == "conv":
                                    rk = [("bigA", kc, t)]
                                else:
                                    rk = [(blk, kc, t, 0), (blk, kc, t, 64)]
                                P.add("pe", lambda e, kc=kc, t=t, by=by, ywb=yw[br], ob=ob: e.matmul(
                                    ps[by][:], lhsT=ywb[:, kc, :], rhs=ob[:, kc, TT(t)],
                                    start=(kc == 0), stop=(kc == 3)),
                                    reads=[keysy[br]] + rk, writes=[("ps", by)])
                            gi = gcnt["i"] % 2
                            gcnt["i"] += 1
                            P.add("act", lambda e, br=br, bgate=bgate, dc=dc, gi=gi: e.activation(
                                out=gtb[gi], in_=ps[bgate][:], func=AF.Sigmoid, bias=vcol(l, V_GB, br * 8 + dc)),
                                reads=[("ps", bgate), ("vecs",)], writes=[("scr", "gt", gi)])
                            P.add("dve", lambda e, br=br, by=by, gi=gi, tt_=tt_: e.tensor_tensor(
                                out=tt_[br], in0=gtb[gi], in1=ps[by][:], op=ALU.mult),
                                reads=[("scr", "gt", gi), ("ps", by)], writes=[("scr", "tt", t2, br)])
                        live = [br for br, s_ in enumerate(srcs) if s_[0] in have]
                        mo = mixed[:, dc, t2 * 512:(t2 + 1) * 512]
                        mk_ = ("scr", "mixed", dc, t2)
                        if len(live) == 1:
                            P.add("dve", lambda e, mo=mo, a=live[0], tt_=tt_: e.tensor_copy(out=mo, in_=tt_[a]),
                                  reads=[("scr", "tt", t2, live[0])], writes=[mk_])
                        elif len(live) == 2:
                            P.add("dve", lambda e, mo=mo, a=live[0], b_=live[1], tt_=tt_: e.tensor_tensor(
                                out=mo, in0=tt_[a], in1=tt_[b_], op=ALU.add),
                                reads=[("scr", "tt", t2, live[0]), ("scr", "tt", t2, live[1])], writes=[mk_])
                        else:
                            P.add("dve", lambda e, tt_=tt_: e.tensor_tensor(out=tt_[0], in0=tt_[0], in1=tt_[1], op=ALU.add),
                                  reads=[("scr", "tt", t2, 0), ("scr", "tt", t2, 1)], writes=[("scr", "tt", t2, 0)])
                            P.add("dve", lambda e, mo=mo, tt_=tt_: e.tensor_tensor(out=mo, in0=tt_[0], in1=tt_[2], op=ALU.add),
                                  reads=[("scr", "tt", t2, 0), ("scr", "tt", t2, 2)], writes=[mk_])
                dump("mixed", scr[:, 0:4096].bitcast(BF16), [("scr", "mixed", dc_, t_) for dc_ in range(8) for t_ in range(2)])
                for d0 in range(0, 8, 2):
                    keys, (wo,) = wload([wview(WO, l, 0, D, d0 * 128, 256)])
                    for di in range(2):
                        dc = d0 + di
                        for t2 in range(2):
                            t = half * 2 + t2
                            b = rr8()
                            for kc in range(8):
                                P.add("pe", lambda e, kc=kc, t2=t2, b=b, di=di, wo=wo: e.matmul(
                                    ps[b][:], lhsT=wo[:, kc, di * 128:(di + 1) * 128],
                                    rhs=mixed[:, kc, t2 * 512:(t2 + 1) * 512], start=(kc == 0), stop=(kc == 7)),
                                    reads=[keys[0], ("scr", "mixed", kc, t2)], writes=[("ps", b)])
                            P.add("dve", lambda e, b=b, dc=dc, t=t: e.tensor_tensor(
                                out=h[:, dc, TT(t)], in0=ps[b][:], in1=h[:, dc, TT(t)], op=ALU.add),
                                reads=[("ps", b)], writes=[("h", dc, t)])

        outs = []
        for s_i in range(nseq):
            for dc in range(8):
                P.add("sp", lambda e, dc=dc, s_i=s_i: e.dma_start(out=h[:, dc, :], in_=xT[s_i, dc * 128:(dc + 1) * 128, :]),
                      writes=[("h", dc, t) for t in range(4)], dma="x%d" % dc)
            for l in range(depth):
                if "ffn1" in stages:
                    ffn(l, W1i, W1o, V_FFN1N)
                if "mix" in stages:
                    rmsnorm(l, V_MIXN)
                    if "conv" in mix_parts:
                        conv_branch(l)
                    if "sb" in mix_parts:
                        sb_attention(l)
                    if "mb" in mix_parts:
                        moba_attention(l)
                    mix_out(l, mix_parts)
                if "ffn2" in stages:
                    ffn(l, W2i, W2o, V_FFN2N)
            if "final" in stages:
                rmsnorm(None, depth * NVEC_L, out_f32=h)
            for dc in range(8):
                outs.append(P.add("sp", lambda e, dc=dc, s_i=s_i: e.dma_start(
                    out=outT[s_i, dc * 128:(dc + 1) * 128, :], in_=h[:, dc, :]),
                    reads=[("h", dc, t) for t in range(4)], dma="o%d" % dc))
        P.emit(nc, final_waits=outs[-8:] + dbg_ops)
    return nc


_CACHE = {}


def kernel(**inputs):
    x = np.asarray(inputs["x"], np.float32)
    B = x.shape[0]
    per = B // NCORES
    cst, kst, sel = host_consts()
    vecs = host_vecs(inputs, DEPTH_FULL)
    nc = build(per, DEPTH_FULL)
    shared = {k: np.ascontiguousarray(np.asarray(inputs[k], np.float32)) for k in
              ("ffn1_w_in", "ffn1_w_out", "ffn2_w_in", "ffn2_w_out", "w_in", "sb_w_out", "mb_w_out",
               "conv_w_out", "w_o")}
    shared.update({"vecs": vecs, "cst": cst, "kst": kst, "selst": sel})
    in_maps = []
    for c in range(NCORES):
        xs = x[c * per:(c + 1) * per]
        m = dict(shared)
        m["xT"] = np.ascontiguousarray(xs.transpose(0, 2, 1))
        in_maps.append(m)
    res = run_bass_kernel_spmd(nc, in_maps, core_ids=list(range(NCORES)))
    out = np.empty((B, S, D), np.float32)
    for c in range(NCORES):
        out[c * per:(c + 1) * per] = res.results[c]["outT"].transpose(0, 2, 1)
    return out
```
